# Optimizing a Trainium2 kernel written in Bass

```python
import math
import jax, jax.numpy as jnp
from jax import lax
import numpy as np

D_MODEL = 1024
BATCH = 8
SEQ = 4096
DEPTH = 1

MIX_WIDTH = D_MODEL
N_MLSTM_HEADS = 4
MLSTM_HEAD_DIM = 128
MLSTM_WIDTH = N_MLSTM_HEADS * MLSTM_HEAD_DIM
N_SB_HEADS = 8
SB_HEAD_DIM = 64
SB_WIDTH = N_SB_HEADS * SB_HEAD_DIM
CONV_WIDTH = 4
MLSTM_CHUNK = 128
SB_BLOCK = 128
PROJ_WIDTH = 4 * MLSTM_WIDTH + 2 * N_MLSTM_HEADS + 3 * SB_WIDTH
PROJ_SPLITS = (2 * MLSTM_WIDTH, 3 * MLSTM_WIDTH, 4 * MLSTM_WIDTH,
               4 * MLSTM_WIDTH + N_MLSTM_HEADS, 4 * MLSTM_WIDTH + 2 * N_MLSTM_HEADS)
MEM_TOKENS = 256
N_XATTN_HEADS = 4
XATTN_HEAD_DIM = D_MODEL // N_XATTN_HEADS
PEER_HEADS = 8
PEER_N_KEYS = 128
PEER_N_EXPERTS = PEER_N_KEYS * PEER_N_KEYS
PEER_TOPK = 16
PEER_QUERY_DIM = 256
PEER_HALF = PEER_QUERY_DIM // 2
PEER_CHUNK = 128
EPS = 1e-6

kernel_name = 'hymba_mlstm_stickbreak_peer_block'


def rms_norm(x, g):
    xf = x.astype(jnp.float32)
    y = xf * lax.rsqrt(jnp.mean(xf * xf, axis=-1, keepdims=True) + EPS) * g.astype(jnp.float32)
    return y.astype(x.dtype)


def head_rms_norm(a, g):
    H, dh = a.shape[1], a.shape[3]
    y = a * lax.rsqrt(jnp.mean(a * a, axis=-1, keepdims=True) + EPS)
    return y * g.astype(jnp.float32).reshape(H, 1, dh)


def split_heads(a, n):
    B_, S, W = a.shape
    return a.reshape(B_, S, n, W // n).transpose(0, 2, 1, 3)


def merge_heads(a):
    B_, H, S, dh = a.shape
    return a.transpose(0, 2, 1, 3).reshape(B_, S, H * dh)


def causal_depthwise_conv(a, w, b):
    C = a.shape[-1]
    out = lax.conv_general_dilated(a, w[:, None, :].astype(a.dtype), window_strides=(1,),
                                   padding=[(CONV_WIDTH - 1, 0)],
                                   dimension_numbers=('NWC', 'WIO', 'NWC'),
                                   feature_group_count=C)
    return out + b.astype(a.dtype)


def mlstm_chunkwise(q, k, v, log_i, log_f):
    B_, H, S, dh = q.shape
    L = MLSTM_CHUNK
    nc = S // L

    def to_chunks(a):
        return jnp.moveaxis(a.reshape(B_, H, nc, L, *a.shape[3:]), 2, 0)

    xs = (to_chunks(q), to_chunks(k), to_chunks(v), to_chunks(log_i), to_chunks(log_f))
    causal = jnp.tril(jnp.ones((L, L), dtype=bool))

    def step(carry, inp):
        C, n, m = carry
        qb, kb, vb, ib, fb = inp
        b = jnp.cumsum(fb, axis=-1)
        g = b + m[..., None]
        Dm = jnp.where(causal, b[..., :, None] - b[..., None, :] + ib[..., None, :], -jnp.inf)
        m_t = jnp.maximum(g, jnp.max(Dm, axis=-1))
        W = jnp.exp(Dm - m_t[..., None])
        inter = jnp.exp(g - m_t)
        qk = jnp.einsum('bhtd,bhsd->bhts', qb, kb) * W
        num = inter[..., None] * jnp.einsum('bhvk,bhtk->bhtv', C, qb) + jnp.einsum('bhts,bhsv->bhtv', qk, vb)
        den = inter * jnp.einsum('bhk,bhtk->bht', n, qb) + jnp.sum(qk, axis=-1)
        h = num / jnp.maximum(jnp.abs(den), jnp.exp(-m_t))[..., None]
        F = b[..., -1]
        a = F[..., None] - b + ib
        m_new = jnp.maximum(F + m, jnp.max(a, axis=-1))
        decay = jnp.exp(F + m - m_new)
        w_s = jnp.exp(a - m_new[..., None])
        C_new = decay[..., None, None] * C + jnp.einsum('bhsv,bhsk->bhvk', vb * w_s[..., None], kb)
        n_new = decay[..., None] * n + jnp.einsum('bhs,bhsk->bhk', w_s, kb)
        return (C_new, n_new, m_new), h

    init = (jnp.zeros((B_, H, dh, dh), jnp.float32), jnp.zeros((B_, H, dh), jnp.float32),
            jnp.zeros((B_, H), jnp.float32))
    _, hs = lax.scan(step, init, xs)
    return jnp.moveaxis(hs, 0, 2).reshape(B_, H, S, dh)


def stick_breaking_attention(q, k, v):
    B_, H, S, dh = q.shape
    L = SB_BLOCK
    scale = dh ** -0.5
    outs = []
    for blk in range(S // L):
        n_prefix = (blk + 1) * L
        q_blk = q[:, :, blk * L:(blk + 1) * L]
        k_pre = k[:, :, :n_prefix]
        v_pre = v[:, :, :n_prefix]
        z = jnp.einsum('bhtd,bhsd->bhts', q_blk, k_pre) * scale
        t_idx = blk * L + jnp.arange(L)[:, None]
        s_idx = jnp.arange(n_prefix)[None, :]
        strict = s_idx < t_idx
        log_1m_beta = jnp.where(strict, jax.nn.log_sigmoid(-z), 0.0)
        log_a = z + lax.cumsum(log_1m_beta, axis=3, reverse=True)
        A = jnp.exp(jnp.where(strict, log_a, -jnp.inf))
        outs.append(jnp.einsum('bhts,bhsd->bhtd', A, v_pre))
    return jnp.concatenate(outs, axis=2)


def parallel_mixer(h, w_in, conv_w, conv_b, igate_b, fgate_b, mlstm_norm_g, sb_norm_g, w_out):
    dtype = h.dtype
    f32 = jnp.float32
    proj = h @ w_in
    qk_m, v_m, o_m, i_pre, f_pre, qkv_s = jnp.split(proj, PROJ_SPLITS, axis=-1)
    qk_m = jax.nn.silu(causal_depthwise_conv(qk_m, conv_w, conv_b))
    q_m, k_m = jnp.split(qk_m, 2, axis=-1)
    q_m = split_heads(q_m, N_MLSTM_HEADS).astype(f32)
    k_m = split_heads(k_m, N_MLSTM_HEADS).astype(f32) * (MLSTM_HEAD_DIM ** -0.5)
    v_m = split_heads(v_m, N_MLSTM_HEADS).astype(f32)
    log_i = jnp.swapaxes((i_pre + igate_b).astype(f32), 1, 2)
    log_f = jax.nn.log_sigmoid(jnp.swapaxes((f_pre + fgate_b).astype(f32), 1, 2))
    h_m = mlstm_chunkwise(q_m, k_m, v_m, log_i, log_f)
    h_m = jax.nn.sigmoid(split_heads(o_m, N_MLSTM_HEADS).astype(f32)) * h_m
    h_m = head_rms_norm(h_m, mlstm_norm_g)
    q_s, k_s, v_s = jnp.split(qkv_s, 3, axis=-1)
    h_s = stick_breaking_attention(split_heads(q_s, N_SB_HEADS).astype(f32),
                                   split_heads(k_s, N_SB_HEADS).astype(f32),
                                   split_heads(v_s, N_SB_HEADS).astype(f32))
    h_s = head_rms_norm(h_s, sb_norm_g)
    mixed = jnp.concatenate([merge_heads(h_m), merge_heads(h_s)], axis=-1).astype(dtype)
    return mixed @ w_out


def memory_cross_attention(h, mem_n, wq, wkv, wo):
    B_, S, _ = h.shape
    M = mem_n.shape[1]
    q = (h @ wq).reshape(B_, S, N_XATTN_HEADS, XATTN_HEAD_DIM).astype(jnp.float32)
    k, v = jnp.split(mem_n @ wkv, 2, axis=-1)
    k = k.reshape(B_, M, N_XATTN_HEADS, XATTN_HEAD_DIM).astype(jnp.float32)
    v = v.reshape(B_, M, N_XATTN_HEADS, XATTN_HEAD_DIM).astype(jnp.float32)
    p = jax.nn.softmax(jnp.einsum('bshd,bmhd->bhsm', q, k) * (XATTN_HEAD_DIM ** -0.5), axis=-1)
    o = jnp.einsum('bhsm,bmhd->bshd', p, v).reshape(B_, S, N_XATTN_HEADS * XATTN_HEAD_DIM)
    return o.astype(h.dtype) @ wo


def peer_ffn(h, wq, subkeys, u, v):
    B_, S, D = h.shape
    T = B_ * S
    xt = h.reshape(T, D)
    q = (xt @ wq).reshape(T, PEER_HEADS, 2, PEER_HALF)
    s = jnp.einsum('thpc,hpnc->thpn', q, subkeys).astype(jnp.float32)
    s_top, i_top = lax.top_k(s, PEER_TOPK)
    cand = s_top[:, :, 0, :, None] + s_top[:, :, 1, None, :]
    cand_idx = i_top[:, :, 0, :, None] * PEER_N_KEYS + i_top[:, :, 1, None, :]
    best, pos = lax.top_k(cand.reshape(T, PEER_HEADS, PEER_TOPK * PEER_TOPK), PEER_TOPK)
    experts = jnp.take_along_axis(cand_idx.reshape(T, PEER_HEADS, PEER_TOPK * PEER_TOPK), pos, axis=-1)
    gates = jax.nn.softmax(best, axis=-1).astype(h.dtype)
    nb = T // PEER_CHUNK
    E = PEER_HEADS * PEER_TOPK

    def block(args):
        xb, eb, gb = args
        ub = u[eb]
        vb = v[eb]
        act = jax.nn.gelu(jnp.einsum('cd,ced->ce', xb, ub), approximate=False) * gb
        return jnp.einsum('ce,ced->cd', act, vb)

    y = lax.map(block, (xt.reshape(nb, PEER_CHUNK, D), experts.reshape(nb, PEER_CHUNK, E),
                        gates.reshape(nb, PEER_CHUNK, E)))
    return y.reshape(B_, S, D)


def setup_inputs(seed: int = 0) -> dict:
    key = jax.random.key(seed)
    ks = jax.random.split(key, 24)
    f32 = jnp.float32

    def nrm(k, shape, scale):
        return jax.random.normal(k, shape, f32) * scale

    def gain(k, shape):
        return 1.0 + 0.02 * jax.random.normal(k, shape, f32)

    return {
        'x': nrm(ks[0], (BATCH, SEQ, D_MODEL), 1.0),
        'mem': nrm(ks[1], (BATCH, MEM_TOKENS, D_MODEL), 1.0),
        'mix_norm_g': gain(ks[2], (DEPTH, D_MODEL)),
        'w_in': nrm(ks[3], (DEPTH, D_MODEL, PROJ_WIDTH), D_MODEL ** -0.5),
        'conv_w': nrm(ks[4], (DEPTH, CONV_WIDTH, 2 * MLSTM_WIDTH), CONV_WIDTH ** -0.5),
        'conv_b': nrm(ks[5], (DEPTH, 2 * MLSTM_WIDTH), 0.02),
        'igate_b': nrm(ks[6], (DEPTH, N_MLSTM_HEADS), 0.1),
        'fgate_b': jnp.linspace(3.0, 6.0, N_MLSTM_HEADS, dtype=f32)[None, :] + nrm(ks[7], (DEPTH, N_MLSTM_HEADS), 0.1),
        'mlstm_norm_g': gain(ks[8], (DEPTH, MLSTM_WIDTH)),
        'sb_norm_g': gain(ks[9], (DEPTH, SB_WIDTH)),
        'w_out': nrm(ks[10], (DEPTH, MIX_WIDTH, D_MODEL), MIX_WIDTH ** -0.5),
        'xattn_norm_g': gain(ks[11], (DEPTH, D_MODEL)),
        'mem_norm_g': gain(ks[12], (DEPTH, D_MODEL)),
        'xattn_wq': nrm(ks[13], (DEPTH, D_MODEL, D_MODEL), D_MODEL ** -0.5),
        'xattn_wkv': nrm(ks[14], (DEPTH, D_MODEL, 2 * D_MODEL), D_MODEL ** -0.5),
        'xattn_wo': nrm(ks[15], (DEPTH, D_MODEL, D_MODEL), D_MODEL ** -0.5),
        'ffn_norm_g': gain(ks[16], (DEPTH, D_MODEL)),
        'peer_wq': nrm(ks[17], (DEPTH, D_MODEL, PEER_HEADS * PEER_QUERY_DIM), D_MODEL ** -0.5),
        'peer_subkeys': nrm(ks[18], (DEPTH, PEER_HEADS, 2, PEER_N_KEYS, PEER_HALF), PEER_HALF ** -0.5),
        'peer_u': nrm(ks[19], (DEPTH, PEER_N_EXPERTS, D_MODEL), D_MODEL ** -0.5),
        'peer_v': nrm(ks[20], (DEPTH, PEER_N_EXPERTS, D_MODEL), (PEER_HEADS * PEER_TOPK) ** -0.5),
        'final_norm_g': gain(ks[21], (D_MODEL,)),
    }


def reference(x, mem, mix_norm_g, w_in, conv_w, conv_b, igate_b, fgate_b, mlstm_norm_g, sb_norm_g,
              w_out, xattn_norm_g, mem_norm_g, xattn_wq, xattn_wkv, xattn_wo, ffn_norm_g, peer_wq,
              peer_subkeys, peer_u, peer_v, final_norm_g):
    for l in range(DEPTH):
        x = x + parallel_mixer(rms_norm(x, mix_norm_g[l]), w_in[l], conv_w[l], conv_b[l], igate_b[l],
                               fgate_b[l], mlstm_norm_g[l], sb_norm_g[l], w_out[l])
        x = x + memory_cross_attention(rms_norm(x, xattn_norm_g[l]), rms_norm(mem, mem_norm_g[l]),
                                       xattn_wq[l], xattn_wkv[l], xattn_wo[l])
        x = x + peer_ffn(rms_norm(x, ffn_norm_g[l]), peer_wq[l], peer_subkeys[l], peer_u[l], peer_v[l])
    return rms_norm(x, final_norm_g)
```

```python
import contextlib
import os
import numpy as np
import concourse.bass as bass
import concourse.mybir as mybir
from concourse.bass_utils import run_bass_kernel_spmd

F32 = mybir.dt.float32
BF16 = mybir.dt.bfloat16
U32 = mybir.dt.uint32
I32 = mybir.dt.int32
ALU = mybir.AluOpType
AF = mybir.ActivationFunctionType
AX = mybir.AxisListType

P = 128
D = 1024
DC = D // P
NH_M, HD_M = 4, 128
NH_S, HD_S = 8, 64
PROJ = 3592
C_QKM, C_VM, C_OM, C_I, C_F, C_QS, C_KS, C_VS = 0, 1024, 1536, 2048, 2052, 2056, 2568, 3080
MEM = 256
NXH, XHD = 4, 256
PH, PK, PTOP = 8, 128, 16
NEXP = PK * PK
EPS = 1e-6
NEG = -1.0e30


class Res:
    def __init__(self, name, dram=False):
        self.name = name
        self.w = []
        self.r = []
        self.dsem = None
        self.dcnt = 0
        self.dram = dram


class Buf:
    def __init__(self, ap, res):
        self.ap = ap
        self.res = res


class KB:
    ENG = ('sp', 'pe', 'dve', 'act', 'pool')

    def __init__(self, nc):
        self.nc = nc
        self.sem = {e: nc.alloc_semaphore(name='s_' + e) for e in self.ENG}
        self.cnt = {e: 0 for e in self.ENG}
        self.waited = {e: {} for e in self.ENG}
        self.prog = {e: [] for e in self.ENG}
        self.nsem = len(self.ENG)
        self.ninst = 0
        self.owners = []
        self.marks = []

    def _wait(self, e, deps):
        for d in deps:
            if d is None:
                continue
            s, v = d
            key = id(s)
            if self.waited[e].get(key, 0) >= v:
                continue
            self.waited[e][key] = v
            self.prog[e].append(lambda eng, s=s, v=v: eng.wait_ge(s, v))

    @staticmethod
    def _deps(reads, writes, dma_write=False):
        deps = []
        for r in reads:
            deps.extend(r.w)
        for w in writes:
            if not (dma_write and w.dram):
                deps.extend(w.w)
            deps.extend(w.r)
        return deps

    def op(self, e, meth, R=(), W=(), **kw):
        self._wait(e, self._deps(R, W))
        self.cnt[e] += 1
        self.ninst += 1
        sem = self.sem[e]
        ev = (sem, self.cnt[e])
        self.prog[e].append(lambda eng, meth=meth, kw=kw, sem=sem: getattr(eng, meth)(**kw).then_inc(sem, 1))
        for r in R:
            r.r.append(ev)
        for w in W:
            w.w = [ev]
            w.r = []
        return ev

    def dma(self, q, out, in_, reads=(), writes=(), indirect=None, **kw):
        self._wait(q, self._deps(reads, writes, dma_write=True))
        (dst,) = writes
        owner = dst if not dst.dram else [r for r in reads if not r.dram][0]
        if owner.dsem is None:
            owner.dsem = self.nc.alloc_semaphore(name='d_' + owner.name)
            self.nsem += 1
            self.owners.append(owner)
        owner.dcnt += 16
        self.ninst += 1
        ev = (owner.dsem, owner.dcnt)
        ds = owner.dsem
        if indirect is None:
            self.prog[q].append(lambda eng, out=out, in_=in_, kw=kw, ds=ds:
                                eng.dma_start(out=out, in_=in_, **kw).then_inc(ds, 16))
        else:
            self.prog[q].append(lambda eng, out=out, in_=in_, io=indirect, kw=kw, ds=ds:
                                eng.indirect_dma_start(out=out, out_offset=None, in_=in_, in_offset=io,
                                                       **kw).then_inc(ds, 16))
        for r in reads:
            r.r.append(ev)
        if dst.dram:
            dst.w = [x for x in dst.w if x[0] is not ds] + [ev]
        else:
            dst.w = [ev]
        dst.r = []
        return ev

    def barrier(self):
        evs = [(self.sem[e], self.cnt[e]) for e in self.ENG if self.cnt[e] > 0]
        evs += [(o.dsem, o.dcnt) for o in self.owners]
        for e in self.ENG:
            self._wait(e, evs)

    def emit(self, final_events):
        self._wait('sp', final_events)
        prog = self.prog
        with self.nc.Block() as block:
            @block.sync
            def _(eng):
                for f in prog['sp']:
                    f(eng)

            @block.tensor
            def _(eng):
                for f in prog['pe']:
                    f(eng)

            @block.vector
            def _(eng):
                for f in prog['dve']:
                    f(eng)

            @block.scalar
            def _(eng):
                for f in prog['act']:
                    f(eng)

            @block.gpsimd
            def _(eng):
                for f in prog['pool']:
                    f(eng)


class Ctx:
    def __init__(self, nc, k):
        self.nc = nc
        self.k = k
        self.n = 0
        self.stack = [contextlib.ExitStack()]

    def push(self):
        self.stack.append(contextlib.ExitStack())

    def pop(self):
        self.k.marks.append(dict(self.k.cnt))
        self.k.barrier()
        self.stack.pop().close()

    def sb(self, name, shape, dt):
        self.n += 1
        t = self.stack[-1].enter_context(self.nc.sbuf_tensor(f"{name}_{self.n}", list(shape), dt))
        return Buf(t[:] if not hasattr(t, 'ap') else t.ap(), Res(f"{name}_{self.n}"))

    def dram(self, name, shape, dt, kind="Internal"):
        t = self.nc.dram_tensor(name, list(shape), dt, kind=kind)
        return Buf(t.ap(), Res(name, dram=True))


def bc_last(ap, n):
    return ap.unsqueeze(ap.ndim).to_broadcast(list(ap.shape) + [n])


def bc_mid(ap, axis, n):
    shp = list(ap.shape)
    shp.insert(axis, n)
    return ap.unsqueeze(axis).to_broadcast(shp)


def build(S, phases=('A', 'B', 'C', 'D'), debug=()):
    NT = S // P
    NG = S // 512
    assert S % 512 == 0
    nc = bass.Bass("TRN2", target_bir_lowering=False)
    k = KB(nc)
    cx = Ctx(nc, k)

    def dkind(name):
        return "ExternalOutput" if name in debug else "Internal"

    def din(name, shape, dt=F32):
        return Buf(nc.dram_tensor(name, list(shape), dt, kind="ExternalInput").ap(), Res(name, dram=True))

    x_d = din("x", [S, D])
    mem_d = din("mem", [MEM, D])
    w_in_d = din("w_in", [D, PROJ])
    w_out_d = din("w_out", [D, D])
    wq_d = din("xattn_wq", [D, D])
    wkv_d = din("xattn_wkv", [D, 2 * D])
    wo_d = din("xattn_wo", [D, D])
    pwq_d = din("peer_wq", [D, 2 * D])
    skT_d = din("subkeysT", [P, 16, P])
    pu_d = din("peer_u", [NEXP, D])
    pv_d = din("peer_v", [NEXP, D])
    gvec_d = din("gvec", [P, 5, DC])
    grow_d = din("grow", [2, D])
    convw_d = din("conv_wT", [P, DC, 4])
    convb_d = din("conv_bT", [P, DC])
    gateb_d = din("gate_b", [4, 2])
    out_d = Buf(nc.dram_tensor("out", [S, D], F32, kind="ExternalOutput").ap(), Res("out", dram=True))

    qkm_s = cx.dram("qkm_s", [DC, P, S], BF16, kind=dkind("qkm_s"))
    sbq_s = cx.dram("sbq_s", [NH_S * HD_S, S], BF16, kind=dkind("sbq_s"))
    sbk_s = cx.dram("sbk_s", [NH_S * HD_S, S], BF16, kind=dkind("sbk_s"))
    vm_s = cx.dram("vm_s", [S, NH_M * HD_M], BF16, kind=dkind("vm_s"))
    og_s = cx.dram("og_s", [S, NH_M * HD_M], BF16, kind=dkind("og_s"))
    vs_s = cx.dram("vs_s", [S, NH_S * HD_S], BF16, kind=dkind("vs_s"))
    gates_s = cx.dram("gates_s", [4, 2, S], F32, kind=dkind("gates_s"))
    mixed_s = cx.dram("mixed_s", [S, D], BF16, kind=dkind("mixed_s"))
    x2_s = cx.dram("x2_s", [S, D], F32, kind=dkind("x2_s"))
    uv_s = cx.dram("uv_s", [NEXP, 2 * D], BF16)

    banks = []
    for i in range(8):
        t = nc.alloc_psum_tensor(f"bank{i}", [P, 512], F32)
        banks.append(Buf(t.ap(), Res(f"bank{i}")))

    ident_b = cx.sb("ident_b", [P, P], BF16)
    ident_f = cx.sb("ident_f", [P, P], F32)
    for idt in (ident_b, ident_f):
        k.op('pool', 'memset', W=[idt.res], ap=idt.ap, constant=1.0)
        k.op('pool', 'affine_select', R=[idt.res], W=[idt.res], out=idt.ap, in_=idt.ap, pattern=[[-1, P]],
             compare_op=ALU.is_equal, fill=0.0, base=0, channel_multiplier=1)
    gvec = cx.sb("gvec", [P, 5, DC], F32)
    k.dma('sp', gvec.ap, gvec_d.ap, writes=[gvec.res])
    convw = cx.sb("convw", [P, DC, 4], F32)
    k.dma('sp', convw.ap, convw_d.ap, writes=[convw.res])
    convb = cx.sb("convb", [P, DC], F32)
    k.dma('sp', convb.ap, convb_d.ap, writes=[convb.res])
    gateb = cx.sb("gateb", [4, 2], F32)
    k.dma('sp', gateb.ap, gateb_d.ap, writes=[gateb.res])

    STG = 2048
    stage = [cx.sb(f"stage{i}", [P, STG], F32) for i in range(2)]
    stage_i = [0]

    def load_cast(dst_aps, src_aps, dst_res, cast_engs=('dve', 'pool')):
        for dst, src in zip(dst_aps, src_aps):
            st = stage[stage_i[0] % 2]
            n = int(np.prod(src.shape[1:]))
            sview = st.ap[:, 0:n]
            if len(src.shape) == 3:
                sview = sview.rearrange("p (a b) -> p a b", a=src.shape[1])
            k.dma('sp', sview, src, writes=[st.res])
            eng = cast_engs[stage_i[0] % len(cast_engs)]
            k.op(eng, 'tensor_copy', R=[st.res], W=[dst_res], out=dst, in_=sview)
            stage_i[0] += 1

    def make_gbc(idx, name):
        g = cx.sb(name, [P, DC, P], F32)
        k.op('dve', 'tensor_copy', R=[gvec.res], W=[g.res], out=g.ap, in_=bc_last(gvec.ap[:, idx, :], P))
        return g

    def rms_stats(ss, rstd, n_cols):
        k.op('act', 'activation', R=[ss.res], W=[rstd.res], out=rstd.ap[:, 0:n_cols], in_=ss.ap[:, 0:n_cols],
             func=AF.Ln, scale=1.0 / D, bias=EPS)
        k.op('act', 'activation', R=[rstd.res], W=[rstd.res], out=rstd.ap[:, 0:n_cols], in_=rstd.ap[:, 0:n_cols],
             func=AF.Exp, scale=-0.5)

    if 'D' in phases:
        cx.push()
        RT = 2
        tin_u = [cx.sb(f"tin_u{i}", [P, RT, D], F32) for i in range(2)]
        tin_v = [cx.sb(f"tin_v{i}", [P, RT, D], F32) for i in range(2)]
        tout = [cx.sb(f"tout{i}", [P, RT, 2 * D], BF16) for i in range(2)]
        nchunk = NEXP // (P * RT)
        for c in range(nchunk):
            rs_ = slice(c * P * RT, (c + 1) * P * RT)
            tu, tv, to = tin_u[c % 2], tin_v[c % 2], tout[c % 2]
            k.dma('sp', tu.ap, pu_d.ap[rs_, :].rearrange("(p r) d -> p r d", r=RT), writes=[tu.res])
            k.dma('sp', tv.ap, pv_d.ap[rs_, :].rearrange("(p r) d -> p r d", r=RT), writes=[tv.res])
            k.op('dve', 'tensor_copy', R=[tu.res], W=[to.res], out=to.ap[:, :, 0:D], in_=tu.ap)
            k.op('act', 'copy', R=[tv.res], W=[to.res], out=to.ap[:, :, D:2 * D], in_=tv.ap)
            k.dma('pool', uv_s.ap[rs_, :].rearrange("(p r) d -> p r d", r=RT), to.ap, reads=[to.res], writes=[uv_s.res])
        cx.pop()

    if 'A' in phases:
        cx.push()
        gst = [cx.sb(f"gst{i}", [4, 2, 512], F32) for i in range(2)]
        w_in = cx.sb("w_in", [P, DC, PROJ], BF16)
        load_cast([w_in.ap[:, c, a:min(a + STG, PROJ)] for c in range(DC) for a in range(0, PROJ, STG)],
                  [w_in_d.ap[c * P:(c + 1) * P, a:min(a + STG, PROJ)] for c in range(DC) for a in range(0, PROJ, STG)], w_in.res)
        g_mix = make_gbc(0, "g_mix")
        xg = [cx.sb(f"xg{i}", [P, 4, D], F32) for i in range(2)]
        xn = cx.sb("xn", [P, 4, D], BF16)
        junk = cx.sb("junk", [P, D], F32)
        ssq = [cx.sb(f"ssq{i}", [P, 4], F32) for i in range(2)]
        rstd = [cx.sb(f"rstd{i}", [P, 4], F32) for i in range(2)]
        hT = [cx.sb(f"hT{i}", [P, DC, 512], BF16) for i in range(2)]
        pre = cx.sb("pre", [P, DC, 3 + 512], F32)
        cacc = [cx.sb(f"cacc{i}", [P, 512], F32) for i in range(2)]
        qkm_o = [cx.sb(f"qkm_o{i}", [P, 512], BF16) for i in range(3)]
        sb_o = [cx.sb(f"sb_o{i}", [P, 512], BF16) for i in range(3)]
        tm_o = [cx.sb(f"tm_o{i}", [P, 512], BF16) for i in range(4)]
        k.op('pool', 'memset', W=[pre.res], ap=pre.ap[:, :, 0:3], constant=0.0)
        rot = {'mm': 0, 'q': 0, 's': 0, 't': 0}
        MMB = [2, 3, 4, 5, 6, 7]

        def mm_bank():
            b = banks[MMB[rot['mm'] % len(MMB)]]
            rot['mm'] += 1
            return b

        def load_x(g):
            xb = xg[g % 2]
            k.dma('sp', xb.ap, x_d.ap[g * 512:(g + 1) * 512, :].rearrange("(a p) d -> p a d", p=P), writes=[xb.res])

        load_x(0)
        for g in range(NG):
            if g + 1 < NG:
                load_x(g + 1)
            xb, ss, rs, hTg = xg[g % 2], ssq[g % 2], rstd[g % 2], hT[g % 2]
            gs = slice(g * 512, (g + 1) * 512)
            for tl in range(4):
                k.op('act', 'activation', R=[xb.res], W=[junk.res, ss.res], out=junk.ap, in_=xb.ap[:, tl, :],
                     func=AF.Square, accum_out=ss.ap[:, tl:tl + 1])
            rms_stats(ss, rs, 4)
            for tl in range(4):
                k.op('dve', 'tensor_scalar', R=[xb.res, rs.res], W=[xn.res], out=xn.ap[:, tl, :], in0=xb.ap[:, tl, :],
                     scalar1=rs.ap[:, tl:tl + 1], scalar2=None, op0=ALU.mult)
            for tl in range(4):
                pb = banks[tl % 2]
                pview = pb.ap.bitcast(BF16).rearrange("p (c t) -> p c t", c=DC)
                for c in range(DC):
                    k.op('pe', 'transpose', R=[xn.res, ident_b.res], W=[pb.res], out=pview[:, c, :],
                         in_=xn.ap[:, tl, c * P:(c + 1) * P], identity=ident_b.ap)
                k.op('dve', 'tensor_tensor', R=[pb.res, g_mix.res], W=[hTg.res], out=hTg.ap[:, :, tl * P:(tl + 1) * P],
                     in0=pview, in1=g_mix.ap, op=ALU.mult)

            def fm_matmul(col0, M):
                b = mm_bank()
                for c in range(DC):
                    k.op('pe', 'matmul', R=[w_in.res, hTg.res], W=[b.res], out=b.ap[0:M, :], lhsT=w_in.ap[:, c, col0:col0 + M],
                         rhs=hTg.ap[:, c, :], start=(c == 0), stop=(c == DC - 1))
                return b

            for c in range(DC):
                b = fm_matmul(C_QKM + c * P, P)
                k.op('act', 'copy', R=[b.res], W=[pre.res], out=pre.ap[:, c, 3:515], in_=b.ap)
            for c in range(DC):
                ca = cacc[c % 2]
                k.op('dve', 'tensor_scalar', R=[pre.res, convw.res, convb.res], W=[ca.res], out=ca.ap, in0=pre.ap[:, c, 0:512],
                     scalar1=convw.ap[:, c, 0:1], scalar2=convb.ap[:, c:c + 1], op0=ALU.mult, op1=ALU.add)
                for j in range(1, 4):
                    k.op('dve', 'scalar_tensor_tensor', R=[pre.res, convw.res, ca.res], W=[ca.res], out=ca.ap,
                         in0=pre.ap[:, c, j:j + 512], scalar=convw.ap[:, c, j:j + 1], in1=ca.ap, op0=ALU.mult, op1=ALU.add)
                qo = qkm_o[rot['q'] % 3]
                rot['q'] += 1
                k.op('act', 'activation', R=[ca.res], W=[qo.res], out=qo.ap, in_=ca.ap, func=AF.Silu)
                k.dma('pool', qkm_s.ap[c, :, gs], qo.ap, reads=[qo.res], writes=[qkm_s.res])
            k.op('dve', 'tensor_copy', R=[pre.res], W=[pre.res], out=pre.ap[:, :, 0:3], in_=pre.ap[:, :, 512:515])
            for col0, dst in ((C_QS, sbq_s), (C_KS, sbk_s)):
                for c in range(4):
                    b = fm_matmul(col0 + c * P, P)
                    so = sb_o[rot['s'] % 3]
                    rot['s'] += 1
                    k.op('act', 'copy', R=[b.res], W=[so.res], out=so.ap, in_=b.ap)
                    k.dma('pool', dst.ap[c * P:(c + 1) * P, gs], so.ap, reads=[so.res], writes=[dst.res])
            gsb = gst[g % 2]
            for gi, col0 in ((0, C_I), (1, C_F)):
                b = fm_matmul(col0, 4)
                k.op('act', 'activation', R=[b.res, gateb.res], W=[gsb.res], out=gsb.ap[:, gi, :], in_=b.ap[0:4, :],
                     func=AF.Identity, bias=gateb.ap[:, gi:gi + 1])
            k.dma('pool', gates_s.ap[:, :, gs], gsb.ap, reads=[gsb.res], writes=[gates_s.res])
            for tl in range(4):
                tt = g * 4 + tl
                for col0, dst, fn in ((C_VM, vm_s, None), (C_OM, og_s, AF.Sigmoid), (C_VS, vs_s, None)):
                    b = mm_bank()
                    for c in range(DC):
                        k.op('pe', 'matmul', R=[w_in.res, hTg.res], W=[b.res], out=b.ap, lhsT=hTg.ap[:, c, tl * P:(tl + 1) * P],
                             rhs=w_in.ap[:, c, col0:col0 + 512], start=(c == 0), stop=(c == DC - 1))
                    to = tm_o[rot['t'] % 4]
                    rot['t'] += 1
                    if fn is None:
                        k.op('dve', 'tensor_copy', R=[b.res], W=[to.res], out=to.ap, in_=b.ap)
                    else:
                        k.op('act', 'activation', R=[b.res], W=[to.res], out=to.ap, in_=b.ap, func=fn)
                    k.dma('pool', dst.ap[tt * P:(tt + 1) * P, :], to.ap, reads=[to.res], writes=[dst.res])
        cx.pop()

    if 'B' in phases:
        cx.push()
        KSC = float(HD_M) ** -0.5
        g4 = cx.sb("g4", [4, 2, S], F32)
        k.dma('sp', g4.ap, gates_s.ap, reads=[gates_s.res], writes=[g4.res])
        t2 = cx.sb("t2", [4, S], F32)
        k.op('act', 'activation', R=[g4.res], W=[t2.res], out=t2.ap, in_=g4.ap[:, 1, :], func=AF.Exp, scale=-1.0)
        k.op('act', 'activation', R=[t2.res], W=[t2.res], out=t2.ap, in_=t2.ap, func=AF.Ln, bias=1.0)
        ones4 = cx.sb("ones4", [4, S], F32)
        k.op('pool', 'memset', W=[ones4.res], ap=ones4.ap, constant=1.0)
        csum = cx.sb("csum", [4, S], F32)
        k.op('dve', 'tensor_tensor_scan', R=[ones4.res, t2.res], W=[csum.res], out=csum.ap, data0=ones4.ap, data1=t2.ap,
             initial=0.0, op0=ALU.mult, op1=ALU.add)
        mi = cx.sb("mi", [4, 1], F32)
        k.op('dve', 'tensor_reduce', R=[g4.res], W=[mi.res], out=mi.ap, in_=g4.ap[:, 0, :], axis=AX.X, op=ALU.max)
        crow = cx.sb("crow", [4, S], F32)
        k.op('dve', 'scalar_tensor_tensor', R=[g4.res, mi.res, csum.res], W=[crow.res], out=crow.ap, in0=g4.ap[:, 0, :],
             scalar=mi.ap[:, 0:1], in1=csum.ap, op0=ALU.subtract, op1=ALU.add)
        brow = cx.sb("brow", [4, S], F32)
        k.op('dve', 'tensor_scalar', R=[csum.res], W=[brow.res], out=brow.ap, in0=csum.ap, scalar1=-1.0, scalar2=None, op0=ALU.mult)
        em = cx.sb("em", [4, 1], F32)
        k.op('act', 'activation', R=[mi.res], W=[em.res], out=em.ap, in_=mi.ap, func=AF.Exp, scale=-1.0)
        sel = cx.sb("sel", [4, 4, P], F32)
        k.op('dve', 'tensor_copy', R=[ident_f.res], W=[sel.res], out=sel.ap, in_=bc_last(ident_f.ap[0:4, 0:4], P))
        dgem = cx.sb("dgem", [4, 4], F32)
        k.op('dve', 'tensor_scalar', R=[ident_f.res, em.res], W=[dgem.res], out=dgem.ap, in0=ident_f.ap[0:4, 0:4],
             scalar1=em.ap[:, 0:1], scalar2=None, op0=ALU.mult)
        ones4p = cx.sb("ones4p", [4, P], F32)
        k.op('pool', 'memset', W=[ones4p.res], ap=ones4p.ap, constant=1.0)
        ccol = cx.sb("ccol", [P, NT, 4], F32)
        embc = cx.sb("embc", [P, 4], F32)
        b2 = banks[2]
        for blk in range(NT):
            k.op('pe', 'matmul', R=[crow.res, ident_f.res], W=[b2.res], out=b2.ap[:, blk * 4:(blk + 1) * 4],
                 lhsT=crow.ap[:, blk * P:(blk + 1) * P], rhs=ident_f.ap[0:4, 0:4], start=True, stop=True)
        k.op('dve', 'tensor_copy', R=[b2.res], W=[ccol.res], out=ccol.ap, in_=b2.ap[:, 0:NT * 4].rearrange("p (n h) -> p n h", h=4))
        b3 = banks[3]
        k.op('pe', 'matmul', R=[ones4p.res, dgem.res], W=[b3.res], out=b3.ap[:, 0:4], lhsT=ones4p.ap, rhs=dgem.ap, start=True, stop=True)
        k.op('dve', 'tensor_copy', R=[b3.res], W=[embc.res], out=embc.ap, in_=b3.ap[:, 0:4])

        qTb = cx.sb("qTb", [P, S], BF16)
        kTb = cx.sb("kTb", [P, S], BF16)
        v1 = cx.sb("v1", [P, NT, HD_M + 1], BF16)
        k.op('pool', 'memset', W=[v1.res], ap=v1.ap, constant=1.0)
        Bbc = cx.sb("Bbc", [P, S], F32)
        Dt = [cx.sb(f"Dt{i}", [P, 512], F32) for i in range(2)]
        At = [cx.sb(f"At{i}", [P, 512], BF16) for i in range(2)]
        ogt = [cx.sb(f"ogt{i}", [P, 4, HD_M], BF16) for i in range(2)]
        hm = [cx.sb(f"hm{i}", [P, 4, HD_M], F32) for i in range(2)]
        hsq = cx.sb("hsq", [P, HD_M], F32)
        dn = [cx.sb(f"dn{i}", [P, 4], F32) for i in range(2)]
        hss = [cx.sb(f"hss{i}", [P, 4], F32) for i in range(2)]
        hrs = [cx.sb(f"hrs{i}", [P, 4], F32) for i in range(2)]
        mo = [cx.sb(f"mo{i}", [P, 4, HD_M], BF16) for i in range(2)]
        it = 0
        ch = 0
        for h in range(int(os.environ.get('NHB', NH_M))):
            k.dma('sp', qTb.ap, qkm_s.ap[h], reads=[qkm_s.res], writes=[qTb.res])
            k.dma('sp', kTb.ap, qkm_s.ap[4 + h], reads=[qkm_s.res], writes=[kTb.res])
            with nc.allow_non_contiguous_dma(reason="per-head v slices, 256B runs"):
                k.dma('sp', v1.ap[:, :, 0:HD_M], vm_s.ap[:, h * HD_M:(h + 1) * HD_M].rearrange("(n p) d -> p n d", p=P),
                      reads=[vm_s.res], writes=[v1.res])
            for c5 in range(S // 512):
                bb = banks[2 + c5 % 2]
                k.op('pe', 'matmul', R=[sel.res, brow.res], W=[bb.res], out=bb.ap, lhsT=sel.ap[:, h, :],
                     rhs=brow.ap[:, c5 * 512:(c5 + 1) * 512], start=True, stop=True)
                k.op('act', 'copy', R=[bb.res], W=[Bbc.res], out=Bbc.ap[:, c5 * 512:(c5 + 1) * 512], in_=bb.ap)
            for q in range(NG):
                og = ogt[ch % 2]
                with nc.allow_non_contiguous_dma(reason="per-head o-gate slices"):
                    k.dma('sp', og.ap, og_s.ap[q * 512:(q + 1) * 512, h * HD_M:(h + 1) * HD_M].rearrange("(a p) d -> p a d", p=P),
                          reads=[og_s.res], writes=[og.res])
                accs = [banks[4 + i] for i in range(4)]
                steps = []
                for j in range(4 * q + 4):
                    t0 = max(P * j, 512 * q)
                    steps.append(dict(j=j, t0=t0, n=512 * (q + 1) - t0, zb=banks[it % 2], dt_=Dt[it % 2], at_=At[it % 2]))
                    it += 1

                def bstage1(st):
                    j, t0, n, zb, dt_, at_ = st['j'], st['t0'], st['n'], st['zb'], st['dt_'], st['at_']
                    k.op('pe', 'matmul', R=[kTb.res, qTb.res], W=[zb.res], out=zb.ap[:, 0:n], lhsT=kTb.ap[:, j * P:(j + 1) * P],
                         rhs=qTb.ap[:, t0:t0 + n], start=True, stop=True)
                    k.op('act', 'activation', R=[Bbc.res, ccol.res], W=[dt_.res], out=dt_.ap[:, 0:n], in_=Bbc.ap[:, t0:t0 + n],
                         func=AF.Exp, bias=ccol.ap[:, j, h:h + 1])
                    if j >= 4 * q:
                        k.op('pool', 'affine_select', R=[dt_.res], W=[dt_.res], out=dt_.ap[:, 0:P], in_=dt_.ap[:, 0:P],
                             pattern=[[1, P]], compare_op=ALU.is_ge, fill=0.0, base=0, channel_multiplier=-1)
                    k.op('dve', 'scalar_tensor_tensor', R=[zb.res, dt_.res], W=[at_.res], out=at_.ap[:, 0:n], in0=zb.ap[:, 0:n],
                         scalar=KSC, in1=dt_.ap[:, 0:n], op0=ALU.mult, op1=ALU.mult)

                def bstage2(st):
                    j, t0, at_ = st['j'], st['t0'], st['at_']
                    for i in range(max(j, 4 * q), 4 * q + 4):
                        o = i * P - t0
                        acc = accs[i - 4 * q]
                        k.op('pe', 'matmul', R=[at_.res, v1.res], W=[acc.res], out=acc.ap[:, 0:HD_M + 1], lhsT=at_.ap[:, o:o + P],
                             rhs=v1.ap[:, j, :], start=(j == 0), stop=(j == i))

                ns = len(steps)
                for x_ in range(ns + 1):
                    if x_ < ns:
                        bstage1(steps[x_])
                    if 0 <= x_ - 1 < ns:
                        bstage2(steps[x_ - 1])
                d_, ss_, rs_, hm_, mo_ = dn[ch % 2], hss[ch % 2], hrs[ch % 2], hm[ch % 2], mo[ch % 2]
                for i in range(4):
                    acc = accs[i]
                    k.op('act', 'activation', R=[acc.res], W=[d_.res], out=d_.ap[:, i:i + 1], in_=acc.ap[:, HD_M:HD_M + 1], func=AF.Abs)
                k.op('dve', 'tensor_scalar', R=[d_.res, embc.res], W=[d_.res], out=d_.ap, in0=d_.ap, scalar1=embc.ap[:, h:h + 1],
                     scalar2=None, op0=ALU.max)
                k.op('dve', 'reciprocal', R=[d_.res], W=[d_.res], out=d_.ap, in_=d_.ap)
                for i in range(4):
                    acc = accs[i]
                    k.op('dve', 'scalar_tensor_tensor', R=[acc.res, d_.res, og.res], W=[hm_.res], out=hm_.ap[:, i, :], in0=acc.ap[:, 0:HD_M],
                         scalar=d_.ap[:, i:i + 1], in1=og.ap[:, i, :], op0=ALU.mult, op1=ALU.mult)
                    k.op('dve', 'scalar_tensor_tensor', R=[hm_.res], W=[hsq.res, ss_.res], out=hsq.ap, in0=hm_.ap[:, i, :], scalar=1.0, in1=hm_.ap[:, i, :],
                         op0=ALU.mult, op1=ALU.mult, accum_out=ss_.ap[:, i:i + 1])
                k.op('act', 'activation', R=[ss_.res], W=[rs_.res], out=rs_.ap, in_=ss_.ap, func=AF.Ln, scale=1.0 / HD_M, bias=EPS)
                k.op('act', 'activation', R=[rs_.res], W=[rs_.res], out=rs_.ap, in_=rs_.ap, func=AF.Exp, scale=-0.5)
                k.op('dve', 'tensor_tensor', R=[hm_.res, rs_.res], W=[mo_.res], out=mo_.ap, in0=hm_.ap, in1=bc_last(rs_.ap, HD_M), op=ALU.mult)
                with nc.allow_non_contiguous_dma(reason="per-head mixed slices"):
                    k.dma('pool', mixed_s.ap[q * 512:(q + 1) * 512, h * HD_M:(h + 1) * HD_M].rearrange("(a p) d -> p a d", p=P), mo_.ap,
                          reads=[mo_.res], writes=[mixed_s.res])
                ch += 1
        cx.pop()

    if 'C' in phases:
        cx.push()
        SSC = float(HD_S) ** -0.5
        tri = cx.sb("tri", [P, P], F32)
        k.op('pool', 'memset', W=[tri.res], ap=tri.ap, constant=1.0)
        k.op('pool', 'affine_select', R=[tri.res], W=[tri.res], out=tri.ap, in_=tri.ap, pattern=[[-1, P]],
             compare_op=ALU.is_ge, fill=0.0, base=0, channel_multiplier=1)
        onesf = cx.sb("onesf", [P, P], F32)
        k.op('pool', 'memset', W=[onesf.res], ap=onesf.ap, constant=1.0)
        sq_ = cx.sb("sq_", [HD_S, S], BF16)
        sk_ = cx.sb("sk_", [HD_S, S], BF16)
        skn = cx.sb("skn", [HD_S, S], BF16)
        sv_ = cx.sb("sv_", [P, NT, HD_S], BF16)
        e1 = [cx.sb(f"e1{i}", [P, 512], F32) for i in range(2)]
        sA = [cx.sb(f"sA{i}", [P, 512], BF16) for i in range(2)]
        racc = cx.sb("racc", [P, 512], F32)
        os_ = [cx.sb(f"os{i}", [P, 4, HD_S], F32) for i in range(2)]
        osq = cx.sb("osq", [P, HD_S], F32)
        sss = [cx.sb(f"sss{i}", [P, 4], F32) for i in range(2)]
        srs = [cx.sb(f"srs{i}", [P, 4], F32) for i in range(2)]
        smo = [cx.sb(f"smo{i}", [P, 4, HD_S], BF16) for i in range(2)]
        it = 0
        ch = 0
        for h in range(NH_S):
            k.dma('sp', sq_.ap, sbq_s.ap[h * HD_S:(h + 1) * HD_S, :], reads=[sbq_s.res], writes=[sq_.res])
            k.dma('sp', sk_.ap, sbk_s.ap[h * HD_S:(h + 1) * HD_S, :], reads=[sbk_s.res], writes=[sk_.res])
            with nc.allow_non_contiguous_dma(reason="per-head v slices, 128B runs"):
                k.dma('sp', sv_.ap, vs_s.ap[:, h * HD_S:(h + 1) * HD_S].rearrange("(n p) d -> p n d", p=P),
                      reads=[vs_s.res], writes=[sv_.res])
            k.op('dve', 'tensor_scalar', R=[sk_.res], W=[skn.res], out=skn.ap, in0=sk_.ap, scalar1=-SSC, scalar2=None, op0=ALU.mult)
            for q in range(NG):
                accs = [banks[4 + i] for i in range(4)]
                k.op('pool', 'memset', W=[racc.res], ap=racc.ap, constant=0.0)
                steps = []
                for j in range(4 * q + 3, -1, -1):
                    c0 = max(P * j - 512 * q, 0)
                    steps.append(dict(j=j, c0=c0, t0=512 * q + c0, n=512 - c0, diag=(j >= 4 * q), first=(j == 4 * q + 3),
                                      zb=banks[it % 2], cp=banks[2 + it % 2], e_=e1[it % 2], a_=sA[it % 2]))
                    it += 1

                def stage1(st):
                    j, t0, n, zb, e_ = st['j'], st['t0'], st['n'], st['zb'], st['e_']
                    k.op('pe', 'matmul', R=[sk_.res, sq_.res], W=[zb.res], out=zb.ap[:, 0:n], lhsT=sk_.ap[:, j * P:(j + 1) * P],
                         rhs=sq_.ap[:, t0:t0 + n], start=True, stop=True)
                    k.op('act', 'activation', R=[zb.res], W=[e_.res], out=e_.ap[:, 0:n], in_=zb.ap[:, 0:n], func=AF.Exp, scale=SSC)
                    k.op('act', 'activation', R=[e_.res], W=[e_.res], out=e_.ap[:, 0:n], in_=e_.ap[:, 0:n], func=AF.Ln, bias=1.0)
                    if st['diag']:
                        k.op('pool', 'affine_select', R=[e_.res], W=[e_.res], out=e_.ap[:, 0:P], in_=e_.ap[:, 0:P], pattern=[[1, P]],
                             compare_op=ALU.is_ge, fill=0.0, base=-1, channel_multiplier=-1)

                def stage2(st):
                    j, c0, t0, n, cp, e_, a_ = st['j'], st['c0'], st['t0'], st['n'], st['cp'], st['e_'], st['a_']
                    k.op('pe', 'matmul', R=[tri.res, e_.res], W=[cp.res], out=cp.ap[:, 0:n], lhsT=tri.ap, rhs=e_.ap[:, 0:n],
                         start=True, stop=False)
                    if not st['first']:
                        k.op('pe', 'matmul', R=[onesf.res, racc.res], W=[cp.res], out=cp.ap[:, 0:n], lhsT=onesf.ap, rhs=racc.ap[:, c0:512],
                             start=False, stop=False)
                    k.op('pe', 'matmul', R=[skn.res, sq_.res], W=[cp.res], out=cp.ap[:, 0:n], lhsT=skn.ap[:, j * P:(j + 1) * P],
                         rhs=sq_.ap[:, t0:t0 + n], start=False, stop=True)
                    if j > 0:
                        k.op('pool', 'tensor_tensor', R=[racc.res, e_.res], W=[racc.res], out=racc.ap[:, c0:512], in0=racc.ap[:, c0:512],
                             in1=e_.ap[:, 0:n], op=ALU.add)
                    k.op('act', 'activation', R=[cp.res], W=[a_.res], out=a_.ap[:, 0:n], in_=cp.ap[:, 0:n], func=AF.Exp, scale=-1.0)
                    if st['diag']:
                        k.op('pool', 'affine_select', R=[a_.res], W=[a_.res], out=a_.ap[:, 0:P], in_=a_.ap[:, 0:P], pattern=[[1, P]],
                             compare_op=ALU.is_ge, fill=0.0, base=-1, channel_multiplier=-1)

                def stage3(st):
                    j, t0, a_ = st['j'], st['t0'], st['a_']
                    for i in range(max(j, 4 * q), 4 * q + 4):
                        o = i * P - t0
                        acc = accs[i - 4 * q]
                        k.op('pe', 'matmul', R=[a_.res, sv_.res], W=[acc.res], out=acc.ap[:, 0:HD_S], lhsT=a_.ap[:, o:o + P],
                             rhs=sv_.ap[:, j, :], start=(j == i), stop=(j == 0))

                ns = len(steps)
                for x_ in range(ns + 2):
                    if x_ < ns:
                        stage1(steps[x_])
                    if 0 <= x_ - 1 < ns:
                        stage2(steps[x_ - 1])
                    if 0 <= x_ - 2 < ns:
                        stage3(steps[x_ - 2])
                o_, ss_, rs_, mo_ = os_[ch % 2], sss[ch % 2], srs[ch % 2], smo[ch % 2]
                for i in range(4):
                    k.op('act', 'copy', R=[accs[i].res], W=[o_.res], out=o_.ap[:, i, :], in_=accs[i].ap[:, 0:HD_S])
                for i in range(4):
                    k.op('dve', 'scalar_tensor_tensor', R=[o_.res], W=[osq.res, ss_.res], out=osq.ap, in0=o_.ap[:, i, :], scalar=1.0, in1=o_.ap[:, i, :],
                         op0=ALU.mult, op1=ALU.mult, accum_out=ss_.ap[:, i:i + 1])
                k.op('act', 'activation', R=[ss_.res], W=[rs_.res], out=rs_.ap, in_=ss_.ap, func=AF.Ln, scale=1.0 / HD_S, bias=EPS)
                k.op('act', 'activation', R=[rs_.res], W=[rs_.res], out=rs_.ap, in_=rs_.ap, func=AF.Exp, scale=-0.5)
                k.op('dve', 'tensor_tensor', R=[o_.res, rs_.res], W=[mo_.res], out=mo_.ap, in0=o_.ap, in1=bc_last(rs_.ap, HD_S), op=ALU.mult)
                with nc.allow_non_contiguous_dma(reason="per-head mixed slices"):
                    k.dma('pool', mixed_s.ap[q * 512:(q + 1) * 512, 512 + h * HD_S:512 + (h + 1) * HD_S].rearrange("(a p) d -> p a d", p=P),
                          mo_.ap, reads=[mo_.res], writes=[mixed_s.res])
                ch += 1
        cx.pop()

    if 'D' in phases:
        cx.push()
        XSC = float(XHD) ** -0.5
        w_out = cx.sb("w_out", [P, DC, D], BF16)
        wq = cx.sb("wq", [P, DC, D], BF16)
        wo = cx.sb("wo", [P, DC, D], BF16)
        kmT = cx.sb("kmT", [P, DC, MEM], BF16)
        vmem = cx.sb("vmem", [P, 2, D], BF16)
        g_mixed = make_gbc(4, "g_mixed")
        g_xat = make_gbc(1, "g_xat")
        junk1 = cx.sb("junk1", [P, D], F32)

        def rows(wd, c, a, n):
            return wd.ap[c * P:(c + 1) * P, a:a + n]

        def norm_T(src, ss, rs, xn_b, gbc, dstT, ncol_off=0, tbank=None):
            k.op('act', 'activation', R=[src.res], W=[junk1.res, ss.res], out=junk1.ap, in_=src.ap, func=AF.Square, accum_out=ss.ap[:, 0:1])
            rms_stats(ss, rs, 1)
            k.op('dve', 'tensor_scalar', R=[src.res, rs.res], W=[xn_b.res], out=xn_b.ap, in0=src.ap, scalar1=rs.ap[:, 0:1], scalar2=None, op0=ALU.mult)
            transpose_T(xn_b, gbc, dstT, ncol_off, tbank)

        trot = [0]

        def transpose_T(src_b, gbc, dstT, ncol_off=0, tbank=None):
            pb = tbank if tbank is not None else banks[trot[0] % 2]
            trot[0] += 1
            pview = pb.ap.bitcast(BF16).rearrange("p (c t) -> p c t", c=DC)
            for c in range(DC):
                k.op('pe', 'transpose', R=[src_b.res, ident_b.res], W=[pb.res], out=pview[:, c, :], in_=src_b.ap[:, c * P:(c + 1) * P],
                     identity=ident_b.ap)
            k.op('dve', 'tensor_tensor', R=[pb.res, gbc.res], W=[dstT.res], out=dstT.ap[:, :, ncol_off:ncol_off + P], in0=pview, in1=gbc.ap,
                 op=ALU.mult)

        cx.push()
        wkv = cx.sb("wkv", [P, DC, 2 * D], BF16)
        load_cast([wkv.ap[:, c, :] for c in range(DC)], [rows(wkv_d, c, 0, 2 * D) for c in range(DC)], wkv.res)
        g_mem = make_gbc(2, "g_mem")
        memt = cx.sb("memt", [P, D], F32)
        memn = cx.sb("memn", [P, D], BF16)
        memT = cx.sb("memT", [P, DC, MEM], BF16)
        mss = cx.sb("mss", [P, 1], F32)
        mrs = cx.sb("mrs", [P, 1], F32)
        for mc in range(2):
            k.dma('sp', memt.ap, mem_d.ap[mc * P:(mc + 1) * P, :], writes=[memt.res])
            norm_T(memt, mss, mrs, memn, g_mem, memT, ncol_off=mc * P)
        for c in range(DC):
            b = banks[2 + c % 2]
            for dc in range(DC):
                k.op('pe', 'matmul', R=[wkv.res, memT.res], W=[b.res], out=b.ap[:, 0:MEM], lhsT=wkv.ap[:, dc, c * P:(c + 1) * P], rhs=memT.ap[:, dc, :],
                     start=(dc == 0), stop=(dc == DC - 1))
            k.op('act', 'copy', R=[b.res], W=[kmT.res], out=kmT.ap[:, c, :], in_=b.ap[:, 0:MEM])
        for mc in range(2):
            for hf in range(2):
                b = banks[4 + (mc * 2 + hf) % 2]
                for dc in range(DC):
                    k.op('pe', 'matmul', R=[wkv.res, memT.res], W=[b.res], out=b.ap, lhsT=memT.ap[:, dc, mc * P:(mc + 1) * P],
                         rhs=wkv.ap[:, dc, D + hf * 512:D + (hf + 1) * 512], start=(dc == 0), stop=(dc == DC - 1))
                k.op('dve', 'tensor_copy', R=[b.res], W=[vmem.res], out=vmem.ap[:, mc, hf * 512:(hf + 1) * 512], in_=b.ap)
        cx.pop()

        load_cast([w_out.ap[:, c, :] for c in range(DC)], [rows(w_out_d, c, 0, D) for c in range(DC)], w_out.res)
        load_cast([wq.ap[:, c, :] for c in range(DC)], [rows(wq_d, c, 0, D) for c in range(DC)], wq.res)
        load_cast([wo.ap[:, c, :] for c in range(DC)], [rows(wo_d, c, 0, D) for c in range(DC)], wo.res)

        NB = 2
        xt_ = [cx.sb(f"xt{i}", [P, D], F32) for i in range(NB)]
        mx_ = [cx.sb(f"mx{i}", [P, D], BF16) for i in range(NB)]
        mT_ = [cx.sb(f"mT{i}", [P, DC, P], BF16) for i in range(NB)]
        x1_ = [cx.sb(f"x1{i}", [P, D], F32) for i in range(NB)]
        x2_ = [cx.sb(f"x2{i}", [P, D], F32) for i in range(NB)]
        xnb = [cx.sb(f"xnb{i}", [P, D], BF16) for i in range(NB)]
        h2T = [cx.sb(f"h2T{i}", [P, DC, P], BF16) for i in range(NB)]
        qT_ = [cx.sb(f"qT{i}", [P, DC, P], BF16) for i in range(NB)]
        pex = [cx.sb(f"pex{i}", [P, NXH, MEM], F32) for i in range(NB)]
        pn_ = [cx.sb(f"pn{i}", [P, NXH, MEM], BF16) for i in range(NB)]
        pT_ = [cx.sb(f"pT{i}", [P, DC, P], BF16) for i in range(NB)]
        oT_ = [cx.sb(f"oT{i}", [P, DC, P], BF16) for i in range(NB)]
        st1 = [cx.sb(f"st1{i}", [P, 16], F32) for i in range(NB)]
        mmr = [0]
        PAIRS = [(2, 3), (4, 5), (6, 7)]

        def pair():
            p_ = PAIRS[mmr[0] % 3]
            mmr[0] += 1
            return banks[p_[0]], banks[p_[1]]

        for tt in range(NT):
            r = tt % NB
            xt, mxt, mT, x1, x2, xn_b, hT2, qT, pe_, pn, pT, oT, st = (xt_[r], mx_[r], mT_[r], x1_[r], x2_[r], xnb[r], h2T[r], qT_[r], pex[r],
                                                                      pn_[r], pT_[r], oT_[r], st1[r])
            ts = slice(tt * P, (tt + 1) * P)
            k.dma('sp', xt.ap, x_d.ap[ts, :], writes=[xt.res])
            k.dma('sp', mxt.ap, mixed_s.ap[ts, :], reads=[mixed_s.res], writes=[mxt.res])
            transpose_T(mxt, g_mixed, mT)
            ba, bb = pair()
            for hf, b in ((0, ba), (1, bb)):
                for dc in range(DC):
                    k.op('pe', 'matmul', R=[mT.res, w_out.res], W=[b.res], out=b.ap, lhsT=mT.ap[:, dc, :], rhs=w_out.ap[:, dc, hf * 512:(hf + 1) * 512],
                         start=(dc == 0), stop=(dc == DC - 1))
                k.op('dve', 'tensor_tensor', R=[b.res, xt.res], W=[x1.res], out=x1.ap[:, hf * 512:(hf + 1) * 512], in0=b.ap,
                     in1=xt.ap[:, hf * 512:(hf + 1) * 512], op=ALU.add)
            ssb = Buf(st.ap[:, 0:1], st.res)
            rsb = Buf(st.ap[:, 1:2], st.res)
            norm_T(x1, ssb, rsb, xn_b, g_xat, hT2)
            ba, bb = pair()
            for c in range(DC):
                b = ba if c < 4 else bb
                for dc in range(DC):
                    k.op('pe', 'matmul', R=[wq.res, hT2.res], W=[b.res], out=b.ap[:, (c % 4) * P:(c % 4 + 1) * P], lhsT=wq.ap[:, dc, c * P:(c + 1) * P],
                         rhs=hT2.ap[:, dc, :], start=(dc == 0), stop=(dc == DC - 1))
            for hf, b in ((0, ba), (1, bb)):
                k.op('act', 'copy', R=[b.res], W=[qT.res], out=qT.ap[:, hf * 4:(hf + 1) * 4, :], in_=b.ap.rearrange("p (c t) -> p c t", c=4))
            ba, bb = pair()
            for hh in range(NXH):
                b = ba if hh < 2 else bb
                for c2 in range(2):
                    k.op('pe', 'matmul', R=[qT.res, kmT.res], W=[b.res], out=b.ap[:, (hh % 2) * MEM:(hh % 2 + 1) * MEM], lhsT=qT.ap[:, 2 * hh + c2, :],
                         rhs=kmT.ap[:, 2 * hh + c2, :], start=(c2 == 0), stop=(c2 == 1))
            for hf, b in ((0, ba), (1, bb)):
                k.op('dve', 'tensor_reduce', R=[b.res], W=[st.res], out=st.ap[:, 4 + 2 * hf:6 + 2 * hf], in_=b.ap.rearrange("p (h m) -> p h m", h=2),
                     axis=AX.X, op=ALU.max)
            k.op('dve', 'tensor_scalar', R=[st.res], W=[st.res], out=st.ap[:, 4:8], in0=st.ap[:, 4:8], scalar1=-XSC, scalar2=None, op0=ALU.mult)
            for hh in range(NXH):
                b = ba if hh < 2 else bb
                k.op('act', 'activation', R=[b.res, st.res], W=[pe_.res, st.res], out=pe_.ap[:, hh, :], in_=b.ap[:, (hh % 2) * MEM:(hh % 2 + 1) * MEM],
                     func=AF.Exp, scale=XSC, bias=st.ap[:, 4 + hh:5 + hh], accum_out=st.ap[:, 8 + hh:9 + hh])
            k.op('dve', 'reciprocal', R=[st.res], W=[st.res], out=st.ap[:, 12:16], in_=st.ap[:, 8:12])
            k.op('dve', 'tensor_tensor', R=[pe_.res, st.res], W=[pn.res], out=pn.ap, in0=pe_.ap, in1=bc_last(st.ap[:, 12:16], MEM), op=ALU.mult)
            pb = banks[trot[0] % 2]
            trot[0] += 1
            pview = pb.ap.bitcast(BF16).rearrange("p (c t) -> p c t", c=DC)
            for hh in range(NXH):
                for mc in range(2):
                    k.op('pe', 'transpose', R=[pn.res, ident_b.res], W=[pb.res], out=pview[:, hh * 2 + mc, :], in_=pn.ap[:, hh, mc * P:(mc + 1) * P],
                         identity=ident_b.ap)
            k.op('act', 'copy', R=[pb.res], W=[pT.res], out=pT.ap, in_=pview)
            ba, bb = pair()
            for c in range(DC):
                b = ba if c < 4 else bb
                hh = c // 2
                for mc in range(2):
                    k.op('pe', 'matmul', R=[vmem.res, pT.res], W=[b.res], out=b.ap[:, (c % 4) * P:(c % 4 + 1) * P], lhsT=vmem.ap[:, mc, c * P:(c + 1) * P],
                         rhs=pT.ap[:, hh * 2 + mc, :], start=(mc == 0), stop=(mc == 1))
            for hf, b in ((0, ba), (1, bb)):
                k.op('act', 'copy', R=[b.res], W=[oT.res], out=oT.ap[:, hf * 4:(hf + 1) * 4, :], in_=b.ap.rearrange("p (c t) -> p c t", c=4))
            ba, bb = pair()
            for hf, b in ((0, ba), (1, bb)):
                for c in range(DC):
                    k.op('pe', 'matmul', R=[oT.res, wo.res], W=[b.res], out=b.ap, lhsT=oT.ap[:, c, :], rhs=wo.ap[:, c, hf * 512:(hf + 1) * 512],
                         start=(c == 0), stop=(c == DC - 1))
                k.op('dve', 'tensor_tensor', R=[b.res, x1.res], W=[x2.res], out=x2.ap[:, hf * 512:(hf + 1) * 512], in0=b.ap,
                     in1=x1.ap[:, hf * 512:(hf + 1) * 512], op=ALU.add)
            k.dma('pool', x2_s.ap[ts, :], x2.ap, reads=[x2.res], writes=[x2_s.res])
        cx.pop()

    if 'D' in phases:
        cx.push()
        pwq = cx.sb("pwq", [P, DC, 2 * D], BF16)
        load_cast([pwq.ap[:, c, :] for c in range(DC)], [pwq_d.ap[c * P:(c + 1) * P, :] for c in range(DC)], pwq.res)
        skT = cx.sb("skT", [P, 16, P], BF16)
        load_cast([skT.ap], [skT_d.ap], skT.res)
        g_ffn = make_gbc(3, "g_ffn")
        ffn_bc = cx.sb("ffn_bc", [P, D], F32)
        fin_bc = cx.sb("fin_bc", [P, D], F32)
        gt = grow_d.ap.tensor
        k.dma('sp', ffn_bc.ap, bass.AP(gt, 0, [[0, P], [1, D]]), writes=[ffn_bc.res])
        k.dma('sp', fin_bc.ap, bass.AP(gt, D, [[0, P], [1, D]]), writes=[fin_bc.res])
        iota_i = cx.sb("iota_i", [P, 16], I32)
        k.op('pool', 'iota', W=[iota_i.res], out=iota_i.ap, pattern=[[1, 16]], base=0, channel_multiplier=0)
        iota16 = cx.sb("iota16", [P, 16], F32)
        k.op('dve', 'tensor_copy', R=[iota_i.res], W=[iota16.res], out=iota16.ap, in_=iota_i.ap)
        thr16 = cx.sb("thr16", [P, 16], F32)
        k.op('dve', 'tensor_scalar', R=[iota16.res], W=[thr16.res], out=thr16.ap, in0=iota16.ap, scalar1=16.0, scalar2=15.5,
             op0=ALU.mult, op1=ALU.add)
        junk2 = cx.sb("junk2", [P, D], F32)

        NB = 1
        x2t = [cx.sb(f"x2t{i}", [P, D], F32) for i in range(2)]
        h3 = [cx.sb(f"h3{i}", [P, D], F32) for i in range(NB)]
        xn3 = [cx.sb(f"xn3{i}", [P, D], BF16) for i in range(NB)]
        h3T = [cx.sb(f"h3T{i}", [P, DC, P], BF16) for i in range(NB)]
        pqT = [cx.sb(f"pqT{i}", [P, 16, P], BF16) for i in range(NB)]
        ssb_ = [cx.sb(f"ssb{i}", [P, 16, P], F32) for i in range(NB)]
        s2b_ = cx.sb("s2b", [P, 16, P], F32)
        cand2 = Buf(s2b_.ap.rearrange("p a b -> p (a b)").rearrange("p (h c) -> p h c", h=PH), s2b_.res)
        mxs = [cx.sb(f"mxs{i}", [P, 16, 16], F32) for i in range(NB)]
        ixs = [cx.sb(f"ixs{i}", [P, 16, 16], U32) for i in range(NB)]
        ixf = [cx.sb(f"ixf{i}", [P, 16, 16], F32) for i in range(NB)]
        cand = [cx.sb(f"cand{i}", [P, PH, 256], F32) for i in range(NB)]
        best = [cx.sb(f"best{i}", [P, PH, 16], F32) for i in range(NB)]
        pos = [cx.sb(f"pos{i}", [P, PH, 16], U32) for i in range(NB)]
        posf = [cx.sb(f"posf{i}", [P, PH, 16], F32) for i in range(NB)]
        paf = [cx.sb(f"paf{i}", [P, PH, 16], F32) for i in range(NB)]
        pbf = [cx.sb(f"pbf{i}", [P, PH, 16], F32) for i in range(NB)]
        oh = cx.sb("oh", [P, PH, 16, 16], F32)
        oh2 = cx.sb("oh2", [P, PH, 16, 16], F32)
        i1s = [cx.sb(f"i1s{i}", [P, PH, 16], F32) for i in range(NB)]
        i2s = [cx.sb(f"i2s{i}", [P, PH, 16], F32) for i in range(NB)]
        eidf = [cx.sb(f"eidf{i}", [P, PH * 16], F32) for i in range(NB)]
        eid = [cx.sb(f"eid{i}", [P, PH * 16], U32) for i in range(NB)]
        gate = [cx.sb(f"gate{i}", [P, PH, 16], F32) for i in range(NB)]
        gst_ = [cx.sb(f"gstat{i}", [P, 16], F32) for i in range(NB)]
        yacc = [cx.sb(f"yacc{i}", [P, D], F32) for i in range(2)]
        st3 = [cx.sb(f"st3{i}", [P, 4], F32) for i in range(NB)]
        NSL = 14
        GK = 8
        uvg = [cx.sb(f"uvg{i}", [P, 2 * D], BF16) for i in range(NSL)]
        sdg = [cx.sb(f"sdg{i}", [P, GK], F32) for i in range(2)]
        avg = [cx.sb(f"avg{i}", [P, GK], F32) for i in range(2)]
        trot = [0]
        gsl = [0, 0]

        for tt in range(NT):
            r = 0
            ts = slice(tt * P, (tt + 1) * P)
            x2, h3_, xn_b, hT3, pq, ssb, mx, ix, ixf_ = x2t[tt % 2], h3[r], xn3[r], h3T[r], pqT[r], ssb_[r], mxs[r], ixs[r], ixf[r]
            cd, bst, ps_, psf, pa, pb_ = cand[r], best[r], pos[r], posf[r], paf[r], pbf[r]
            st = st3[r]
            k.dma('sp', x2.ap, x2_s.ap[ts, :], reads=[x2_s.res], writes=[x2.res])
            k.op('act', 'activation', R=[x2.res], W=[junk2.res, st.res], out=junk2.ap, in_=x2.ap, func=AF.Square, accum_out=st.ap[:, 0:1])
            k.op('act', 'activation', R=[st.res], W=[st.res], out=st.ap[:, 1:2], in_=st.ap[:, 0:1], func=AF.Ln, scale=1.0 / D, bias=EPS)
            k.op('act', 'activation', R=[st.res], W=[st.res], out=st.ap[:, 1:2], in_=st.ap[:, 1:2], func=AF.Exp, scale=-0.5)
            k.op('dve', 'tensor_scalar', R=[x2.res, st.res], W=[xn_b.res], out=xn_b.ap, in0=x2.ap, scalar1=st.ap[:, 1:2], scalar2=None, op0=ALU.mult)
            k.op('dve', 'scalar_tensor_tensor', R=[x2.res, st.res, ffn_bc.res], W=[h3_.res], out=h3_.ap, in0=x2.ap, scalar=st.ap[:, 1:2],
                 in1=ffn_bc.ap, op0=ALU.mult, op1=ALU.mult)
            pb = banks[trot[0] % 2]
            trot[0] += 1
            pview = pb.ap.bitcast(BF16).rearrange("p (c t) -> p c t", c=DC)
            for c in range(DC):
                k.op('pe', 'transpose', R=[xn_b.res, ident_b.res], W=[pb.res], out=pview[:, c, :], in_=xn_b.ap[:, c * P:(c + 1) * P], identity=ident_b.ap)
            k.op('dve', 'tensor_tensor', R=[pb.res, g_ffn.res], W=[hT3.res], out=hT3.ap, in0=pview, in1=g_ffn.ap, op=ALU.mult)
            for bq in range(4):
                b = banks[2 + bq]
                for cc in range(4):
                    c = bq * 4 + cc
                    for dc in range(DC):
                        k.op('pe', 'matmul', R=[pwq.res, hT3.res], W=[b.res], out=b.ap[:, cc * P:(cc + 1) * P], lhsT=pwq.ap[:, dc, c * P:(c + 1) * P],
                             rhs=hT3.ap[:, dc, :], start=(dc == 0), stop=(dc == DC - 1))
                k.op('act', 'copy', R=[b.res], W=[pq.res], out=pq.ap[:, bq * 4:(bq + 1) * 4, :], in_=b.ap.rearrange("p (c t) -> p c t", c=4))
            for bq in range(4):
                b = banks[2 + (bq + 2) % 4 + 0] if False else banks[6 + bq % 2] if False else banks[2 + bq]
                for cc in range(4):
                    hp = bq * 4 + cc
                    k.op('pe', 'matmul', R=[pq.res, skT.res], W=[b.res], out=b.ap[:, cc * P:(cc + 1) * P], lhsT=pq.ap[:, hp, :], rhs=skT.ap[:, hp, :],
                         start=True, stop=True)
                k.op('act', 'copy', R=[b.res], W=[ssb.res], out=ssb.ap[:, bq * 4:(bq + 1) * 4, :], in_=b.ap.rearrange("p (c t) -> p c t", c=4))
            for hp in range(16):
                k.op('dve', 'max', R=[ssb.res], W=[mx.res], out=mx.ap[:, hp, 0:8], in_=ssb.ap[:, hp, :])
                k.op('dve', 'match_replace', R=[mx.res, ssb.res], W=[s2b_.res], out=s2b_.ap[:, hp, :], in_to_replace=mx.ap[:, hp, 0:8],
                     in_values=ssb.ap[:, hp, :], imm_value=NEG)
                k.op('dve', 'max', R=[s2b_.res], W=[mx.res], out=mx.ap[:, hp, 8:16], in_=s2b_.ap[:, hp, :])
                k.op('dve', 'max_index', R=[mx.res, ssb.res], W=[ix.res], out=ix.ap[:, hp, 0:8], in_max=mx.ap[:, hp, 0:8], in_values=ssb.ap[:, hp, :])
                k.op('dve', 'max_index', R=[mx.res, ssb.res], W=[ix.res], out=ix.ap[:, hp, 8:16], in_max=mx.ap[:, hp, 8:16], in_values=ssb.ap[:, hp, :])
            k.op('dve', 'tensor_copy', R=[ix.res], W=[ixf_.res], out=ixf_.ap, in_=ix.ap)
            mx4 = mx.ap.rearrange("p (h two) k -> p h two k", two=2)
            cd4 = cd.ap.rearrange("p h (a b) -> p h a b", a=16)
            k.op('dve', 'tensor_tensor', R=[mx.res], W=[cd.res], out=cd4, in0=bc_last(mx4[:, :, 0, :], 16), in1=bc_mid(mx4[:, :, 1, :], 2, 16), op=ALU.add)
            for hh in range(PH):
                k.op('dve', 'max', R=[cd.res], W=[bst.res], out=bst.ap[:, hh, 0:8], in_=cd.ap[:, hh, :])
                k.op('dve', 'match_replace', R=[bst.res, cd.res], W=[cand2.res], out=cand2.ap[:, hh, :], in_to_replace=bst.ap[:, hh, 0:8],
                     in_values=cd.ap[:, hh, :], imm_value=NEG)
                k.op('dve', 'max', R=[cand2.res], W=[bst.res], out=bst.ap[:, hh, 8:16], in_=cand2.ap[:, hh, :])
                k.op('dve', 'max_index', R=[bst.res, cd.res], W=[ps_.res], out=ps_.ap[:, hh, 0:8], in_max=bst.ap[:, hh, 0:8], in_values=cd.ap[:, hh, :])
                k.op('dve', 'max_index', R=[bst.res, cd.res], W=[ps_.res], out=ps_.ap[:, hh, 8:16], in_max=bst.ap[:, hh, 8:16], in_values=cd.ap[:, hh, :])
            gs_, gt_ = gst_[r], gate[r]
            k.op('dve', 'tensor_tensor', R=[bst.res], W=[gt_.res], out=gt_.ap, in0=bst.ap, in1=bc_last(bst.ap[:, :, 0], 16), op=ALU.subtract)
            k.op('act', 'activation', R=[gt_.res], W=[gt_.res], out=gt_.ap, in_=gt_.ap, func=AF.Exp)
            k.op('dve', 'tensor_reduce', R=[gt_.res], W=[gs_.res], out=gs_.ap[:, 0:8], in_=gt_.ap, axis=AX.X, op=ALU.add)
            k.op('dve', 'reciprocal', R=[gs_.res], W=[gs_.res], out=gs_.ap[:, 8:16], in_=gs_.ap[:, 0:8])
            k.op('dve', 'tensor_tensor', R=[gt_.res, gs_.res], W=[gt_.res], out=gt_.ap, in0=gt_.ap, in1=bc_last(gs_.ap[:, 8:16], 16), op=ALU.mult)
            k.op('dve', 'tensor_copy', R=[ps_.res], W=[psf.res], out=psf.ap, in_=ps_.ap)
            thr_bc = bc_mid(bc_mid(thr16.ap, 1, 16), 1, PH)
            iota_bc = bc_mid(bc_mid(iota16.ap, 1, 16), 1, PH)
            k.op('dve', 'tensor_tensor', R=[psf.res, thr16.res], W=[oh.res], out=oh.ap, in0=bc_last(psf.ap, 16), in1=thr_bc, op=ALU.is_ge)
            k.op('dve', 'tensor_reduce', R=[oh.res], W=[pa.res], out=pa.ap, in_=oh.ap, axis=AX.X, op=ALU.add)
            k.op('dve', 'scalar_tensor_tensor', R=[pa.res, psf.res], W=[pb_.res], out=pb_.ap, in0=pa.ap, scalar=-16.0, in1=psf.ap, op0=ALU.mult, op1=ALU.add)
            ixf4 = ixf_.ap.rearrange("p (h two) k -> p h two k", two=2)
            for which, pidx, dsti in ((0, pa, i1s[r]), (1, pb_, i2s[r])):
                k.op('dve', 'tensor_tensor', R=[pidx.res, iota16.res], W=[oh.res], out=oh.ap, in0=bc_last(pidx.ap, 16), in1=iota_bc, op=ALU.is_equal)
                k.op('dve', 'tensor_tensor', R=[oh.res, ixf_.res], W=[oh2.res], out=oh2.ap, in0=oh.ap, in1=bc_mid(ixf4[:, :, which, :], 2, 16), op=ALU.mult)
                k.op('dve', 'tensor_reduce', R=[oh2.res], W=[dsti.res], out=dsti.ap, in_=oh2.ap, axis=AX.X, op=ALU.add)
            ef, ei = eidf[r], eid[r]
            k.op('dve', 'scalar_tensor_tensor', R=[i1s[r].res, i2s[r].res], W=[ef.res], out=ef.ap, in0=i1s[r].ap.rearrange("p h k -> p (h k)"),
                 scalar=float(PK), in1=i2s[r].ap.rearrange("p h k -> p (h k)"), op0=ALU.mult, op1=ALU.add)
            k.op('dve', 'tensor_copy', R=[ef.res], W=[ei.res], out=ei.ap, in_=ef.ap)
            ya = yacc[tt % 2]
            gflat = gt_.ap.rearrange("p h k -> p (h k)")
            for kg in range(PH * 16 // GK):
                sd, av = sdg[kg % 2], avg[kg % 2]
                slots = []
                for kk in range(kg * GK, (kg + 1) * GK):
                    u_ = uvg[gsl[0] % NSL]
                    gsl[0] += 1
                    slots.append(u_)
                    k.dma('pool', u_.ap, uv_s.ap, reads=[ei.res, uv_s.res], writes=[u_.res],
                          indirect=bass.IndirectOffsetOnAxis(ap=ei.ap[:, kk:kk + 1], axis=0))
                    k.op('dve', 'scalar_tensor_tensor', R=[u_.res, h3_.res], W=[junk2.res, sd.res], out=junk2.ap, in0=u_.ap[:, 0:D], scalar=1.0,
                         in1=h3_.ap, op0=ALU.mult, op1=ALU.mult, accum_out=sd.ap[:, kk - kg * GK:kk - kg * GK + 1])
                k.op('act', 'activation', R=[sd.res], W=[av.res], out=av.ap, in_=sd.ap, func=AF.Gelu)
                k.op('dve', 'tensor_tensor', R=[av.res, gt_.res], W=[av.res], out=av.ap, in0=av.ap, in1=gflat[:, kg * GK:(kg + 1) * GK], op=ALU.mult)
                for i_, u_ in enumerate(slots):
                    if kg == 0 and i_ == 0:
                        k.op('dve', 'tensor_scalar', R=[u_.res, av.res], W=[ya.res], out=ya.ap, in0=u_.ap[:, D:2 * D], scalar1=av.ap[:, 0:1], scalar2=None,
                             op0=ALU.mult)
                    else:
                        k.op('dve', 'scalar_tensor_tensor', R=[u_.res, av.res, ya.res], W=[ya.res], out=ya.ap, in0=u_.ap[:, D:2 * D],
                             scalar=av.ap[:, i_:i_ + 1], in1=ya.ap, op0=ALU.mult, op1=ALU.add)
            k.op('dve', 'tensor_tensor', R=[x2.res, ya.res], W=[ya.res], out=ya.ap, in0=x2.ap, in1=ya.ap, op=ALU.add)
            k.op('act', 'activation', R=[ya.res], W=[junk2.res, st.res], out=junk2.ap, in_=ya.ap, func=AF.Square, accum_out=st.ap[:, 2:3])
            k.op('act', 'activation', R=[st.res], W=[st.res], out=st.ap[:, 3:4], in_=st.ap[:, 2:3], func=AF.Ln, scale=1.0 / D, bias=EPS)
            k.op('act', 'activation', R=[st.res], W=[st.res], out=st.ap[:, 3:4], in_=st.ap[:, 3:4], func=AF.Exp, scale=-0.5)
            k.op('dve', 'scalar_tensor_tensor', R=[ya.res, st.res, fin_bc.res], W=[ya.res], out=ya.ap, in0=ya.ap, scalar=st.ap[:, 3:4], in1=fin_bc.ap,
                 op0=ALU.mult, op1=ALU.mult)
            o_ = ya
            k.dma('sp', out_d.ap[ts, :], o_.ap, reads=[o_.res], writes=[out_d.res])
        cx.pop()

    finals = []
    for b in (qkm_s, sbq_s, sbk_s, vm_s, og_s, vs_s, gates_s, mixed_s, x2_s, out_d):
        if b is out_d or b.res.name in debug:
            finals.extend(b.res.w)
    k.emit(finals)
    return nc, k


def prep_shared(inp):
    f = lambda a: np.ascontiguousarray(np.asarray(a, dtype=np.float32))
    g = lambda v: f(v).reshape(DC, P).T
    mixed_g = np.concatenate([f(inp['mlstm_norm_g'][0]), f(inp['sb_norm_g'][0])])
    gvec = np.stack([g(inp['mix_norm_g'][0]), g(inp['xattn_norm_g'][0]), g(inp['mem_norm_g'][0]),
                     g(inp['ffn_norm_g'][0]), g(mixed_g)], axis=1)
    sh = {
        'w_in': f(inp['w_in'][0]), 'w_out': f(inp['w_out'][0]), 'xattn_wq': f(inp['xattn_wq'][0]),
        'xattn_wkv': f(inp['xattn_wkv'][0]), 'xattn_wo': f(inp['xattn_wo'][0]), 'peer_wq': f(inp['peer_wq'][0]),
        'subkeysT': f(np.transpose(f(inp['peer_subkeys'][0]).reshape(16, P, P), (2, 0, 1))),
        'peer_u': f(inp['peer_u'][0]), 'peer_v': f(inp['peer_v'][0]),
        'gvec': f(gvec),
        'grow': f(np.stack([f(inp['ffn_norm_g'][0]), f(inp['final_norm_g'])])),
        'conv_wT': f(np.transpose(f(inp['conv_w'][0]).reshape(4, DC, P), (2, 1, 0))),
        'conv_bT': g(inp['conv_b'][0]),
        'gate_b': f(np.stack([f(inp['igate_b'][0]), f(inp['fgate_b'][0])], axis=1)),
    }
    return sh


_CACHE = {}


def kernel(**inputs):
    x = np.asarray(inputs['x'], dtype=np.float32)
    mem = np.asarray(inputs['mem'], dtype=np.float32)
    B, S, _ = x.shape
    sh = prep_shared(inputs)
    if S not in _CACHE:
        _CACHE[S] = build(S)[0]
    nc = _CACHE[S]
    in_maps = []
    for b in range(B):
        m = dict(sh)
        m['x'] = np.ascontiguousarray(x[b])
        m['mem'] = np.ascontiguousarray(mem[b])
        in_maps.append(m)
    res = run_bass_kernel_spmd(nc, in_maps, core_ids=list(range(B)))
    return np.stack([np.asarray(r['out'], dtype=np.float32) for r in res.results], axis=0)
```

```python
import contextlib
import os
import numpy as np
import concourse.bass as bass
import concourse.mybir as mybir
from concourse.bass_utils import run_bass_kernel_spmd

F32 = mybir.dt.float32
BF16 = mybir.dt.bfloat16
U32 = mybir.dt.uint32
I32 = mybir.dt.int32
ALU = mybir.AluOpType
AF = mybir.ActivationFunctionType
AX = mybir.AxisListType

P = 128
D = 1024
DC = D // P
NH_M, HD_M = 4, 128
NH_S, HD_S = 8, 64
PROJ = 3592
C_QKM, C_VM, C_OM, C_I, C_F, C_QS, C_KS, C_VS = 0, 1024, 1536, 2048, 2052, 2056, 2568, 3080
MEM = 256
NXH, XHD = 4, 256
PH, PK, PTOP = 8, 128, 16
NEXP = PK * PK
EPS = 1e-6
NEG = -1.0e30


class Res:
    def __init__(self, name, dram=False):
        self.name = name
        self.w = []
        self.r = []
        self.dsem = None
        self.dcnt = 0
        self.dram = dram


class Buf:
    def __init__(self, ap, res):
        self.ap = ap
        self.res = res


class KB:
    ENG = ('sp', 'pe', 'dve', 'act', 'pool')

    def __init__(self, nc):
        self.nc = nc
        self.sem = {e: nc.alloc_semaphore(name='s_' + e) for e in self.ENG}
        self.cnt = {e: 0 for e in self.ENG}
        self.waited = {e: {} for e in self.ENG}
        self.prog = {e: [] for e in self.ENG}
        self.nsem = len(self.ENG)
        self.ninst = 0
        self.owners = []
        self.marks = []
        self.rec = None

    def _wait(self, e, deps):
        for d in deps:
            if d is None:
                continue
            s, v = d
            key = id(s)
            if self.waited[e].get(key, 0) >= v:
                continue
            self.waited[e][key] = v
            self.prog[e].append(lambda eng, s=s, v=v: eng.wait_ge(s, v))

    @staticmethod
    def _deps(reads, writes, dma_write=False):
        deps = []
        for r in reads:
            deps.extend(r.w)
        for w in writes:
            if not (dma_write and w.dram):
                deps.extend(w.w)
            deps.extend(w.r)
        return deps

    def op(self, e, meth, R=(), W=(), **kw):
        if self.rec is not None:
            self.rec.append(('op', (e, meth), dict(R=R, W=W, **kw)))
            return None
        self._wait(e, self._deps(R, W))
        self.cnt[e] += 1
        self.ninst += 1
        sem = self.sem[e]
        ev = (sem, self.cnt[e])
        self.prog[e].append(lambda eng, meth=meth, kw=kw, sem=sem: getattr(eng, meth)(**kw).then_inc(sem, 1))
        for r in R:
            r.r.append(ev)
        for w in W:
            w.w = [ev]
            w.r = []
        return ev

    def dma(self, q, out, in_, reads=(), writes=(), indirect=None, **kw):
        if self.rec is not None:
            self.rec.append(('dma', (q, out, in_), dict(reads=reads, writes=writes, indirect=indirect, **kw)))
            return None
        self._wait(q, self._deps(reads, writes, dma_write=True))
        (dst,) = writes
        owner = dst if not dst.dram else [r for r in reads if not r.dram][0]
        if owner.dsem is None:
            owner.dsem = self.nc.alloc_semaphore(name='d_' + owner.name)
            self.nsem += 1
            self.owners.append(owner)
        owner.dcnt += 16
        self.ninst += 1
        ev = (owner.dsem, owner.dcnt)
        ds = owner.dsem
        if indirect is None:
            self.prog[q].append(lambda eng, out=out, in_=in_, kw=kw, ds=ds:
                                eng.dma_start(out=out, in_=in_, **kw).then_inc(ds, 16))
        else:
            self.prog[q].append(lambda eng, out=out, in_=in_, io=indirect, kw=kw, ds=ds:
                                eng.indirect_dma_start(out=out, out_offset=None, in_=in_, in_offset=io,
                                                       **kw).then_inc(ds, 16))
        for r in reads:
            r.r.append(ev)
        if dst.dram:
            dst.w = [x for x in dst.w if x[0] is not ds] + [ev]
        else:
            dst.w = [ev]
        dst.r = []
        return ev

    def record(self, fn):
        assert self.rec is None
        self.rec = []
        fn()
        out, self.rec = self.rec, None
        return out

    def play(self, items):
        for kind, a, kw in items:
            if kind == 'op':
                self.op(*a, **kw)
            else:
                self.dma(*a, **kw)

    def barrier(self):
        evs = [(self.sem[e], self.cnt[e]) for e in self.ENG if self.cnt[e] > 0]
        evs += [(o.dsem, o.dcnt) for o in self.owners]
        for e in self.ENG:
            self._wait(e, evs)

    def emit(self, final_events):
        self._wait('sp', final_events)
        prog = self.prog
        with self.nc.Block() as block:
            @block.sync
            def _(eng):
                for f in prog['sp']:
                    f(eng)

            @block.tensor
            def _(eng):
                for f in prog['pe']:
                    f(eng)

            @block.vector
            def _(eng):
                for f in prog['dve']:
                    f(eng)

            @block.scalar
            def _(eng):
                for f in prog['act']:
                    f(eng)

            @block.gpsimd
            def _(eng):
                for f in prog['pool']:
                    f(eng)


class Ctx:
    def __init__(self, nc, k):
        self.nc = nc
        self.k = k
        self.n = 0
        self.stack = [contextlib.ExitStack()]

    def push(self):
        self.stack.append(contextlib.ExitStack())

    def pop(self):
        self.k.marks.append(dict(self.k.cnt))
        self.k.barrier()
        self.stack.pop().close()

    def sb(self, name, shape, dt):
        self.n += 1
        t = self.stack[-1].enter_context(self.nc.sbuf_tensor(f"{name}_{self.n}", list(shape), dt))
        return Buf(t[:] if not hasattr(t, 'ap') else t.ap(), Res(f"{name}_{self.n}"))

    def dram(self, name, shape, dt, kind="Internal"):
        t = self.nc.dram_tensor(name, list(shape), dt, kind=kind)
        return Buf(t.ap(), Res(name, dram=True))


def bc_last(ap, n):
    return ap.unsqueeze(ap.ndim).to_broadcast(list(ap.shape) + [n])


def bc_mid(ap, axis, n):
    shp = list(ap.shape)
    shp.insert(axis, n)
    return ap.unsqueeze(axis).to_broadcast(shp)


def build(S, phases=('A', 'B', 'C', 'D'), debug=()):
    NT = S // P
    NG = S // 512
    assert S % 512 == 0
    nc = bass.Bass("TRN2", target_bir_lowering=False)
    k = KB(nc)
    cx = Ctx(nc, k)

    def dkind(name):
        return "ExternalOutput" if name in debug else "Internal"

    def din(name, shape, dt=F32):
        return Buf(nc.dram_tensor(name, list(shape), dt, kind="ExternalInput").ap(), Res(name, dram=True))

    x_d = din("x", [S, D])
    mem_d = din("mem", [MEM, D])
    w_in_d = din("w_in", [D, PROJ])
    w_out_d = din("w_out", [D, D])
    wq_d = din("xattn_wq", [D, D])
    wkv_d = din("xattn_wkv", [D, 2 * D])
    wo_d = din("xattn_wo", [D, D])
    pwq_d = din("peer_wq", [D, 2 * D])
    skT_d = din("subkeysT", [P, 16, P])
    pu_d = din("peer_u", [NEXP, D])
    pv_d = din("peer_v", [NEXP, D])
    gvec_d = din("gvec", [P, 5, DC])
    grow_d = din("grow", [2, D])
    convw_d = din("conv_wT", [P, DC, 4])
    convb_d = din("conv_bT", [P, DC])
    gateb_d = din("gate_b", [4, 2])
    out_d = Buf(nc.dram_tensor("out", [S, D], F32, kind="ExternalOutput").ap(), Res("out", dram=True))

    qkm_s = cx.dram("qkm_s", [DC, P, S], BF16, kind=dkind("qkm_s"))
    sbq_s = cx.dram("sbq_s", [NH_S * HD_S, S], BF16, kind=dkind("sbq_s"))
    sbk_s = cx.dram("sbk_s", [NH_S * HD_S, S], BF16, kind=dkind("sbk_s"))
    vm_s = cx.dram("vm_s", [S, NH_M * HD_M], BF16, kind=dkind("vm_s"))
    og_s = cx.dram("og_s", [S, NH_M * HD_M], BF16, kind=dkind("og_s"))
    vs_s = cx.dram("vs_s", [S, NH_S * HD_S], BF16, kind=dkind("vs_s"))
    gates_s = cx.dram("gates_s", [4, 2, S], F32, kind=dkind("gates_s"))
    mixed_s = cx.dram("mixed_s", [S, D], BF16, kind=dkind("mixed_s"))
    x2_s = cx.dram("x2_s", [S, D], F32, kind=dkind("x2_s"))
    uv_s = cx.dram("uv_s", [NEXP, 2 * D], BF16)

    banks = []
    for i in range(8):
        t = nc.alloc_psum_tensor(f"bank{i}", [P, 512], F32)
        banks.append(Buf(t.ap(), Res(f"bank{i}")))

    ident_b = cx.sb("ident_b", [P, P], BF16)
    ident_f = cx.sb("ident_f", [P, P], F32)
    for idt in (ident_b, ident_f):
        k.op('pool', 'memset', W=[idt.res], ap=idt.ap, constant=1.0)
        k.op('pool', 'affine_select', R=[idt.res], W=[idt.res], out=idt.ap, in_=idt.ap, pattern=[[-1, P]],
             compare_op=ALU.is_equal, fill=0.0, base=0, channel_multiplier=1)
    gvec = cx.sb("gvec", [P, 5, DC], F32)
    k.dma('sp', gvec.ap, gvec_d.ap, writes=[gvec.res])
    convw = cx.sb("convw", [P, DC, 4], F32)
    k.dma('sp', convw.ap, convw_d.ap, writes=[convw.res])
    convb = cx.sb("convb", [P, DC], F32)
    k.dma('sp', convb.ap, convb_d.ap, writes=[convb.res])
    gateb = cx.sb("gateb", [4, 2], F32)
    k.dma('sp', gateb.ap, gateb_d.ap, writes=[gateb.res])

    STG = 2048
    stage = [cx.sb(f"stage{i}", [P, STG], F32) for i in range(2)]
    stage_i = [0]

    def load_cast(dst_aps, src_aps, dst_res, cast_engs=('dve', 'pool')):
        for dst, src in zip(dst_aps, src_aps):
            st = stage[stage_i[0] % 2]
            n = int(np.prod(src.shape[1:]))
            sview = st.ap[:, 0:n]
            if len(src.shape) == 3:
                sview = sview.rearrange("p (a b) -> p a b", a=src.shape[1])
            k.dma('sp', sview, src, writes=[st.res])
            eng = cast_engs[stage_i[0] % len(cast_engs)]
            k.op(eng, 'tensor_copy', R=[st.res], W=[dst_res], out=dst, in_=sview)
            stage_i[0] += 1

    def make_gbc(idx, name):
        g = cx.sb(name, [P, DC, P], F32)
        k.op('dve', 'tensor_copy', R=[gvec.res], W=[g.res], out=g.ap, in_=bc_last(gvec.ap[:, idx, :], P))
        return g

    def rms_stats(ss, rstd, n_cols):
        k.op('act', 'activation', R=[ss.res], W=[rstd.res], out=rstd.ap[:, 0:n_cols], in_=ss.ap[:, 0:n_cols],
             func=AF.Ln, scale=1.0 / D, bias=EPS)
        k.op('act', 'activation', R=[rstd.res], W=[rstd.res], out=rstd.ap[:, 0:n_cols], in_=rstd.ap[:, 0:n_cols],
             func=AF.Exp, scale=-0.5)

    if 'D' in phases:
        cx.push()
        RT = 2
        tin_u = [cx.sb(f"tin_u{i}", [P, RT, D], F32) for i in range(2)]
        tin_v = [cx.sb(f"tin_v{i}", [P, RT, D], F32) for i in range(2)]
        tout = [cx.sb(f"tout{i}", [P, RT, 2 * D], BF16) for i in range(2)]
        nchunk = NEXP // (P * RT)
        for c in range(nchunk):
            rs_ = slice(c * P * RT, (c + 1) * P * RT)
            tu, tv, to = tin_u[c % 2], tin_v[c % 2], tout[c % 2]
            k.dma('sp', tu.ap, pu_d.ap[rs_, :].rearrange("(p r) d -> p r d", r=RT), writes=[tu.res])
            k.dma('sp', tv.ap, pv_d.ap[rs_, :].rearrange("(p r) d -> p r d", r=RT), writes=[tv.res])
            k.op('dve', 'tensor_copy', R=[tu.res], W=[to.res], out=to.ap[:, :, 0:D], in_=tu.ap)
            k.op('act', 'copy', R=[tv.res], W=[to.res], out=to.ap[:, :, D:2 * D], in_=tv.ap)
            k.dma('pool', uv_s.ap[rs_, :].rearrange("(p r) d -> p r d", r=RT), to.ap, reads=[to.res], writes=[uv_s.res])
        cx.pop()

    if 'A' in phases:
        cx.push()
        gst = [cx.sb(f"gst{i}", [4, 2, 512], F32) for i in range(2)]
        w_in = cx.sb("w_in", [P, DC, PROJ], BF16)
        load_cast([w_in.ap[:, c, a:min(a + STG, PROJ)] for c in range(DC) for a in range(0, PROJ, STG)],
                  [w_in_d.ap[c * P:(c + 1) * P, a:min(a + STG, PROJ)] for c in range(DC) for a in range(0, PROJ, STG)], w_in.res)
        g_mix = make_gbc(0, "g_mix")
        xg = [cx.sb(f"xg{i}", [P, 4, D], F32) for i in range(2)]
        xn = cx.sb("xn", [P, 4, D], BF16)
        junk = cx.sb("junk", [P, D], F32)
        ssq = [cx.sb(f"ssq{i}", [P, 4], F32) for i in range(2)]
        rstd = [cx.sb(f"rstd{i}", [P, 4], F32) for i in range(2)]
        hT = [cx.sb(f"hT{i}", [P, DC, 512], BF16) for i in range(2)]
        pre = cx.sb("pre", [P, DC, 3 + 512], F32)
        cacc = [cx.sb(f"cacc{i}", [P, 512], F32) for i in range(2)]
        qkm_o = [cx.sb(f"qkm_o{i}", [P, 512], BF16) for i in range(3)]
        sb_o = [cx.sb(f"sb_o{i}", [P, 512], BF16) for i in range(3)]
        tm_o = [cx.sb(f"tm_o{i}", [P, 512], BF16) for i in range(4)]
        k.op('pool', 'memset', W=[pre.res], ap=pre.ap[:, :, 0:3], constant=0.0)
        rot = {'mm': 0, 'q': 0, 's': 0, 't': 0}
        MMB = [2, 3, 4, 5, 6, 7]

        def mm_bank():
            b = banks[MMB[rot['mm'] % len(MMB)]]
            rot['mm'] += 1
            return b

        def load_x(g):
            xb = xg[g % 2]
            k.dma('sp', xb.ap, x_d.ap[g * 512:(g + 1) * 512, :].rearrange("(a p) d -> p a d", p=P), writes=[xb.res])

        load_x(0)
        for g in range(NG):
            if g + 1 < NG:
                load_x(g + 1)
            xb, ss, rs, hTg = xg[g % 2], ssq[g % 2], rstd[g % 2], hT[g % 2]
            gs = slice(g * 512, (g + 1) * 512)
            for tl in range(4):
                k.op('act', 'activation', R=[xb.res], W=[junk.res, ss.res], out=junk.ap, in_=xb.ap[:, tl, :],
                     func=AF.Square, accum_out=ss.ap[:, tl:tl + 1])
            rms_stats(ss, rs, 4)
            for tl in range(4):
                k.op('dve', 'tensor_scalar', R=[xb.res, rs.res], W=[xn.res], out=xn.ap[:, tl, :], in0=xb.ap[:, tl, :],
                     scalar1=rs.ap[:, tl:tl + 1], scalar2=None, op0=ALU.mult)
            for tl in range(4):
                pb = banks[tl % 2]
                pview = pb.ap.bitcast(BF16).rearrange("p (c t) -> p c t", c=DC)
                for c in range(DC):
                    k.op('pe', 'transpose', R=[xn.res, ident_b.res], W=[pb.res], out=pview[:, c, :],
                         in_=xn.ap[:, tl, c * P:(c + 1) * P], identity=ident_b.ap)
                k.op('dve', 'tensor_tensor', R=[pb.res, g_mix.res], W=[hTg.res], out=hTg.ap[:, :, tl * P:(tl + 1) * P],
                     in0=pview, in1=g_mix.ap, op=ALU.mult)

            def fm_matmul(col0, M):
                b = mm_bank()
                for c in range(DC):
                    k.op('pe', 'matmul', R=[w_in.res, hTg.res], W=[b.res], out=b.ap[0:M, :], lhsT=w_in.ap[:, c, col0:col0 + M],
                         rhs=hTg.ap[:, c, :], start=(c == 0), stop=(c == DC - 1))
                return b

            for c in range(DC):
                b = fm_matmul(C_QKM + c * P, P)
                k.op('act', 'copy', R=[b.res], W=[pre.res], out=pre.ap[:, c, 3:515], in_=b.ap)
            for c in range(DC):
                ca = cacc[c % 2]
                k.op('dve', 'tensor_scalar', R=[pre.res, convw.res, convb.res], W=[ca.res], out=ca.ap, in0=pre.ap[:, c, 0:512],
                     scalar1=convw.ap[:, c, 0:1], scalar2=convb.ap[:, c:c + 1], op0=ALU.mult, op1=ALU.add)
                for j in range(1, 4):
                    k.op('dve', 'scalar_tensor_tensor', R=[pre.res, convw.res, ca.res], W=[ca.res], out=ca.ap,
                         in0=pre.ap[:, c, j:j + 512], scalar=convw.ap[:, c, j:j + 1], in1=ca.ap, op0=ALU.mult, op1=ALU.add)
                qo = qkm_o[rot['q'] % 3]
                rot['q'] += 1
                k.op('act', 'activation', R=[ca.res], W=[qo.res], out=qo.ap, in_=ca.ap, func=AF.Silu)
                k.dma('pool', qkm_s.ap[c, :, gs], qo.ap, reads=[qo.res], writes=[qkm_s.res])
            k.op('dve', 'tensor_copy', R=[pre.res], W=[pre.res], out=pre.ap[:, :, 0:3], in_=pre.ap[:, :, 512:515])
            for col0, dst in ((C_QS, sbq_s), (C_KS, sbk_s)):
                for c in range(4):
                    b = fm_matmul(col0 + c * P, P)
                    so = sb_o[rot['s'] % 3]
                    rot['s'] += 1
                    k.op('act', 'copy', R=[b.res], W=[so.res], out=so.ap, in_=b.ap)
                    k.dma('pool', dst.ap[c * P:(c + 1) * P, gs], so.ap, reads=[so.res], writes=[dst.res])
            gsb = gst[g % 2]
            for gi, col0 in ((0, C_I), (1, C_F)):
                b = fm_matmul(col0, 4)
                k.op('act', 'activation', R=[b.res, gateb.res], W=[gsb.res], out=gsb.ap[:, gi, :], in_=b.ap[0:4, :],
                     func=AF.Identity, bias=gateb.ap[:, gi:gi + 1])
            k.dma('pool', gates_s.ap[:, :, gs], gsb.ap, reads=[gsb.res], writes=[gates_s.res])
            for tl in range(4):
                tt = g * 4 + tl
                for col0, dst, fn in ((C_VM, vm_s, None), (C_OM, og_s, AF.Sigmoid), (C_VS, vs_s, None)):
                    b = mm_bank()
                    for c in range(DC):
                        k.op('pe', 'matmul', R=[w_in.res, hTg.res], W=[b.res], out=b.ap, lhsT=hTg.ap[:, c, tl * P:(tl + 1) * P],
                             rhs=w_in.ap[:, c, col0:col0 + 512], start=(c == 0), stop=(c == DC - 1))
                    to = tm_o[rot['t'] % 4]
                    rot['t'] += 1
                    if fn is None:
                        k.op('dve', 'tensor_copy', R=[b.res], W=[to.res], out=to.ap, in_=b.ap)
                    else:
                        k.op('act', 'activation', R=[b.res], W=[to.res], out=to.ap, in_=b.ap, func=fn)
                    k.dma('pool', dst.ap[tt * P:(tt + 1) * P, :], to.ap, reads=[to.res], writes=[dst.res])
        cx.pop()

    if 'B' in phases:
        cx.push()
        KSC = float(HD_M) ** -0.5
        g4 = cx.sb("g4", [4, 2, S], F32)
        k.dma('sp', g4.ap, gates_s.ap, reads=[gates_s.res], writes=[g4.res])
        t2 = cx.sb("t2", [4, S], F32)
        k.op('act', 'activation', R=[g4.res], W=[t2.res], out=t2.ap, in_=g4.ap[:, 1, :], func=AF.Exp, scale=-1.0)
        k.op('act', 'activation', R=[t2.res], W=[t2.res], out=t2.ap, in_=t2.ap, func=AF.Ln, bias=1.0)
        ones4 = cx.sb("ones4", [4, S], F32)
        k.op('pool', 'memset', W=[ones4.res], ap=ones4.ap, constant=1.0)
        csum = cx.sb("csum", [4, S], F32)
        k.op('dve', 'tensor_tensor_scan', R=[ones4.res, t2.res], W=[csum.res], out=csum.ap, data0=ones4.ap, data1=t2.ap,
             initial=0.0, op0=ALU.mult, op1=ALU.add)
        mi = cx.sb("mi", [4, 1], F32)
        k.op('dve', 'tensor_reduce', R=[g4.res], W=[mi.res], out=mi.ap, in_=g4.ap[:, 0, :], axis=AX.X, op=ALU.max)
        crow = cx.sb("crow", [4, S], F32)
        k.op('dve', 'scalar_tensor_tensor', R=[g4.res, mi.res, csum.res], W=[crow.res], out=crow.ap, in0=g4.ap[:, 0, :],
             scalar=mi.ap[:, 0:1], in1=csum.ap, op0=ALU.subtract, op1=ALU.add)
        brow = cx.sb("brow", [4, S], F32)
        k.op('dve', 'tensor_scalar', R=[csum.res], W=[brow.res], out=brow.ap, in0=csum.ap, scalar1=-1.0, scalar2=None, op0=ALU.mult)
        em = cx.sb("em", [4, 1], F32)
        k.op('act', 'activation', R=[mi.res], W=[em.res], out=em.ap, in_=mi.ap, func=AF.Exp, scale=-1.0)
        sel = cx.sb("sel", [4, 4, P], F32)
        k.op('dve', 'tensor_copy', R=[ident_f.res], W=[sel.res], out=sel.ap, in_=bc_last(ident_f.ap[0:4, 0:4], P))
        dgem = cx.sb("dgem", [4, 4], F32)
        k.op('dve', 'tensor_scalar', R=[ident_f.res, em.res], W=[dgem.res], out=dgem.ap, in0=ident_f.ap[0:4, 0:4],
             scalar1=em.ap[:, 0:1], scalar2=None, op0=ALU.mult)
        ones4p = cx.sb("ones4p", [4, P], F32)
        k.op('pool', 'memset', W=[ones4p.res], ap=ones4p.ap, constant=1.0)
        ccol = cx.sb("ccol", [P, NT, 4], F32)
        embc = cx.sb("embc", [P, 4], F32)
        b2 = banks[2]
        for blk in range(NT):
            k.op('pe', 'matmul', R=[crow.res, ident_f.res], W=[b2.res], out=b2.ap[:, blk * 4:(blk + 1) * 4],
                 lhsT=crow.ap[:, blk * P:(blk + 1) * P], rhs=ident_f.ap[0:4, 0:4], start=True, stop=True)
        k.op('dve', 'tensor_copy', R=[b2.res], W=[ccol.res], out=ccol.ap, in_=b2.ap[:, 0:NT * 4].rearrange("p (n h) -> p n h", h=4))
        b3 = banks[3]
        k.op('pe', 'matmul', R=[ones4p.res, dgem.res], W=[b3.res], out=b3.ap[:, 0:4], lhsT=ones4p.ap, rhs=dgem.ap, start=True, stop=True)
        k.op('dve', 'tensor_copy', R=[b3.res], W=[embc.res], out=embc.ap, in_=b3.ap[:, 0:4])

        qTb = cx.sb("qTb", [P, S], BF16)
        kTb = cx.sb("kTb", [P, S], BF16)
        v1 = cx.sb("v1", [P, NT, HD_M + 1], BF16)
        k.op('pool', 'memset', W=[v1.res], ap=v1.ap, constant=1.0)
        Bbc = cx.sb("Bbc", [P, S], F32)
        Dt = [cx.sb(f"Dt{i}", [P, 512], F32) for i in range(2)]
        At = [cx.sb(f"At{i}", [P, 512], BF16) for i in range(2)]
        ogt = [cx.sb(f"ogt{i}", [P, 4, HD_M], BF16) for i in range(2)]
        hm = [cx.sb(f"hm{i}", [P, 4, HD_M], F32) for i in range(2)]
        hsq = cx.sb("hsq", [P, HD_M], F32)
        dn = [cx.sb(f"dn{i}", [P, 4], F32) for i in range(2)]
        hss = [cx.sb(f"hss{i}", [P, 4], F32) for i in range(2)]
        hrs = [cx.sb(f"hrs{i}", [P, 4], F32) for i in range(2)]
        mo = [cx.sb(f"mo{i}", [P, 4, HD_M], BF16) for i in range(2)]
        it = 0
        ch = 0
        for h in range(int(os.environ.get('NHB', NH_M))):
            k.dma('sp', qTb.ap, qkm_s.ap[h], reads=[qkm_s.res], writes=[qTb.res])
            k.dma('sp', kTb.ap, qkm_s.ap[4 + h], reads=[qkm_s.res], writes=[kTb.res])
            with nc.allow_non_contiguous_dma(reason="per-head v slices, 256B runs"):
                k.dma('sp', v1.ap[:, :, 0:HD_M], vm_s.ap[:, h * HD_M:(h + 1) * HD_M].rearrange("(n p) d -> p n d", p=P),
                      reads=[vm_s.res], writes=[v1.res])
            for c5 in range(S // 512):
                bb = banks[2 + c5 % 2]
                k.op('pe', 'matmul', R=[sel.res, brow.res], W=[bb.res], out=bb.ap, lhsT=sel.ap[:, h, :],
                     rhs=brow.ap[:, c5 * 512:(c5 + 1) * 512], start=True, stop=True)
                k.op('act', 'copy', R=[bb.res], W=[Bbc.res], out=Bbc.ap[:, c5 * 512:(c5 + 1) * 512], in_=bb.ap)
            for q in range(NG):
                og = ogt[ch % 2]
                with nc.allow_non_contiguous_dma(reason="per-head o-gate slices"):
                    k.dma('sp', og.ap, og_s.ap[q * 512:(q + 1) * 512, h * HD_M:(h + 1) * HD_M].rearrange("(a p) d -> p a d", p=P),
                          reads=[og_s.res], writes=[og.res])
                accs = [banks[4 + i] for i in range(4)]
                steps = []
                for j in range(4 * q + 4):
                    t0 = max(P * j, 512 * q)
                    steps.append(dict(j=j, t0=t0, n=512 * (q + 1) - t0, zb=banks[it % 2], dt_=Dt[it % 2], at_=At[it % 2]))
                    it += 1

                def bstage1(st):
                    j, t0, n, zb, dt_, at_ = st['j'], st['t0'], st['n'], st['zb'], st['dt_'], st['at_']
                    k.op('pe', 'matmul', R=[kTb.res, qTb.res], W=[zb.res], out=zb.ap[:, 0:n], lhsT=kTb.ap[:, j * P:(j + 1) * P],
                         rhs=qTb.ap[:, t0:t0 + n], start=True, stop=True)
                    k.op('act', 'activation', R=[Bbc.res, ccol.res], W=[dt_.res], out=dt_.ap[:, 0:n], in_=Bbc.ap[:, t0:t0 + n],
                         func=AF.Exp, bias=ccol.ap[:, j, h:h + 1])
                    if j >= 4 * q:
                        k.op('pool', 'affine_select', R=[dt_.res], W=[dt_.res], out=dt_.ap[:, 0:P], in_=dt_.ap[:, 0:P],
                             pattern=[[1, P]], compare_op=ALU.is_ge, fill=0.0, base=0, channel_multiplier=-1)
                    k.op('dve', 'scalar_tensor_tensor', R=[zb.res, dt_.res], W=[at_.res], out=at_.ap[:, 0:n], in0=zb.ap[:, 0:n],
                         scalar=KSC, in1=dt_.ap[:, 0:n], op0=ALU.mult, op1=ALU.mult)

                def bstage2(st):
                    j, t0, at_ = st['j'], st['t0'], st['at_']
                    for i in range(max(j, 4 * q), 4 * q + 4):
                        o = i * P - t0
                        acc = accs[i - 4 * q]
                        k.op('pe', 'matmul', R=[at_.res, v1.res], W=[acc.res], out=acc.ap[:, 0:HD_M + 1], lhsT=at_.ap[:, o:o + P],
                             rhs=v1.ap[:, j, :], start=(j == 0), stop=(j == i))

                ns = len(steps)
                for x_ in range(ns + 1):
                    if x_ < ns:
                        bstage1(steps[x_])
                    if 0 <= x_ - 1 < ns:
                        bstage2(steps[x_ - 1])
                d_, ss_, rs_, hm_, mo_ = dn[ch % 2], hss[ch % 2], hrs[ch % 2], hm[ch % 2], mo[ch % 2]
                for i in range(4):
                    acc = accs[i]
                    k.op('act', 'activation', R=[acc.res], W=[d_.res], out=d_.ap[:, i:i + 1], in_=acc.ap[:, HD_M:HD_M + 1], func=AF.Abs)
                k.op('dve', 'tensor_scalar', R=[d_.res, embc.res], W=[d_.res], out=d_.ap, in0=d_.ap, scalar1=embc.ap[:, h:h + 1],
                     scalar2=None, op0=ALU.max)
                k.op('dve', 'reciprocal', R=[d_.res], W=[d_.res], out=d_.ap, in_=d_.ap)
                for i in range(4):
                    acc = accs[i]
                    k.op('dve', 'scalar_tensor_tensor', R=[acc.res, d_.res, og.res], W=[hm_.res], out=hm_.ap[:, i, :], in0=acc.ap[:, 0:HD_M],
                         scalar=d_.ap[:, i:i + 1], in1=og.ap[:, i, :], op0=ALU.mult, op1=ALU.mult)
                    k.op('dve', 'scalar_tensor_tensor', R=[hm_.res], W=[hsq.res, ss_.res], out=hsq.ap, in0=hm_.ap[:, i, :], scalar=1.0, in1=hm_.ap[:, i, :],
                         op0=ALU.mult, op1=ALU.mult, accum_out=ss_.ap[:, i:i + 1])
                k.op('act', 'activation', R=[ss_.res], W=[rs_.res], out=rs_.ap, in_=ss_.ap, func=AF.Ln, scale=1.0 / HD_M, bias=EPS)
                k.op('act', 'activation', R=[rs_.res], W=[rs_.res], out=rs_.ap, in_=rs_.ap, func=AF.Exp, scale=-0.5)
                k.op('dve', 'tensor_tensor', R=[hm_.res, rs_.res], W=[mo_.res], out=mo_.ap, in0=hm_.ap, in1=bc_last(rs_.ap, HD_M), op=ALU.mult)
                with nc.allow_non_contiguous_dma(reason="per-head mixed slices"):
                    k.dma('pool', mixed_s.ap[q * 512:(q + 1) * 512, h * HD_M:(h + 1) * HD_M].rearrange("(a p) d -> p a d", p=P), mo_.ap,
                          reads=[mo_.res], writes=[mixed_s.res])
                ch += 1
        cx.pop()

    if 'C' in phases:
        cx.push()
        SSC = float(HD_S) ** -0.5
        tri = cx.sb("tri", [P, P], F32)
        k.op('pool', 'memset', W=[tri.res], ap=tri.ap, constant=1.0)
        k.op('pool', 'affine_select', R=[tri.res], W=[tri.res], out=tri.ap, in_=tri.ap, pattern=[[-1, P]],
             compare_op=ALU.is_ge, fill=0.0, base=0, channel_multiplier=1)
        onesf = cx.sb("onesf", [P, P], F32)
        k.op('pool', 'memset', W=[onesf.res], ap=onesf.ap, constant=1.0)
        sq_ = cx.sb("sq_", [HD_S, S], BF16)
        sk_ = cx.sb("sk_", [HD_S, S], BF16)
        skn = cx.sb("skn", [HD_S, S], BF16)
        sv_ = cx.sb("sv_", [P, NT, HD_S], BF16)
        e1 = [cx.sb(f"e1{i}", [P, 512], F32) for i in range(2)]
        sA = [cx.sb(f"sA{i}", [P, 512], BF16) for i in range(2)]
        raccs = [cx.sb(f"racc{i}", [P, 512], F32) for i in range(2)]
        os_ = [cx.sb(f"os{i}", [P, 4, HD_S], F32) for i in range(2)]
        osq = cx.sb("osq", [P, HD_S], F32)
        sss = [cx.sb(f"sss{i}", [P, 4], F32) for i in range(2)]
        srs = [cx.sb(f"srs{i}", [P, 4], F32) for i in range(2)]
        smo = [cx.sb(f"smo{i}", [P, 4, HD_S], BF16) for i in range(2)]
        it = 0
        ch = 0
        for h in range(NH_S):
            k.dma('sp', sq_.ap, sbq_s.ap[h * HD_S:(h + 1) * HD_S, :], reads=[sbq_s.res], writes=[sq_.res])
            k.dma('sp', sk_.ap, sbk_s.ap[h * HD_S:(h + 1) * HD_S, :], reads=[sbk_s.res], writes=[sk_.res])
            with nc.allow_non_contiguous_dma(reason="per-head v slices, 128B runs"):
                k.dma('sp', sv_.ap, vs_s.ap[:, h * HD_S:(h + 1) * HD_S].rearrange("(n p) d -> p n d", p=P),
                      reads=[vs_s.res], writes=[sv_.res])
            k.op('dve', 'tensor_scalar', R=[sk_.res], W=[skn.res], out=skn.ap, in0=sk_.ap, scalar1=-SSC, scalar2=None, op0=ALU.mult)
            for q in range(NG):
                accs = [banks[4 + i] for i in range(4)]
                for rb in raccs:
                    k.op('pool', 'memset', W=[rb.res], ap=rb.ap, constant=0.0)
                steps = []
                for j in range(4 * q + 3, -1, -1):
                    c0 = max(P * j - 512 * q, 0)
                    steps.append(dict(j=j, c0=c0, t0=512 * q + c0, n=512 - c0, diag=(j >= 4 * q), first=(j == 4 * q + 3),
                                      zb=banks[it % 2], cp=banks[2 + it % 2], e_=e1[it % 2], a_=sA[it % 2],
                                      rc=raccs[len(steps) % 2], rn=raccs[(len(steps) + 1) % 2]))
                    it += 1

                def stage1(st):
                    j, t0, n, zb, e_ = st['j'], st['t0'], st['n'], st['zb'], st['e_']
                    k.op('pe', 'matmul', R=[sk_.res, sq_.res], W=[zb.res], out=zb.ap[:, 0:n], lhsT=sk_.ap[:, j * P:(j + 1) * P],
                         rhs=sq_.ap[:, t0:t0 + n], start=True, stop=True)
                    k.op('act', 'activation', R=[zb.res], W=[e_.res], out=e_.ap[:, 0:n], in_=zb.ap[:, 0:n], func=AF.Exp, scale=SSC)
                    k.op('act', 'activation', R=[e_.res], W=[e_.res], out=e_.ap[:, 0:n], in_=e_.ap[:, 0:n], func=AF.Ln, bias=1.0)
                    if st['diag']:
                        k.op('pool', 'affine_select', R=[e_.res], W=[e_.res], out=e_.ap[:, 0:P], in_=e_.ap[:, 0:P], pattern=[[1, P]],
                             compare_op=ALU.is_ge, fill=0.0, base=-1, channel_multiplier=-1)

                def stage2(st):
                    j, c0, t0, n, cp, e_, a_ = st['j'], st['c0'], st['t0'], st['n'], st['cp'], st['e_'], st['a_']
                    k.op('pe', 'matmul', R=[tri.res, e_.res], W=[cp.res], out=cp.ap[:, 0:n], lhsT=tri.ap, rhs=e_.ap[:, 0:n],
                         start=True, stop=False)
                    rc, rn = st['rc'], st['rn']
                    if not st['first']:
                        k.op('pe', 'matmul', R=[onesf.res, rc.res], W=[cp.res], out=cp.ap[:, 0:n], lhsT=onesf.ap, rhs=rc.ap[:, c0:512],
                             start=False, stop=False)
                    k.op('pe', 'matmul', R=[skn.res, sq_.res], W=[cp.res], out=cp.ap[:, 0:n], lhsT=skn.ap[:, j * P:(j + 1) * P],
                         rhs=sq_.ap[:, t0:t0 + n], start=False, stop=True)
                    if j > 0:
                        k.op('pool', 'tensor_tensor', R=[rc.res, e_.res], W=[rn.res], out=rn.ap[:, c0:512], in0=rc.ap[:, c0:512],
                             in1=e_.ap[:, 0:n], op=ALU.add)
                    k.op('act', 'activation', R=[cp.res], W=[a_.res], out=a_.ap[:, 0:n], in_=cp.ap[:, 0:n], func=AF.Exp, scale=-1.0)
                    if st['diag']:
                        k.op('pool', 'affine_select', R=[a_.res], W=[a_.res], out=a_.ap[:, 0:P], in_=a_.ap[:, 0:P], pattern=[[1, P]],
                             compare_op=ALU.is_ge, fill=0.0, base=-1, channel_multiplier=-1)

                def stage3(st):
                    j, t0, a_ = st['j'], st['t0'], st['a_']
                    for i in range(max(j, 4 * q), 4 * q + 4):
                        o = i * P - t0
                        acc = accs[i - 4 * q]
                        k.op('pe', 'matmul', R=[a_.res, sv_.res], W=[acc.res], out=acc.ap[:, 0:HD_S], lhsT=a_.ap[:, o:o + P],
                             rhs=sv_.ap[:, j, :], start=(j == i), stop=(j == 0))

                ns = len(steps)
                for x_ in range(ns + 2):
                    if x_ < ns:
                        stage1(steps[x_])
                    if 0 <= x_ - 1 < ns:
                        stage2(steps[x_ - 1])
                    if 0 <= x_ - 2 < ns:
                        stage3(steps[x_ - 2])
                o_, ss_, rs_, mo_ = os_[ch % 2], sss[ch % 2], srs[ch % 2], smo[ch % 2]
                for i in range(4):
                    k.op('act', 'copy', R=[accs[i].res], W=[o_.res], out=o_.ap[:, i, :], in_=accs[i].ap[:, 0:HD_S])
                for i in range(4):
                    k.op('dve', 'scalar_tensor_tensor', R=[o_.res], W=[osq.res, ss_.res], out=osq.ap, in0=o_.ap[:, i, :], scalar=1.0, in1=o_.ap[:, i, :],
                         op0=ALU.mult, op1=ALU.mult, accum_out=ss_.ap[:, i:i + 1])
                k.op('act', 'activation', R=[ss_.res], W=[rs_.res], out=rs_.ap, in_=ss_.ap, func=AF.Ln, scale=1.0 / HD_S, bias=EPS)
                k.op('act', 'activation', R=[rs_.res], W=[rs_.res], out=rs_.ap, in_=rs_.ap, func=AF.Exp, scale=-0.5)
                k.op('dve', 'tensor_tensor', R=[o_.res, rs_.res], W=[mo_.res], out=mo_.ap, in0=o_.ap, in1=bc_last(rs_.ap, HD_S), op=ALU.mult)
                with nc.allow_non_contiguous_dma(reason="per-head mixed slices"):
                    k.dma('pool', mixed_s.ap[q * 512:(q + 1) * 512, 512 + h * HD_S:512 + (h + 1) * HD_S].rearrange("(a p) d -> p a d", p=P),
                          mo_.ap, reads=[mo_.res], writes=[mixed_s.res])
                ch += 1
        cx.pop()

    if 'D' in phases:
        cx.push()
        XSC = float(XHD) ** -0.5
        w_out = cx.sb("w_out", [P, DC, D], BF16)
        wq = cx.sb("wq", [P, DC, D], BF16)
        wo = cx.sb("wo", [P, DC, D], BF16)
        kmT = cx.sb("kmT", [P, DC, MEM], BF16)
        vmem = cx.sb("vmem", [P, 2, D], BF16)
        g_mixed = make_gbc(4, "g_mixed")
        g_xat = make_gbc(1, "g_xat")
        junk1 = cx.sb("junk1", [P, D], F32)

        def rows(wd, c, a, n):
            return wd.ap[c * P:(c + 1) * P, a:a + n]

        def norm_T(src, ss, rs, xn_b, gbc, dstT, ncol_off=0, tbank=None):
            k.op('act', 'activation', R=[src.res], W=[junk1.res, ss.res], out=junk1.ap, in_=src.ap, func=AF.Square, accum_out=ss.ap[:, 0:1])
            rms_stats(ss, rs, 1)
            k.op('dve', 'tensor_scalar', R=[src.res, rs.res], W=[xn_b.res], out=xn_b.ap, in0=src.ap, scalar1=rs.ap[:, 0:1], scalar2=None, op0=ALU.mult)
            transpose_T(xn_b, gbc, dstT, ncol_off, tbank)

        trot = [0]

        def transpose_T(src_b, gbc, dstT, ncol_off=0, tbank=None):
            pb = tbank if tbank is not None else banks[trot[0] % 2]
            trot[0] += 1
            pview = pb.ap.bitcast(BF16).rearrange("p (c t) -> p c t", c=DC)
            for c in range(DC):
                k.op('pe', 'transpose', R=[src_b.res, ident_b.res], W=[pb.res], out=pview[:, c, :], in_=src_b.ap[:, c * P:(c + 1) * P],
                     identity=ident_b.ap)
            k.op('dve', 'tensor_tensor', R=[pb.res, gbc.res], W=[dstT.res], out=dstT.ap[:, :, ncol_off:ncol_off + P], in0=pview, in1=gbc.ap,
                 op=ALU.mult)

        cx.push()
        wkv = cx.sb("wkv", [P, DC, 2 * D], BF16)
        load_cast([wkv.ap[:, c, :] for c in range(DC)], [rows(wkv_d, c, 0, 2 * D) for c in range(DC)], wkv.res)
        g_mem = make_gbc(2, "g_mem")
        memt = cx.sb("memt", [P, D], F32)
        memn = cx.sb("memn", [P, D], BF16)
        memT = cx.sb("memT", [P, DC, MEM], BF16)
        mss = cx.sb("mss", [P, 1], F32)
        mrs = cx.sb("mrs", [P, 1], F32)
        for mc in range(2):
            k.dma('sp', memt.ap, mem_d.ap[mc * P:(mc + 1) * P, :], writes=[memt.res])
            norm_T(memt, mss, mrs, memn, g_mem, memT, ncol_off=mc * P)
        for c in range(DC):
            b = banks[2 + c % 2]
            for dc in range(DC):
                k.op('pe', 'matmul', R=[wkv.res, memT.res], W=[b.res], out=b.ap[:, 0:MEM], lhsT=wkv.ap[:, dc, c * P:(c + 1) * P], rhs=memT.ap[:, dc, :],
                     start=(dc == 0), stop=(dc == DC - 1))
            k.op('act', 'copy', R=[b.res], W=[kmT.res], out=kmT.ap[:, c, :], in_=b.ap[:, 0:MEM])
        for mc in range(2):
            for hf in range(2):
                b = banks[4 + (mc * 2 + hf) % 2]
                for dc in range(DC):
                    k.op('pe', 'matmul', R=[wkv.res, memT.res], W=[b.res], out=b.ap, lhsT=memT.ap[:, dc, mc * P:(mc + 1) * P],
                         rhs=wkv.ap[:, dc, D + hf * 512:D + (hf + 1) * 512], start=(dc == 0), stop=(dc == DC - 1))
                k.op('dve', 'tensor_copy', R=[b.res], W=[vmem.res], out=vmem.ap[:, mc, hf * 512:(hf + 1) * 512], in_=b.ap)
        cx.pop()

        load_cast([w_out.ap[:, c, :] for c in range(DC)], [rows(w_out_d, c, 0, D) for c in range(DC)], w_out.res)
        load_cast([wq.ap[:, c, :] for c in range(DC)], [rows(wq_d, c, 0, D) for c in range(DC)], wq.res)
        load_cast([wo.ap[:, c, :] for c in range(DC)], [rows(wo_d, c, 0, D) for c in range(DC)], wo.res)

        NB = 2
        xt_ = [cx.sb(f"xt{i}", [P, D], F32) for i in range(NB)]
        mx_ = [cx.sb(f"mx{i}", [P, D], BF16) for i in range(NB)]
        mT_ = [cx.sb(f"mT{i}", [P, DC, P], BF16) for i in range(NB)]
        x1_ = [cx.sb(f"x1{i}", [P, D], F32) for i in range(NB)]
        x2_ = [cx.sb(f"x2{i}", [P, D], F32) for i in range(NB)]
        xnb = [cx.sb(f"xnb{i}", [P, D], BF16) for i in range(NB)]
        h2T = [cx.sb(f"h2T{i}", [P, DC, P], BF16) for i in range(NB)]
        qT_ = [cx.sb(f"qT{i}", [P, DC, P], BF16) for i in range(NB)]
        pex = [cx.sb(f"pex{i}", [P, NXH, MEM], F32) for i in range(NB)]
        pn_ = [cx.sb(f"pn{i}", [P, NXH, MEM], BF16) for i in range(NB)]
        pT_ = [cx.sb(f"pT{i}", [P, DC, P], BF16) for i in range(NB)]
        oT_ = [cx.sb(f"oT{i}", [P, DC, P], BF16) for i in range(NB)]
        st1 = [cx.sb(f"st1{i}", [P, 16], F32) for i in range(NB)]
        mmr = [0]
        PAIRS = [(2, 3), (4, 5), (6, 7)]

        def pair():
            p_ = PAIRS[mmr[0] % 3]
            mmr[0] += 1
            return banks[p_[0]], banks[p_[1]]

        for tt in range(NT):
            r = tt % NB
            xt, mxt, mT, x1, x2, xn_b, hT2, qT, pe_, pn, pT, oT, st = (xt_[r], mx_[r], mT_[r], x1_[r], x2_[r], xnb[r], h2T[r], qT_[r], pex[r],
                                                                      pn_[r], pT_[r], oT_[r], st1[r])
            ts = slice(tt * P, (tt + 1) * P)
            k.dma('sp', xt.ap, x_d.ap[ts, :], writes=[xt.res])
            k.dma('sp', mxt.ap, mixed_s.ap[ts, :], reads=[mixed_s.res], writes=[mxt.res])
            transpose_T(mxt, g_mixed, mT)
            ba, bb = pair()
            for hf, b in ((0, ba), (1, bb)):
                for dc in range(DC):
                    k.op('pe', 'matmul', R=[mT.res, w_out.res], W=[b.res], out=b.ap, lhsT=mT.ap[:, dc, :], rhs=w_out.ap[:, dc, hf * 512:(hf + 1) * 512],
                         start=(dc == 0), stop=(dc == DC - 1))
                k.op('dve', 'tensor_tensor', R=[b.res, xt.res], W=[x1.res], out=x1.ap[:, hf * 512:(hf + 1) * 512], in0=b.ap,
                     in1=xt.ap[:, hf * 512:(hf + 1) * 512], op=ALU.add)
            ssb = Buf(st.ap[:, 0:1], st.res)
            rsb = Buf(st.ap[:, 1:2], st.res)
            norm_T(x1, ssb, rsb, xn_b, g_xat, hT2)
            ba, bb = pair()
            for c in range(DC):
                b = ba if c < 4 else bb
                for dc in range(DC):
                    k.op('pe', 'matmul', R=[wq.res, hT2.res], W=[b.res], out=b.ap[:, (c % 4) * P:(c % 4 + 1) * P], lhsT=wq.ap[:, dc, c * P:(c + 1) * P],
                         rhs=hT2.ap[:, dc, :], start=(dc == 0), stop=(dc == DC - 1))
            for hf, b in ((0, ba), (1, bb)):
                k.op('act', 'copy', R=[b.res], W=[qT.res], out=qT.ap[:, hf * 4:(hf + 1) * 4, :], in_=b.ap.rearrange("p (c t) -> p c t", c=4))
            ba, bb = pair()
            for hh in range(NXH):
                b = ba if hh < 2 else bb
                for c2 in range(2):
                    k.op('pe', 'matmul', R=[qT.res, kmT.res], W=[b.res], out=b.ap[:, (hh % 2) * MEM:(hh % 2 + 1) * MEM], lhsT=qT.ap[:, 2 * hh + c2, :],
                         rhs=kmT.ap[:, 2 * hh + c2, :], start=(c2 == 0), stop=(c2 == 1))
            for hf, b in ((0, ba), (1, bb)):
                k.op('dve', 'tensor_reduce', R=[b.res], W=[st.res], out=st.ap[:, 4 + 2 * hf:6 + 2 * hf], in_=b.ap.rearrange("p (h m) -> p h m", h=2),
                     axis=AX.X, op=ALU.max)
            k.op('dve', 'tensor_scalar', R=[st.res], W=[st.res], out=st.ap[:, 4:8], in0=st.ap[:, 4:8], scalar1=-XSC, scalar2=None, op0=ALU.mult)
            for hh in range(NXH):
                b = ba if hh < 2 else bb
                k.op('act', 'activation', R=[b.res, st.res], W=[pe_.res, st.res], out=pe_.ap[:, hh, :], in_=b.ap[:, (hh % 2) * MEM:(hh % 2 + 1) * MEM],
                     func=AF.Exp, scale=XSC, bias=st.ap[:, 4 + hh:5 + hh], accum_out=st.ap[:, 8 + hh:9 + hh])
            k.op('dve', 'reciprocal', R=[st.res], W=[st.res], out=st.ap[:, 12:16], in_=st.ap[:, 8:12])
            k.op('dve', 'tensor_tensor', R=[pe_.res, st.res], W=[pn.res], out=pn.ap, in0=pe_.ap, in1=bc_last(st.ap[:, 12:16], MEM), op=ALU.mult)
            pb = banks[trot[0] % 2]
            trot[0] += 1
            pview = pb.ap.bitcast(BF16).rearrange("p (c t) -> p c t", c=DC)
            for hh in range(NXH):
                for mc in range(2):
                    k.op('pe', 'transpose', R=[pn.res, ident_b.res], W=[pb.res], out=pview[:, hh * 2 + mc, :], in_=pn.ap[:, hh, mc * P:(mc + 1) * P],
                         identity=ident_b.ap)
            k.op('act', 'copy', R=[pb.res], W=[pT.res], out=pT.ap, in_=pview)
            ba, bb = pair()
            for c in range(DC):
                b = ba if c < 4 else bb
                hh = c // 2
                for mc in range(2):
                    k.op('pe', 'matmul', R=[vmem.res, pT.res], W=[b.res], out=b.ap[:, (c % 4) * P:(c % 4 + 1) * P], lhsT=vmem.ap[:, mc, c * P:(c + 1) * P],
                         rhs=pT.ap[:, hh * 2 + mc, :], start=(mc == 0), stop=(mc == 1))
            for hf, b in ((0, ba), (1, bb)):
                k.op('act', 'copy', R=[b.res], W=[oT.res], out=oT.ap[:, hf * 4:(hf + 1) * 4, :], in_=b.ap.rearrange("p (c t) -> p c t", c=4))
            ba, bb = pair()
            for hf, b in ((0, ba), (1, bb)):
                for c in range(DC):
                    k.op('pe', 'matmul', R=[oT.res, wo.res], W=[b.res], out=b.ap, lhsT=oT.ap[:, c, :], rhs=wo.ap[:, c, hf * 512:(hf + 1) * 512],
                         start=(c == 0), stop=(c == DC - 1))
                k.op('dve', 'tensor_tensor', R=[b.res, x1.res], W=[x2.res], out=x2.ap[:, hf * 512:(hf + 1) * 512], in0=b.ap,
                     in1=x1.ap[:, hf * 512:(hf + 1) * 512], op=ALU.add)
            k.dma('pool', x2_s.ap[ts, :], x2.ap, reads=[x2.res], writes=[x2_s.res])
        cx.pop()

    if 'D' in phases:
        cx.push()
        pwq = cx.sb("pwq", [P, DC, 2 * D], BF16)
        load_cast([pwq.ap[:, c, :] for c in range(DC)], [pwq_d.ap[c * P:(c + 1) * P, :] for c in range(DC)], pwq.res)
        skT = cx.sb("skT", [P, 16, P], BF16)
        load_cast([skT.ap], [skT_d.ap], skT.res)
        g_ffn = make_gbc(3, "g_ffn")
        ffn_bc = cx.sb("ffn_bc", [P, D], F32)
        fin_bc = cx.sb("fin_bc", [P, D], F32)
        gt = grow_d.ap.tensor
        k.dma('sp', ffn_bc.ap, bass.AP(gt, 0, [[0, P], [1, D]]), writes=[ffn_bc.res])
        k.dma('sp', fin_bc.ap, bass.AP(gt, D, [[0, P], [1, D]]), writes=[fin_bc.res])
        iota_i = cx.sb("iota_i", [P, 16], I32)
        k.op('pool', 'iota', W=[iota_i.res], out=iota_i.ap, pattern=[[1, 16]], base=0, channel_multiplier=0)
        iota16 = cx.sb("iota16", [P, 16], F32)
        k.op('dve', 'tensor_copy', R=[iota_i.res], W=[iota16.res], out=iota16.ap, in_=iota_i.ap)
        thr16 = cx.sb("thr16", [P, 16], F32)
        k.op('dve', 'tensor_scalar', R=[iota16.res], W=[thr16.res], out=thr16.ap, in0=iota16.ap, scalar1=16.0, scalar2=15.5,
             op0=ALU.mult, op1=ALU.add)
        junk2 = cx.sb("junk2", [P, D], F32)

        NB = 1
        x2t = [cx.sb(f"x2t{i}", [P, D], F32) for i in range(2)]
        h3 = [cx.sb(f"h3{i}", [P, D], F32) for i in range(2)]
        xn3 = [cx.sb(f"xn3{i}", [P, D], BF16) for i in range(NB)]
        h3T = [cx.sb(f"h3T{i}", [P, DC, P], BF16) for i in range(NB)]
        pqT = [cx.sb(f"pqT{i}", [P, 16, P], BF16) for i in range(NB)]
        ssb_ = [cx.sb(f"ssb{i}", [P, 16, P], F32) for i in range(NB)]
        s2b_ = cx.sb("s2b", [P, 16, P], F32)
        cand2 = Buf(s2b_.ap.rearrange("p a b -> p (a b)").rearrange("p (h c) -> p h c", h=PH), s2b_.res)
        mxs = [cx.sb(f"mxs{i}", [P, 16, 16], F32) for i in range(NB)]
        ixs = [cx.sb(f"ixs{i}", [P, 16, 16], U32) for i in range(NB)]
        ixf = [cx.sb(f"ixf{i}", [P, 16, 16], F32) for i in range(NB)]
        cand = [cx.sb(f"cand{i}", [P, PH, 256], F32) for i in range(NB)]
        best = [cx.sb(f"best{i}", [P, PH, 16], F32) for i in range(NB)]
        pos = [cx.sb(f"pos{i}", [P, PH, 16], U32) for i in range(NB)]
        posf = [cx.sb(f"posf{i}", [P, PH, 16], F32) for i in range(NB)]
        paf = [cx.sb(f"paf{i}", [P, PH, 16], F32) for i in range(NB)]
        pbf = [cx.sb(f"pbf{i}", [P, PH, 16], F32) for i in range(NB)]
        oh = cx.sb("oh", [P, PH, 16, 16], F32)
        oh2 = cx.sb("oh2", [P, PH, 16, 16], F32)
        i1s = [cx.sb(f"i1s{i}", [P, PH, 16], F32) for i in range(NB)]
        i2s = [cx.sb(f"i2s{i}", [P, PH, 16], F32) for i in range(NB)]
        eidf = [cx.sb(f"eidf{i}", [P, PH * 16], F32) for i in range(NB)]
        eid = [cx.sb(f"eid{i}", [P, PH * 16], U32) for i in range(2)]
        gate = [cx.sb(f"gate{i}", [P, PH, 16], F32) for i in range(2)]
        gst_ = [cx.sb(f"gstat{i}", [P, 16], F32) for i in range(NB)]
        yacc = [cx.sb(f"yacc{i}", [P, D], F32) for i in range(2)]
        st3 = [cx.sb(f"st3{i}", [P, 4], F32) for i in range(2)]
        NSL = 12
        GK = 8
        uvg = [cx.sb(f"uvg{i}", [P, 2 * D], BF16) for i in range(NSL)]
        sdg = [cx.sb(f"sdg{i}", [P, GK], F32) for i in range(2)]
        avg = [cx.sb(f"avg{i}", [P, GK], F32) for i in range(2)]
        trot = [0]
        gsl = [0, 0]

        def front_part(tt):
            r = 0
            ts = slice(tt * P, (tt + 1) * P)
            x2, h3_, xn_b, hT3, pq, ssb, mx, ix, ixf_ = x2t[tt % 2], h3[tt % 2], xn3[r], h3T[r], pqT[r], ssb_[r], mxs[r], ixs[r], ixf[r]
            cd, bst, ps_, psf, pa, pb_ = cand[r], best[r], pos[r], posf[r], paf[r], pbf[r]
            st = st3[tt % 2]
            k.dma('sp', x2.ap, x2_s.ap[ts, :], reads=[x2_s.res], writes=[x2.res])
            k.op('act', 'activation', R=[x2.res], W=[junk2.res, st.res], out=junk2.ap, in_=x2.ap, func=AF.Square, accum_out=st.ap[:, 0:1])
            k.op('act', 'activation', R=[st.res], W=[st.res], out=st.ap[:, 1:2], in_=st.ap[:, 0:1], func=AF.Ln, scale=1.0 / D, bias=EPS)
            k.op('act', 'activation', R=[st.res], W=[st.res], out=st.ap[:, 1:2], in_=st.ap[:, 1:2], func=AF.Exp, scale=-0.5)
            k.op('dve', 'tensor_scalar', R=[x2.res, st.res], W=[xn_b.res], out=xn_b.ap, in0=x2.ap, scalar1=st.ap[:, 1:2], scalar2=None, op0=ALU.mult)
            k.op('dve', 'scalar_tensor_tensor', R=[x2.res, st.res, ffn_bc.res], W=[h3_.res], out=h3_.ap, in0=x2.ap, scalar=st.ap[:, 1:2],
                 in1=ffn_bc.ap, op0=ALU.mult, op1=ALU.mult)
            pb = banks[trot[0] % 2]
            trot[0] += 1
            pview = pb.ap.bitcast(BF16).rearrange("p (c t) -> p c t", c=DC)
            for c in range(DC):
                k.op('pe', 'transpose', R=[xn_b.res, ident_b.res], W=[pb.res], out=pview[:, c, :], in_=xn_b.ap[:, c * P:(c + 1) * P], identity=ident_b.ap)
            k.op('dve', 'tensor_tensor', R=[pb.res, g_ffn.res], W=[hT3.res], out=hT3.ap, in0=pview, in1=g_ffn.ap, op=ALU.mult)
            for bq in range(4):
                b = banks[2 + bq]
                for cc in range(4):
                    c = bq * 4 + cc
                    for dc in range(DC):
                        k.op('pe', 'matmul', R=[pwq.res, hT3.res], W=[b.res], out=b.ap[:, cc * P:(cc + 1) * P], lhsT=pwq.ap[:, dc, c * P:(c + 1) * P],
                             rhs=hT3.ap[:, dc, :], start=(dc == 0), stop=(dc == DC - 1))
                k.op('act', 'copy', R=[b.res], W=[pq.res], out=pq.ap[:, bq * 4:(bq + 1) * 4, :], in_=b.ap.rearrange("p (c t) -> p c t", c=4))
            for bq in range(4):
                b = banks[2 + (bq + 2) % 4 + 0] if False else banks[6 + bq % 2] if False else banks[2 + bq]
                for cc in range(4):
                    hp = bq * 4 + cc
                    k.op('pe', 'matmul', R=[pq.res, skT.res], W=[b.res], out=b.ap[:, cc * P:(cc + 1) * P], lhsT=pq.ap[:, hp, :], rhs=skT.ap[:, hp, :],
                         start=True, stop=True)
                k.op('act', 'copy', R=[b.res], W=[ssb.res], out=ssb.ap[:, bq * 4:(bq + 1) * 4, :], in_=b.ap.rearrange("p (c t) -> p c t", c=4))
            for hp in range(16):
                k.op('dve', 'max', R=[ssb.res], W=[mx.res], out=mx.ap[:, hp, 0:8], in_=ssb.ap[:, hp, :])
                k.op('dve', 'match_replace', R=[mx.res, ssb.res], W=[s2b_.res], out=s2b_.ap[:, hp, :], in_to_replace=mx.ap[:, hp, 0:8],
                     in_values=ssb.ap[:, hp, :], imm_value=NEG)
                k.op('dve', 'max', R=[s2b_.res], W=[mx.res], out=mx.ap[:, hp, 8:16], in_=s2b_.ap[:, hp, :])
                k.op('dve', 'max_index', R=[mx.res, ssb.res], W=[ix.res], out=ix.ap[:, hp, 0:8], in_max=mx.ap[:, hp, 0:8], in_values=ssb.ap[:, hp, :])
                k.op('dve', 'max_index', R=[mx.res, ssb.res], W=[ix.res], out=ix.ap[:, hp, 8:16], in_max=mx.ap[:, hp, 8:16], in_values=ssb.ap[:, hp, :])
            k.op('dve', 'tensor_copy', R=[ix.res], W=[ixf_.res], out=ixf_.ap, in_=ix.ap)
            mx4 = mx.ap.rearrange("p (h two) k -> p h two k", two=2)
            cd4 = cd.ap.rearrange("p h (a b) -> p h a b", a=16)
            k.op('dve', 'tensor_tensor', R=[mx.res], W=[cd.res], out=cd4, in0=bc_last(mx4[:, :, 0, :], 16), in1=bc_mid(mx4[:, :, 1, :], 2, 16), op=ALU.add)
            for hh in range(PH):
                k.op('dve', 'max', R=[cd.res], W=[bst.res], out=bst.ap[:, hh, 0:8], in_=cd.ap[:, hh, :])
                k.op('dve', 'match_replace', R=[bst.res, cd.res], W=[cand2.res], out=cand2.ap[:, hh, :], in_to_replace=bst.ap[:, hh, 0:8],
                     in_values=cd.ap[:, hh, :], imm_value=NEG)
                k.op('dve', 'max', R=[cand2.res], W=[bst.res], out=bst.ap[:, hh, 8:16], in_=cand2.ap[:, hh, :])
                k.op('dve', 'max_index', R=[bst.res, cd.res], W=[ps_.res], out=ps_.ap[:, hh, 0:8], in_max=bst.ap[:, hh, 0:8], in_values=cd.ap[:, hh, :])
                k.op('dve', 'max_index', R=[bst.res, cd.res], W=[ps_.res], out=ps_.ap[:, hh, 8:16], in_max=bst.ap[:, hh, 8:16], in_values=cd.ap[:, hh, :])
            gs_, gt_ = gst_[r], gate[tt % 2]
            k.op('dve', 'tensor_tensor', R=[bst.res], W=[gt_.res], out=gt_.ap, in0=bst.ap, in1=bc_last(bst.ap[:, :, 0], 16), op=ALU.subtract)
            k.op('act', 'activation', R=[gt_.res], W=[gt_.res], out=gt_.ap, in_=gt_.ap, func=AF.Exp)
            k.op('dve', 'tensor_reduce', R=[gt_.res], W=[gs_.res], out=gs_.ap[:, 0:8], in_=gt_.ap, axis=AX.X, op=ALU.add)
            k.op('dve', 'reciprocal', R=[gs_.res], W=[gs_.res], out=gs_.ap[:, 8:16], in_=gs_.ap[:, 0:8])
            k.op('dve', 'tensor_tensor', R=[gt_.res, gs_.res], W=[gt_.res], out=gt_.ap, in0=gt_.ap, in1=bc_last(gs_.ap[:, 8:16], 16), op=ALU.mult)
            k.op('dve', 'tensor_copy', R=[ps_.res], W=[psf.res], out=psf.ap, in_=ps_.ap)
            thr_bc = bc_mid(bc_mid(thr16.ap, 1, 16), 1, PH)
            iota_bc = bc_mid(bc_mid(iota16.ap, 1, 16), 1, PH)
            k.op('dve', 'tensor_tensor', R=[psf.res, thr16.res], W=[oh.res], out=oh.ap, in0=bc_last(psf.ap, 16), in1=thr_bc, op=ALU.is_ge)
            k.op('dve', 'tensor_reduce', R=[oh.res], W=[pa.res], out=pa.ap, in_=oh.ap, axis=AX.X, op=ALU.add)
            k.op('dve', 'scalar_tensor_tensor', R=[pa.res, psf.res], W=[pb_.res], out=pb_.ap, in0=pa.ap, scalar=-16.0, in1=psf.ap, op0=ALU.mult, op1=ALU.add)
            ixf4 = ixf_.ap.rearrange("p (h two) k -> p h two k", two=2)
            for which, pidx, dsti in ((0, pa, i1s[r]), (1, pb_, i2s[r])):
                k.op('dve', 'tensor_tensor', R=[pidx.res, iota16.res], W=[oh.res], out=oh.ap, in0=bc_last(pidx.ap, 16), in1=iota_bc, op=ALU.is_equal)
                k.op('dve', 'tensor_tensor', R=[oh.res, ixf_.res], W=[oh2.res], out=oh2.ap, in0=oh.ap, in1=bc_mid(ixf4[:, :, which, :], 2, 16), op=ALU.mult)
                k.op('dve', 'tensor_reduce', R=[oh2.res], W=[dsti.res], out=dsti.ap, in_=oh2.ap, axis=AX.X, op=ALU.add)
            ef, ei = eidf[r], eid[tt % 2]
            k.op('dve', 'scalar_tensor_tensor', R=[i1s[r].res, i2s[r].res], W=[ef.res], out=ef.ap, in0=i1s[r].ap.rearrange("p h k -> p (h k)"),
                 scalar=float(PK), in1=i2s[r].ap.rearrange("p h k -> p (h k)"), op0=ALU.mult, op1=ALU.add)
            k.op('dve', 'tensor_copy', R=[ef.res], W=[ei.res], out=ei.ap, in_=ef.ap)

        def expert_group(tt, kg):
            r = 0
            h3_, ei, gt_ = h3[tt % 2], eid[tt % 2], gate[tt % 2]
            ya = yacc[tt % 2]
            gflat = gt_.ap.rearrange("p h k -> p (h k)")
            sd, av = sdg[kg % 2], avg[kg % 2]
            slots = []
            for kk in range(kg * GK, (kg + 1) * GK):
                u_ = uvg[gsl[0] % NSL]
                gsl[0] += 1
                slots.append(u_)
                k.dma('pool', u_.ap, uv_s.ap, reads=[ei.res, uv_s.res], writes=[u_.res],
                      indirect=bass.IndirectOffsetOnAxis(ap=ei.ap[:, kk:kk + 1], axis=0))
                k.op('dve', 'scalar_tensor_tensor', R=[u_.res, h3_.res], W=[junk2.res, sd.res], out=junk2.ap, in0=u_.ap[:, 0:D], scalar=1.0,
                     in1=h3_.ap, op0=ALU.mult, op1=ALU.mult, accum_out=sd.ap[:, kk - kg * GK:kk - kg * GK + 1])
            k.op('act', 'activation', R=[sd.res], W=[av.res], out=av.ap, in_=sd.ap, func=AF.Gelu)
            k.op('dve', 'tensor_tensor', R=[av.res, gt_.res], W=[av.res], out=av.ap, in0=av.ap, in1=gflat[:, kg * GK:(kg + 1) * GK], op=ALU.mult)
            for i_, u_ in enumerate(slots):
                if kg == 0 and i_ == 0:
                    k.op('dve', 'tensor_scalar', R=[u_.res, av.res], W=[ya.res], out=ya.ap, in0=u_.ap[:, D:2 * D], scalar1=av.ap[:, 0:1], scalar2=None,
                         op0=ALU.mult)
                else:
                    k.op('dve', 'scalar_tensor_tensor', R=[u_.res, av.res, ya.res], W=[ya.res], out=ya.ap, in0=u_.ap[:, D:2 * D],
                         scalar=av.ap[:, i_:i_ + 1], in1=ya.ap, op0=ALU.mult, op1=ALU.add)

        def tail_part(tt):
            r = 0
            ts = slice(tt * P, (tt + 1) * P)
            x2, ya, st = x2t[tt % 2], yacc[tt % 2], st3[tt % 2]
            k.op('dve', 'tensor_tensor', R=[x2.res, ya.res], W=[ya.res], out=ya.ap, in0=x2.ap, in1=ya.ap, op=ALU.add)
            k.op('act', 'activation', R=[ya.res], W=[junk2.res, st.res], out=junk2.ap, in_=ya.ap, func=AF.Square, accum_out=st.ap[:, 2:3])
            k.op('act', 'activation', R=[st.res], W=[st.res], out=st.ap[:, 3:4], in_=st.ap[:, 2:3], func=AF.Ln, scale=1.0 / D, bias=EPS)
            k.op('act', 'activation', R=[st.res], W=[st.res], out=st.ap[:, 3:4], in_=st.ap[:, 3:4], func=AF.Exp, scale=-0.5)
            k.op('dve', 'scalar_tensor_tensor', R=[ya.res, st.res, fin_bc.res], W=[ya.res], out=ya.ap, in0=ya.ap, scalar=st.ap[:, 3:4], in1=fin_bc.ap,
                 op0=ALU.mult, op1=ALU.mult)
            o_ = ya
            k.dma('sp', out_d.ap[ts, :], o_.ap, reads=[o_.res], writes=[out_d.res])

        NGR = PH * 16 // GK
        k.play(k.record(lambda: front_part(0)))
        for tt in range(NT):
            nxt = k.record(lambda: front_part(tt + 1)) if tt + 1 < NT else []
            per = -(-len(nxt) // NGR) if nxt else 0
            for kg in range(NGR):
                k.play(k.record(lambda: expert_group(tt, kg)))
                k.play(nxt[kg * per:(kg + 1) * per])
            k.play(k.record(lambda: tail_part(tt)))
        cx.pop()

    finals = []
    for b in (qkm_s, sbq_s, sbk_s, vm_s, og_s, vs_s, gates_s, mixed_s, x2_s, out_d):
        if b is out_d or b.res.name in debug:
            finals.extend(b.res.w)
    k.emit(finals)
    return nc, k


def prep_shared(inp):
    f = lambda a: np.ascontiguousarray(np.asarray(a, dtype=np.float32))
    g = lambda v: f(v).reshape(DC, P).T
    mixed_g = np.concatenate([f(inp['mlstm_norm_g'][0]), f(inp['sb_norm_g'][0])])
    gvec = np.stack([g(inp['mix_norm_g'][0]), g(inp['xattn_norm_g'][0]), g(inp['mem_norm_g'][0]),
                     g(inp['ffn_norm_g'][0]), g(mixed_g)], axis=1)
    sh = {
        'w_in': f(inp['w_in'][0]), 'w_out': f(inp['w_out'][0]), 'xattn_wq': f(inp['xattn_wq'][0]),
        'xattn_wkv': f(inp['xattn_wkv'][0]), 'xattn_wo': f(inp['xattn_wo'][0]), 'peer_wq': f(inp['peer_wq'][0]),
        'subkeysT': f(np.transpose(f(inp['peer_subkeys'][0]).reshape(16, P, P), (2, 0, 1))),
        'peer_u': f(inp['peer_u'][0]), 'peer_v': f(inp['peer_v'][0]),
        'gvec': f(gvec),
        'grow': f(np.stack([f(inp['ffn_norm_g'][0]), f(inp['final_norm_g'])])),
        'conv_wT': f(np.transpose(f(inp['conv_w'][0]).reshape(4, DC, P), (2, 1, 0))),
        'conv_bT': g(inp['conv_b'][0]),
        'gate_b': f(np.stack([f(inp['igate_b'][0]), f(inp['fgate_b'][0])], axis=1)),
    }
    return sh


_CACHE = {}


def kernel(**inputs):
    x = np.asarray(inputs['x'], dtype=np.float32)
    mem = np.asarray(inputs['mem'], dtype=np.float32)
    B, S, _ = x.shape
    sh = prep_shared(inputs)
    if S not in _CACHE:
        _CACHE[S] = build(S)[0]
    nc = _CACHE[S]
    in_maps = []
    for b in range(B):
        m = dict(sh)
        m['x'] = np.ascontiguousarray(x[b])
        m['mem'] = np.ascontiguousarray(mem[b])
        in_maps.append(m)
    res = run_bass_kernel_spmd(nc, in_maps, core_ids=list(range(B)))
    return np.stack([np.asarray(r['out'], dtype=np.float32) for r in res.results], axis=0)
```

```python
import contextlib
import os
import numpy as np
import concourse.bass as bass
import concourse.mybir as mybir
from concourse.bass_utils import run_bass_kernel_spmd

F32 = mybir.dt.float32
BF16 = mybir.dt.bfloat16
U32 = mybir.dt.uint32
I32 = mybir.dt.int32
F32R = mybir.dt.float32r
ALU = mybir.AluOpType
AF = mybir.ActivationFunctionType
AX = mybir.AxisListType

P = 128
D = 1024
DC = D // P
NH_M, HD_M = 4, 128
NH_S, HD_S = 8, 64
PROJ = 3592
C_QKM, C_VM, C_OM, C_I, C_F, C_QS, C_KS, C_VS = 0, 1024, 1536, 2048, 2052, 2056, 2568, 3080
MEM = 256
NXH, XHD = 4, 256
PH, PK, PTOP = 8, 128, 16
NEXP = PK * PK
EPS = 1e-6
NEG = -1.0e30


class Res:
    def __init__(self, name, dram=False):
        self.name = name
        self.w = []
        self.r = []
        self.dsem = None
        self.dcnt = 0
        self.dram = dram


class Buf:
    def __init__(self, ap, res):
        self.ap = ap
        self.res = res


class KB:
    ENG = ('sp', 'pe', 'dve', 'act', 'pool')

    def __init__(self, nc):
        self.nc = nc
        self.sem = {e: nc.alloc_semaphore(name='s_' + e) for e in self.ENG}
        self.cnt = {e: 0 for e in self.ENG}
        self.waited = {e: {} for e in self.ENG}
        self.prog = {e: [] for e in self.ENG}
        self.nsem = len(self.ENG)
        self.ninst = 0
        self.owners = []
        self.marks = []
        self.rec = None

    def _wait(self, e, deps):
        for d in deps:
            if d is None:
                continue
            s, v = d
            key = id(s)
            if self.waited[e].get(key, 0) >= v:
                continue
            self.waited[e][key] = v
            self.prog[e].append(lambda eng, s=s, v=v: eng.wait_ge(s, v))

    @staticmethod
    def _deps(reads, writes, dma_write=False):
        deps = []
        for r in reads:
            deps.extend(r.w)
        for w in writes:
            if not (dma_write and w.dram):
                deps.extend(w.w)
            deps.extend(w.r)
        return deps

    def op(self, e, meth, R=(), W=(), **kw):
        if self.rec is not None:
            self.rec.append(('op', (e, meth), dict(R=R, W=W, **kw)))
            return None
        self._wait(e, self._deps(R, W))
        self.cnt[e] += 1
        self.ninst += 1
        sem = self.sem[e]
        ev = (sem, self.cnt[e])
        self.prog[e].append(lambda eng, meth=meth, kw=kw, sem=sem: getattr(eng, meth)(**kw).then_inc(sem, 1))
        for r in R:
            r.r.append(ev)
        for w in W:
            w.w = [ev]
            w.r = []
        return ev

    def dma(self, q, out, in_, reads=(), writes=(), indirect=None, **kw):
        if self.rec is not None:
            self.rec.append(('dma', (q, out, in_), dict(reads=reads, writes=writes, indirect=indirect, **kw)))
            return None
        self._wait(q, self._deps(reads, writes, dma_write=True))
        (dst,) = writes
        owner = dst if not dst.dram else [r for r in reads if not r.dram][0]
        if owner.dsem is None:
            owner.dsem = self.nc.alloc_semaphore(name='d_' + owner.name)
            self.nsem += 1
            self.owners.append(owner)
        owner.dcnt += 16
        self.ninst += 1
        ev = (owner.dsem, owner.dcnt)
        ds = owner.dsem
        if indirect is None:
            self.prog[q].append(lambda eng, out=out, in_=in_, kw=kw, ds=ds:
                                eng.dma_start(out=out, in_=in_, **kw).then_inc(ds, 16))
        else:
            self.prog[q].append(lambda eng, out=out, in_=in_, io=indirect, kw=kw, ds=ds:
                                eng.indirect_dma_start(out=out, out_offset=None, in_=in_, in_offset=io,
                                                       **kw).then_inc(ds, 16))
        for r in reads:
            r.r.append(ev)
        if dst.dram:
            dst.w = [x for x in dst.w if x[0] is not ds] + [ev]
        else:
            dst.w = [ev]
        dst.r = []
        return ev

    def record(self, fn):
        assert self.rec is None
        self.rec = []
        fn()
        out, self.rec = self.rec, None
        return out

    def play(self, items):
        for kind, a, kw in items:
            if kind == 'op':
                self.op(*a, **kw)
            else:
                self.dma(*a, **kw)

    def barrier(self):
        evs = [(self.sem[e], self.cnt[e]) for e in self.ENG if self.cnt[e] > 0]
        evs += [(o.dsem, o.dcnt) for o in self.owners]
        for e in self.ENG:
            self._wait(e, evs)

    def emit(self, final_events):
        self._wait('sp', final_events)
        prog = self.prog
        with self.nc.Block() as block:
            @block.sync
            def _(eng):
                for f in prog['sp']:
                    f(eng)

            @block.tensor
            def _(eng):
                for f in prog['pe']:
                    f(eng)

            @block.vector
            def _(eng):
                for f in prog['dve']:
                    f(eng)

            @block.scalar
            def _(eng):
                for f in prog['act']:
                    f(eng)

            @block.gpsimd
            def _(eng):
                for f in prog['pool']:
                    f(eng)


class Ctx:
    def __init__(self, nc, k):
        self.nc = nc
        self.k = k
        self.n = 0
        self.stack = [contextlib.ExitStack()]

    def push(self):
        self.stack.append(contextlib.ExitStack())

    def pop(self):
        self.k.marks.append(dict(self.k.cnt))
        self.k.barrier()
        self.stack.pop().close()

    def sb(self, name, shape, dt):
        self.n += 1
        t = self.stack[-1].enter_context(self.nc.sbuf_tensor(f"{name}_{self.n}", list(shape), dt))
        return Buf(t[:] if not hasattr(t, 'ap') else t.ap(), Res(f"{name}_{self.n}"))

    def dram(self, name, shape, dt, kind="Internal"):
        t = self.nc.dram_tensor(name, list(shape), dt, kind=kind)
        return Buf(t.ap(), Res(name, dram=True))


def bc_last(ap, n):
    return ap.unsqueeze(ap.ndim).to_broadcast(list(ap.shape) + [n])


def bc_mid(ap, axis, n):
    shp = list(ap.shape)
    shp.insert(axis, n)
    return ap.unsqueeze(axis).to_broadcast(shp)


def build(S, phases=('A', 'B', 'C', 'D'), debug=()):
    NT = S // P
    NG = S // 512
    assert S % 512 == 0
    nc = bass.Bass("TRN2", target_bir_lowering=False)
    k = KB(nc)
    cx = Ctx(nc, k)

    def dkind(name):
        return "ExternalOutput" if name in debug else "Internal"

    def din(name, shape, dt=F32):
        return Buf(nc.dram_tensor(name, list(shape), dt, kind="ExternalInput").ap(), Res(name, dram=True))

    x_d = din("x", [S, D])
    mem_d = din("mem", [MEM, D])
    w_in_d = din("w_in", [D, PROJ])
    w_out_d = din("w_out", [D, D])
    wq_d = din("xattn_wq", [D, D])
    wkv_d = din("xattn_wkv", [D, 2 * D])
    wo_d = din("xattn_wo", [D, D])
    pwq_d = din("peer_wq", [D, 2 * D])
    skT_d = din("subkeysT", [P, 16, P])
    pu_d = din("peer_u", [NEXP, D])
    pv_d = din("peer_v", [NEXP, D])
    gvec_d = din("gvec", [P, 5, DC])
    grow_d = din("grow", [2, D])
    convw_d = din("conv_wT", [P, DC, 4])
    convb_d = din("conv_bT", [P, DC])
    gateb_d = din("gate_b", [4, 2])
    out_d = Buf(nc.dram_tensor("out", [S, D], F32, kind="ExternalOutput").ap(), Res("out", dram=True))

    qkm_s = cx.dram("qkm_s", [DC, P, S], BF16, kind=dkind("qkm_s"))
    sbq_s = cx.dram("sbq_s", [NH_S * HD_S, S], BF16, kind=dkind("sbq_s"))
    sbk_s = cx.dram("sbk_s", [NH_S * HD_S, S], BF16, kind=dkind("sbk_s"))
    vm_s = cx.dram("vm_s", [S, NH_M * HD_M], BF16, kind=dkind("vm_s"))
    og_s = cx.dram("og_s", [S, NH_M * HD_M], BF16, kind=dkind("og_s"))
    vs_s = cx.dram("vs_s", [S, NH_S * HD_S], BF16, kind=dkind("vs_s"))
    gates_s = cx.dram("gates_s", [4, 2, S], F32, kind=dkind("gates_s"))
    mixed_s = cx.dram("mixed_s", [S, D], BF16, kind=dkind("mixed_s"))
    x2_s = cx.dram("x2_s", [S, D], F32, kind=dkind("x2_s"))
    uv_s = cx.dram("uv_s", [NEXP, 2 * D], BF16)

    banks = []
    for i in range(8):
        t = nc.alloc_psum_tensor(f"bank{i}", [P, 512], F32)
        banks.append(Buf(t.ap(), Res(f"bank{i}")))

    ident_b = cx.sb("ident_b", [P, P], BF16)
    ident_f = cx.sb("ident_f", [P, P], F32)
    for idt in (ident_b, ident_f):
        k.op('pool', 'memset', W=[idt.res], ap=idt.ap, constant=1.0)
        k.op('pool', 'affine_select', R=[idt.res], W=[idt.res], out=idt.ap, in_=idt.ap, pattern=[[-1, P]],
             compare_op=ALU.is_equal, fill=0.0, base=0, channel_multiplier=1)
    gvec = cx.sb("gvec", [P, 5, DC], F32)
    k.dma('sp', gvec.ap, gvec_d.ap, writes=[gvec.res])
    convw = cx.sb("convw", [P, DC, 4], F32)
    k.dma('sp', convw.ap, convw_d.ap, writes=[convw.res])
    convb = cx.sb("convb", [P, DC], F32)
    k.dma('sp', convb.ap, convb_d.ap, writes=[convb.res])
    gateb = cx.sb("gateb", [4, 2], F32)
    k.dma('sp', gateb.ap, gateb_d.ap, writes=[gateb.res])

    STG = 2048
    stage = [cx.sb(f"stage{i}", [P, STG], F32) for i in range(2)]
    stage_i = [0]

    def load_cast(dst_aps, src_aps, dst_res, cast_engs=('dve', 'pool')):
        for dst, src in zip(dst_aps, src_aps):
            st = stage[stage_i[0] % 2]
            n = int(np.prod(src.shape[1:]))
            sview = st.ap[:, 0:n]
            if len(src.shape) == 3:
                sview = sview.rearrange("p (a b) -> p a b", a=src.shape[1])
            k.dma('sp', sview, src, writes=[st.res])
            eng = cast_engs[stage_i[0] % len(cast_engs)]
            k.op(eng, 'tensor_copy', R=[st.res], W=[dst_res], out=dst, in_=sview)
            stage_i[0] += 1

    def make_gbc(idx, name):
        g = cx.sb(name, [P, DC, P], F32)
        k.op('dve', 'tensor_copy', R=[gvec.res], W=[g.res], out=g.ap, in_=bc_last(gvec.ap[:, idx, :], P))
        return g

    def rms_stats(ss, rstd, n_cols):
        k.op('act', 'activation', R=[ss.res], W=[rstd.res], out=rstd.ap[:, 0:n_cols], in_=ss.ap[:, 0:n_cols],
             func=AF.Ln, scale=1.0 / D, bias=EPS)
        k.op('act', 'activation', R=[rstd.res], W=[rstd.res], out=rstd.ap[:, 0:n_cols], in_=rstd.ap[:, 0:n_cols],
             func=AF.Exp, scale=-0.5)

    if 'D' in phases:
        cx.push()
        RT = 2
        tin_u = [cx.sb(f"tin_u{i}", [P, RT, D], F32) for i in range(2)]
        tin_v = [cx.sb(f"tin_v{i}", [P, RT, D], F32) for i in range(2)]
        tout = [cx.sb(f"tout{i}", [P, RT, 2 * D], BF16) for i in range(2)]
        nchunk = NEXP // (P * RT)
        for c in range(nchunk):
            rs_ = slice(c * P * RT, (c + 1) * P * RT)
            tu, tv, to = tin_u[c % 2], tin_v[c % 2], tout[c % 2]
            k.dma('sp', tu.ap, pu_d.ap[rs_, :].rearrange("(p r) d -> p r d", r=RT), writes=[tu.res])
            k.dma('sp', tv.ap, pv_d.ap[rs_, :].rearrange("(p r) d -> p r d", r=RT), writes=[tv.res])
            k.op('dve', 'tensor_copy', R=[tu.res], W=[to.res], out=to.ap[:, :, 0:D], in_=tu.ap)
            k.op('act', 'copy', R=[tv.res], W=[to.res], out=to.ap[:, :, D:2 * D], in_=tv.ap)
            k.dma('pool', uv_s.ap[rs_, :].rearrange("(p r) d -> p r d", r=RT), to.ap, reads=[to.res], writes=[uv_s.res])
        cx.pop()

    if 'A' in phases:
        cx.push()
        gst = [cx.sb(f"gst{i}", [4, 2, 512], F32) for i in range(2)]
        w_in = cx.sb("w_in", [P, DC, PROJ], BF16)
        load_cast([w_in.ap[:, c, a:min(a + STG, PROJ)] for c in range(DC) for a in range(0, PROJ, STG)],
                  [w_in_d.ap[c * P:(c + 1) * P, a:min(a + STG, PROJ)] for c in range(DC) for a in range(0, PROJ, STG)], w_in.res)
        g_mix = make_gbc(0, "g_mix")
        xg = [cx.sb(f"xg{i}", [P, 4, D], F32) for i in range(2)]
        xn = cx.sb("xn", [P, 4, D], BF16)
        junk = cx.sb("junk", [P, D], F32)
        ssq = [cx.sb(f"ssq{i}", [P, 4], F32) for i in range(2)]
        rstd = [cx.sb(f"rstd{i}", [P, 4], F32) for i in range(2)]
        hT = [cx.sb(f"hT{i}", [P, DC, 512], BF16) for i in range(2)]
        pre = cx.sb("pre", [P, DC, 3 + 512], F32)
        cacc = [cx.sb(f"cacc{i}", [P, 512], F32) for i in range(2)]
        qkm_o = [cx.sb(f"qkm_o{i}", [P, 512], BF16) for i in range(3)]
        sb_o = [cx.sb(f"sb_o{i}", [P, 512], BF16) for i in range(3)]
        tm_o = [cx.sb(f"tm_o{i}", [P, 512], BF16) for i in range(4)]
        k.op('pool', 'memset', W=[pre.res], ap=pre.ap[:, :, 0:3], constant=0.0)
        rot = {'mm': 0, 'q': 0, 's': 0, 't': 0}
        MMB = [2, 3, 4, 5, 6, 7]

        def mm_bank():
            b = banks[MMB[rot['mm'] % len(MMB)]]
            rot['mm'] += 1
            return b

        def load_x(g):
            xb = xg[g % 2]
            k.dma('sp', xb.ap, x_d.ap[g * 512:(g + 1) * 512, :].rearrange("(a p) d -> p a d", p=P), writes=[xb.res])

        def a_stage1(g):
            xb, ss, rs, hTg = xg[g % 2], ssq[g % 2], rstd[g % 2], hT[g % 2]
            gs = slice(g * 512, (g + 1) * 512)
            for tl in range(4):
                k.op('act', 'activation', R=[xb.res], W=[junk.res, ss.res], out=junk.ap, in_=xb.ap[:, tl, :],
                     func=AF.Square, accum_out=ss.ap[:, tl:tl + 1])
            rms_stats(ss, rs, 4)
            for tl in range(4):
                k.op('dve', 'tensor_scalar', R=[xb.res, rs.res], W=[xn.res], out=xn.ap[:, tl, :], in0=xb.ap[:, tl, :],
                     scalar1=rs.ap[:, tl:tl + 1], scalar2=None, op0=ALU.mult)
            for tl in range(4):
                pb = banks[tl % 2]
                pview = pb.ap.bitcast(BF16).rearrange("p (c t) -> p c t", c=DC)
                for c in range(DC):
                    k.op('pe', 'transpose', R=[xn.res, ident_b.res], W=[pb.res], out=pview[:, c, :],
                         in_=xn.ap[:, tl, c * P:(c + 1) * P], identity=ident_b.ap)
                k.op('dve', 'tensor_tensor', R=[pb.res, g_mix.res], W=[hTg.res], out=hTg.ap[:, :, tl * P:(tl + 1) * P],
                     in0=pview, in1=g_mix.ap, op=ALU.mult)


        def a_stage2(g):
            hTg = hT[g % 2]
            gs = slice(g * 512, (g + 1) * 512)
            def fm_matmul(col0, M):
                b = mm_bank()
                for c in range(DC):
                    k.op('pe', 'matmul', R=[w_in.res, hTg.res], W=[b.res], out=b.ap[0:M, :], lhsT=w_in.ap[:, c, col0:col0 + M],
                         rhs=hTg.ap[:, c, :], start=(c == 0), stop=(c == DC - 1))
                return b

            for c in range(DC):
                b = fm_matmul(C_QKM + c * P, P)
                k.op('act', 'copy', R=[b.res], W=[pre.res], out=pre.ap[:, c, 3:515], in_=b.ap)
            for c in range(DC):
                ca = cacc[c % 2]
                k.op('dve', 'tensor_scalar', R=[pre.res, convw.res, convb.res], W=[ca.res], out=ca.ap, in0=pre.ap[:, c, 0:512],
                     scalar1=convw.ap[:, c, 0:1], scalar2=convb.ap[:, c:c + 1], op0=ALU.mult, op1=ALU.add)
                for j in range(1, 4):
                    k.op('dve', 'scalar_tensor_tensor', R=[pre.res, convw.res, ca.res], W=[ca.res], out=ca.ap,
                         in0=pre.ap[:, c, j:j + 512], scalar=convw.ap[:, c, j:j + 1], in1=ca.ap, op0=ALU.mult, op1=ALU.add)
                qo = qkm_o[rot['q'] % 3]
                rot['q'] += 1
                k.op('act', 'activation', R=[ca.res], W=[qo.res], out=qo.ap, in_=ca.ap, func=AF.Silu)
                k.dma('pool', qkm_s.ap[c, :, gs], qo.ap, reads=[qo.res], writes=[qkm_s.res])
            k.op('dve', 'tensor_copy', R=[pre.res], W=[pre.res], out=pre.ap[:, :, 0:3], in_=pre.ap[:, :, 512:515])
            for col0, dst in ((C_QS, sbq_s), (C_KS, sbk_s)):
                for c in range(4):
                    b = fm_matmul(col0 + c * P, P)
                    so = sb_o[rot['s'] % 3]
                    rot['s'] += 1
                    k.op('act', 'copy', R=[b.res], W=[so.res], out=so.ap, in_=b.ap)
                    k.dma('pool', dst.ap[c * P:(c + 1) * P, gs], so.ap, reads=[so.res], writes=[dst.res])
            gsb = gst[g % 2]
            for gi, col0 in ((0, C_I), (1, C_F)):
                b = fm_matmul(col0, 4)
                k.op('act', 'activation', R=[b.res, gateb.res], W=[gsb.res], out=gsb.ap[:, gi, :], in_=b.ap[0:4, :],
                     func=AF.Identity, bias=gateb.ap[:, gi:gi + 1])
            k.dma('pool', gates_s.ap[:, :, gs], gsb.ap, reads=[gsb.res], writes=[gates_s.res])
            for tl in range(4):
                tt = g * 4 + tl
                for col0, dst, fn in ((C_VM, vm_s, None), (C_OM, og_s, AF.Sigmoid), (C_VS, vs_s, None)):
                    b = mm_bank()
                    for c in range(DC):
                        k.op('pe', 'matmul', R=[w_in.res, hTg.res], W=[b.res], out=b.ap, lhsT=hTg.ap[:, c, tl * P:(tl + 1) * P],
                             rhs=w_in.ap[:, c, col0:col0 + 512], start=(c == 0), stop=(c == DC - 1))
                    to = tm_o[rot['t'] % 4]
                    rot['t'] += 1
                    if fn is None:
                        k.op('dve', 'tensor_copy', R=[b.res], W=[to.res], out=to.ap, in_=b.ap)
                    else:
                        k.op('act', 'activation', R=[b.res], W=[to.res], out=to.ap, in_=b.ap, func=fn)
                    k.dma('pool', dst.ap[tt * P:(tt + 1) * P, :], to.ap, reads=[to.res], writes=[dst.res])

        load_x(0)
        if NG > 1:
            load_x(1)
        k.play(k.record(lambda: a_stage1(0)))
        for g in range(NG):
            if g + 2 < NG:
                load_x(g + 2)
            if g + 1 < NG:
                k.play(k.record(lambda: a_stage1(g + 1)))
            k.play(k.record(lambda: a_stage2(g)))
        cx.pop()

    if 'B' in phases:
        cx.push()
        KSC = float(HD_M) ** -0.5
        g4 = cx.sb("g4", [4, 2, S], F32)
        k.dma('sp', g4.ap, gates_s.ap, reads=[gates_s.res], writes=[g4.res])
        t2 = cx.sb("t2", [4, S], F32)
        k.op('act', 'activation', R=[g4.res], W=[t2.res], out=t2.ap, in_=g4.ap[:, 1, :], func=AF.Exp, scale=-1.0)
        k.op('act', 'activation', R=[t2.res], W=[t2.res], out=t2.ap, in_=t2.ap, func=AF.Ln, bias=1.0)
        ones4 = cx.sb("ones4", [4, S], F32)
        k.op('pool', 'memset', W=[ones4.res], ap=ones4.ap, constant=1.0)
        csum = cx.sb("csum", [4, S], F32)
        k.op('dve', 'tensor_tensor_scan', R=[ones4.res, t2.res], W=[csum.res], out=csum.ap, data0=ones4.ap, data1=t2.ap,
             initial=0.0, op0=ALU.mult, op1=ALU.add)
        mi = cx.sb("mi", [4, 1], F32)
        k.op('dve', 'tensor_reduce', R=[g4.res], W=[mi.res], out=mi.ap, in_=g4.ap[:, 0, :], axis=AX.X, op=ALU.max)
        crow = cx.sb("crow", [4, S], F32)
        k.op('dve', 'scalar_tensor_tensor', R=[g4.res, mi.res, csum.res], W=[crow.res], out=crow.ap, in0=g4.ap[:, 0, :],
             scalar=mi.ap[:, 0:1], in1=csum.ap, op0=ALU.subtract, op1=ALU.add)
        brow = cx.sb("brow", [4, S], F32)
        k.op('dve', 'tensor_scalar', R=[csum.res], W=[brow.res], out=brow.ap, in0=csum.ap, scalar1=-1.0, scalar2=None, op0=ALU.mult)
        em = cx.sb("em", [4, 1], F32)
        k.op('act', 'activation', R=[mi.res], W=[em.res], out=em.ap, in_=mi.ap, func=AF.Exp, scale=-1.0)
        sel = cx.sb("sel", [4, 4, P], F32)
        k.op('dve', 'tensor_copy', R=[ident_f.res], W=[sel.res], out=sel.ap, in_=bc_last(ident_f.ap[0:4, 0:4], P))
        dgem = cx.sb("dgem", [4, 4], F32)
        k.op('dve', 'tensor_scalar', R=[ident_f.res, em.res], W=[dgem.res], out=dgem.ap, in0=ident_f.ap[0:4, 0:4],
             scalar1=em.ap[:, 0:1], scalar2=None, op0=ALU.mult)
        ones4p = cx.sb("ones4p", [4, P], F32)
        k.op('pool', 'memset', W=[ones4p.res], ap=ones4p.ap, constant=1.0)
        ccol = cx.sb("ccol", [P, NT, 4], F32)
        embc = cx.sb("embc", [P, 4], F32)
        b2 = banks[2]
        for blk in range(NT):
            k.op('pe', 'matmul', R=[crow.res, ident_f.res], W=[b2.res], out=b2.ap[:, blk * 4:(blk + 1) * 4],
                 lhsT=crow.ap[:, blk * P:(blk + 1) * P], rhs=ident_f.ap[0:4, 0:4], start=True, stop=True)
        k.op('dve', 'tensor_copy', R=[b2.res], W=[ccol.res], out=ccol.ap, in_=b2.ap[:, 0:NT * 4].rearrange("p (n h) -> p n h", h=4))
        b3 = banks[3]
        k.op('pe', 'matmul', R=[ones4p.res, dgem.res], W=[b3.res], out=b3.ap[:, 0:4], lhsT=ones4p.ap, rhs=dgem.ap, start=True, stop=True)
        k.op('dve', 'tensor_copy', R=[b3.res], W=[embc.res], out=embc.ap, in_=b3.ap[:, 0:4])

        qTb = cx.sb("qTb", [P, S], BF16)
        kTb = cx.sb("kTb", [P, S], BF16)
        v1 = cx.sb("v1", [P, NT, HD_M + 1], BF16)
        k.op('pool', 'memset', W=[v1.res], ap=v1.ap, constant=1.0)
        Bbc = cx.sb("Bbc", [P, S], F32)
        Dt = [cx.sb(f"Dt{i}", [P, 512], F32) for i in range(2)]
        At = [cx.sb(f"At{i}", [P, 512], BF16) for i in range(2)]
        ogt = [cx.sb(f"ogt{i}", [P, 4, HD_M], BF16) for i in range(2)]
        hm = [cx.sb(f"hm{i}", [P, 4, HD_M], F32) for i in range(2)]
        hsq = cx.sb("hsq", [P, HD_M], F32)
        dn = [cx.sb(f"dn{i}", [P, 4], F32) for i in range(2)]
        hss = [cx.sb(f"hss{i}", [P, 4], F32) for i in range(2)]
        hrs = [cx.sb(f"hrs{i}", [P, 4], F32) for i in range(2)]
        mo = [cx.sb(f"mo{i}", [P, 4, HD_M], BF16) for i in range(2)]
        it = 0
        ch = 0
        for h in range(int(os.environ.get('NHB', NH_M))):
            k.dma('sp', qTb.ap, qkm_s.ap[h], reads=[qkm_s.res], writes=[qTb.res])
            k.dma('sp', kTb.ap, qkm_s.ap[4 + h], reads=[qkm_s.res], writes=[kTb.res])
            with nc.allow_non_contiguous_dma(reason="per-head v slices, 256B runs"):
                k.dma('sp', v1.ap[:, :, 0:HD_M], vm_s.ap[:, h * HD_M:(h + 1) * HD_M].rearrange("(n p) d -> p n d", p=P),
                      reads=[vm_s.res], writes=[v1.res])
            for c5 in range(S // 512):
                bb = banks[2 + c5 % 2]
                k.op('pe', 'matmul', R=[sel.res, brow.res], W=[bb.res], out=bb.ap, lhsT=sel.ap[:, h, :],
                     rhs=brow.ap[:, c5 * 512:(c5 + 1) * 512], start=True, stop=True)
                k.op('act', 'copy', R=[bb.res], W=[Bbc.res], out=Bbc.ap[:, c5 * 512:(c5 + 1) * 512], in_=bb.ap)
            for q in range(NG):
                og = ogt[ch % 2]
                with nc.allow_non_contiguous_dma(reason="per-head o-gate slices"):
                    k.dma('sp', og.ap, og_s.ap[q * 512:(q + 1) * 512, h * HD_M:(h + 1) * HD_M].rearrange("(a p) d -> p a d", p=P),
                          reads=[og_s.res], writes=[og.res])
                accs = [banks[4 + i] for i in range(4)]
                steps = []
                for j in range(4 * q + 4):
                    t0 = max(P * j, 512 * q)
                    steps.append(dict(j=j, t0=t0, n=512 * (q + 1) - t0, zb=banks[it % 2], dt_=Dt[it % 2], at_=At[it % 2]))
                    it += 1

                def bstage1(st):
                    j, t0, n, zb, dt_, at_ = st['j'], st['t0'], st['n'], st['zb'], st['dt_'], st['at_']
                    k.op('pe', 'matmul', R=[kTb.res, qTb.res], W=[zb.res], out=zb.ap[:, 0:n], lhsT=kTb.ap[:, j * P:(j + 1) * P],
                         rhs=qTb.ap[:, t0:t0 + n], start=True, stop=True)
                    k.op('act', 'activation', R=[Bbc.res, ccol.res], W=[dt_.res], out=dt_.ap[:, 0:n], in_=Bbc.ap[:, t0:t0 + n],
                         func=AF.Exp, bias=ccol.ap[:, j, h:h + 1])
                    if j >= 4 * q:
                        k.op('pool', 'affine_select', R=[dt_.res], W=[dt_.res], out=dt_.ap[:, 0:P], in_=dt_.ap[:, 0:P],
                             pattern=[[1, P]], compare_op=ALU.is_ge, fill=0.0, base=0, channel_multiplier=-1)
                    k.op('dve', 'scalar_tensor_tensor', R=[zb.res, dt_.res], W=[at_.res], out=at_.ap[:, 0:n], in0=zb.ap[:, 0:n],
                         scalar=KSC, in1=dt_.ap[:, 0:n], op0=ALU.mult, op1=ALU.mult)

                def bstage2(st):
                    j, t0, at_ = st['j'], st['t0'], st['at_']
                    for i in range(max(j, 4 * q), 4 * q + 4):
                        o = i * P - t0
                        acc = accs[i - 4 * q]
                        k.op('pe', 'matmul', R=[at_.res, v1.res], W=[acc.res], out=acc.ap[:, 0:HD_M + 1], lhsT=at_.ap[:, o:o + P],
                             rhs=v1.ap[:, j, :], start=(j == 0), stop=(j == i))

                ns = len(steps)
                for x_ in range(ns + 1):
                    if x_ < ns:
                        bstage1(steps[x_])
                    if 0 <= x_ - 1 < ns:
                        bstage2(steps[x_ - 1])
                d_, ss_, rs_, hm_, mo_ = dn[ch % 2], hss[ch % 2], hrs[ch % 2], hm[ch % 2], mo[ch % 2]
                for i in range(4):
                    acc = accs[i]
                    k.op('act', 'activation', R=[acc.res], W=[d_.res], out=d_.ap[:, i:i + 1], in_=acc.ap[:, HD_M:HD_M + 1], func=AF.Abs)
                k.op('dve', 'tensor_scalar', R=[d_.res, embc.res], W=[d_.res], out=d_.ap, in0=d_.ap, scalar1=embc.ap[:, h:h + 1],
                     scalar2=None, op0=ALU.max)
                k.op('dve', 'reciprocal', R=[d_.res], W=[d_.res], out=d_.ap, in_=d_.ap)
                for i in range(4):
                    acc = accs[i]
                    k.op('dve', 'scalar_tensor_tensor', R=[acc.res, d_.res, og.res], W=[hm_.res], out=hm_.ap[:, i, :], in0=acc.ap[:, 0:HD_M],
                         scalar=d_.ap[:, i:i + 1], in1=og.ap[:, i, :], op0=ALU.mult, op1=ALU.mult)
                    k.op('dve', 'scalar_tensor_tensor', R=[hm_.res], W=[hsq.res, ss_.res], out=hsq.ap, in0=hm_.ap[:, i, :], scalar=1.0, in1=hm_.ap[:, i, :],
                         op0=ALU.mult, op1=ALU.mult, accum_out=ss_.ap[:, i:i + 1])
                k.op('act', 'activation', R=[ss_.res], W=[rs_.res], out=rs_.ap, in_=ss_.ap, func=AF.Ln, scale=1.0 / HD_M, bias=EPS)
                k.op('act', 'activation', R=[rs_.res], W=[rs_.res], out=rs_.ap, in_=rs_.ap, func=AF.Exp, scale=-0.5)
                k.op('dve', 'tensor_tensor', R=[hm_.res, rs_.res], W=[mo_.res], out=mo_.ap, in0=hm_.ap, in1=bc_last(rs_.ap, HD_M), op=ALU.mult)
                with nc.allow_non_contiguous_dma(reason="per-head mixed slices"):
                    k.dma('pool', mixed_s.ap[q * 512:(q + 1) * 512, h * HD_M:(h + 1) * HD_M].rearrange("(a p) d -> p a d", p=P), mo_.ap,
                          reads=[mo_.res], writes=[mixed_s.res])
                ch += 1
        cx.pop()

    if 'C' in phases:
        cx.push()
        SSC = float(HD_S) ** -0.5
        tri = cx.sb("tri", [P, P], F32)
        k.op('pool', 'memset', W=[tri.res], ap=tri.ap, constant=1.0)
        k.op('pool', 'affine_select', R=[tri.res], W=[tri.res], out=tri.ap, in_=tri.ap, pattern=[[-1, P]],
             compare_op=ALU.is_ge, fill=0.0, base=0, channel_multiplier=1)
        onesf = cx.sb("onesf", [P, P], F32)
        k.op('pool', 'memset', W=[onesf.res], ap=onesf.ap, constant=1.0)
        sq_ = cx.sb("sq_", [HD_S, S], BF16)
        sk_ = cx.sb("sk_", [HD_S, S], BF16)
        skn = cx.sb("skn", [HD_S, S], BF16)
        sv_ = cx.sb("sv_", [P, NT, HD_S], BF16)
        e1 = [cx.sb(f"e1{i}", [P, 512], F32) for i in range(2)]
        sA = [cx.sb(f"sA{i}", [P, 512], BF16) for i in range(2)]
        raccs = [cx.sb(f"racc{i}", [P, 512], F32) for i in range(2)]
        os_ = [cx.sb(f"os{i}", [P, 4, HD_S], F32) for i in range(2)]
        osq = cx.sb("osq", [P, HD_S], F32)
        sss = [cx.sb(f"sss{i}", [P, 4], F32) for i in range(2)]
        srs = [cx.sb(f"srs{i}", [P, 4], F32) for i in range(2)]
        smo = [cx.sb(f"smo{i}", [P, 4, HD_S], BF16) for i in range(2)]
        it = 0
        ch = 0
        for h in range(NH_S):
            k.dma('sp', sq_.ap, sbq_s.ap[h * HD_S:(h + 1) * HD_S, :], reads=[sbq_s.res], writes=[sq_.res])
            k.dma('sp', sk_.ap, sbk_s.ap[h * HD_S:(h + 1) * HD_S, :], reads=[sbk_s.res], writes=[sk_.res])
            with nc.allow_non_contiguous_dma(reason="per-head v slices, 128B runs"):
                k.dma('sp', sv_.ap, vs_s.ap[:, h * HD_S:(h + 1) * HD_S].rearrange("(n p) d -> p n d", p=P),
                      reads=[vs_s.res], writes=[sv_.res])
            k.op('dve', 'tensor_scalar', R=[sk_.res], W=[skn.res], out=skn.ap, in0=sk_.ap, scalar1=-SSC, scalar2=None, op0=ALU.mult)
            for q in range(NG):
                accs = [banks[4 + i] for i in range(4)]
                for rb in raccs:
                    k.op('pool', 'memset', W=[rb.res], ap=rb.ap, constant=0.0)
                steps = []
                for j in range(4 * q + 3, -1, -1):
                    c0 = max(P * j - 512 * q, 0)
                    steps.append(dict(j=j, c0=c0, t0=512 * q + c0, n=512 - c0, diag=(j >= 4 * q), first=(j == 4 * q + 3),
                                      zb=banks[it % 2], cp=banks[2 + it % 2], e_=e1[it % 2], a_=sA[it % 2],
                                      rc=raccs[len(steps) % 2], rn=raccs[(len(steps) + 1) % 2]))
                    it += 1

                def stage1(st):
                    j, t0, n, zb, e_ = st['j'], st['t0'], st['n'], st['zb'], st['e_']
                    k.op('pe', 'matmul', R=[sk_.res, sq_.res], W=[zb.res], out=zb.ap[:, 0:n], lhsT=sk_.ap[:, j * P:(j + 1) * P],
                         rhs=sq_.ap[:, t0:t0 + n], start=True, stop=True)
                    k.op('act', 'activation', R=[zb.res], W=[e_.res], out=e_.ap[:, 0:n], in_=zb.ap[:, 0:n], func=AF.Exp, scale=SSC)
                    k.op('act', 'activation', R=[e_.res], W=[e_.res], out=e_.ap[:, 0:n], in_=e_.ap[:, 0:n], func=AF.Ln, bias=1.0)
                    if st['diag']:
                        k.op('pool', 'affine_select', R=[e_.res], W=[e_.res], out=e_.ap[:, 0:P], in_=e_.ap[:, 0:P], pattern=[[1, P]],
                             compare_op=ALU.is_ge, fill=0.0, base=-1, channel_multiplier=-1)

                def stage2(st):
                    j, c0, t0, n, cp, e_, a_ = st['j'], st['c0'], st['t0'], st['n'], st['cp'], st['e_'], st['a_']
                    k.op('pe', 'matmul', R=[tri.res, e_.res], W=[cp.res], out=cp.ap[:, 0:n], lhsT=tri.ap,
                         rhs=e_.ap[:, 0:n], start=True, stop=False)
                    rc, rn = st['rc'], st['rn']
                    if not st['first']:
                        k.op('pe', 'matmul', R=[onesf.res, rc.res], W=[cp.res], out=cp.ap[:, 0:n], lhsT=onesf.ap,
                             rhs=rc.ap[:, c0:512], start=False, stop=False)
                    k.op('pe', 'matmul', R=[skn.res, sq_.res], W=[cp.res], out=cp.ap[:, 0:n], lhsT=skn.ap[:, j * P:(j + 1) * P],
                         rhs=sq_.ap[:, t0:t0 + n], start=False, stop=True)
                    if j > 0:
                        k.op('pool', 'tensor_tensor', R=[rc.res, e_.res], W=[rn.res], out=rn.ap[:, c0:512], in0=rc.ap[:, c0:512],
                             in1=e_.ap[:, 0:n], op=ALU.add)
                    k.op('act', 'activation', R=[cp.res], W=[a_.res], out=a_.ap[:, 0:n], in_=cp.ap[:, 0:n], func=AF.Exp, scale=-1.0)
                    if st['diag']:
                        k.op('pool', 'affine_select', R=[a_.res], W=[a_.res], out=a_.ap[:, 0:P], in_=a_.ap[:, 0:P], pattern=[[1, P]],
                             compare_op=ALU.is_ge, fill=0.0, base=-1, channel_multiplier=-1)

                def stage3(st):
                    j, t0, a_ = st['j'], st['t0'], st['a_']
                    for i in range(max(j, 4 * q), 4 * q + 4):
                        o = i * P - t0
                        acc = accs[i - 4 * q]
                        k.op('pe', 'matmul', R=[a_.res, sv_.res], W=[acc.res], out=acc.ap[:, 0:HD_S], lhsT=a_.ap[:, o:o + P],
                             rhs=sv_.ap[:, j, :], start=(j == i), stop=(j == 0))

                ns = len(steps)
                for x_ in range(ns + 2):
                    if x_ < ns:
                        stage1(steps[x_])
                    if 0 <= x_ - 1 < ns:
                        stage2(steps[x_ - 1])
                    if 0 <= x_ - 2 < ns:
                        stage3(steps[x_ - 2])
                o_, ss_, rs_, mo_ = os_[ch % 2], sss[ch % 2], srs[ch % 2], smo[ch % 2]
                for i in range(4):
                    k.op('act', 'copy', R=[accs[i].res], W=[o_.res], out=o_.ap[:, i, :], in_=accs[i].ap[:, 0:HD_S])
                for i in range(4):
                    k.op('dve', 'scalar_tensor_tensor', R=[o_.res], W=[osq.res, ss_.res], out=osq.ap, in0=o_.ap[:, i, :], scalar=1.0, in1=o_.ap[:, i, :],
                         op0=ALU.mult, op1=ALU.mult, accum_out=ss_.ap[:, i:i + 1])
                k.op('act', 'activation', R=[ss_.res], W=[rs_.res], out=rs_.ap, in_=ss_.ap, func=AF.Ln, scale=1.0 / HD_S, bias=EPS)
                k.op('act', 'activation', R=[rs_.res], W=[rs_.res], out=rs_.ap, in_=rs_.ap, func=AF.Exp, scale=-0.5)
                k.op('dve', 'tensor_tensor', R=[o_.res, rs_.res], W=[mo_.res], out=mo_.ap, in0=o_.ap, in1=bc_last(rs_.ap, HD_S), op=ALU.mult)
                with nc.allow_non_contiguous_dma(reason="per-head mixed slices"):
                    k.dma('pool', mixed_s.ap[q * 512:(q + 1) * 512, 512 + h * HD_S:512 + (h + 1) * HD_S].rearrange("(a p) d -> p a d", p=P),
                          mo_.ap, reads=[mo_.res], writes=[mixed_s.res])
                ch += 1
        cx.pop()

    if 'D' in phases:
        cx.push()
        XSC = float(XHD) ** -0.5
        w_out = cx.sb("w_out", [P, DC, D], BF16)
        wq = cx.sb("wq", [P, DC, D], BF16)
        wo = cx.sb("wo", [P, DC, D], BF16)
        kmT = cx.sb("kmT", [P, DC, MEM], BF16)
        vmem = cx.sb("vmem", [P, 2, D], BF16)
        g_mixed = make_gbc(4, "g_mixed")
        g_xat = make_gbc(1, "g_xat")
        junk1 = cx.sb("junk1", [P, D], F32)

        def rows(wd, c, a, n):
            return wd.ap[c * P:(c + 1) * P, a:a + n]

        def norm_T(src, ss, rs, xn_b, gbc, dstT, ncol_off=0, tbank=None):
            k.op('act', 'activation', R=[src.res], W=[junk1.res, ss.res], out=junk1.ap, in_=src.ap, func=AF.Square, accum_out=ss.ap[:, 0:1])
            rms_stats(ss, rs, 1)
            k.op('dve', 'tensor_scalar', R=[src.res, rs.res], W=[xn_b.res], out=xn_b.ap, in0=src.ap, scalar1=rs.ap[:, 0:1], scalar2=None, op0=ALU.mult)
            transpose_T(xn_b, gbc, dstT, ncol_off, tbank)

        trot = [0]

        def transpose_T(src_b, gbc, dstT, ncol_off=0, tbank=None):
            pb = tbank if tbank is not None else banks[trot[0] % 2]
            trot[0] += 1
            pview = pb.ap.bitcast(BF16).rearrange("p (c t) -> p c t", c=DC)
            for c in range(DC):
                k.op('pe', 'transpose', R=[src_b.res, ident_b.res], W=[pb.res], out=pview[:, c, :], in_=src_b.ap[:, c * P:(c + 1) * P],
                     identity=ident_b.ap)
            k.op('dve', 'tensor_tensor', R=[pb.res, gbc.res], W=[dstT.res], out=dstT.ap[:, :, ncol_off:ncol_off + P], in0=pview, in1=gbc.ap,
                 op=ALU.mult)

        cx.push()
        wkv = cx.sb("wkv", [P, DC, 2 * D], BF16)
        load_cast([wkv.ap[:, c, :] for c in range(DC)], [rows(wkv_d, c, 0, 2 * D) for c in range(DC)], wkv.res)
        g_mem = make_gbc(2, "g_mem")
        memt = cx.sb("memt", [P, D], F32)
        memn = cx.sb("memn", [P, D], BF16)
        memT = cx.sb("memT", [P, DC, MEM], BF16)
        mss = cx.sb("mss", [P, 1], F32)
        mrs = cx.sb("mrs", [P, 1], F32)
        for mc in range(2):
            k.dma('sp', memt.ap, mem_d.ap[mc * P:(mc + 1) * P, :], writes=[memt.res])
            norm_T(memt, mss, mrs, memn, g_mem, memT, ncol_off=mc * P)
        for c in range(DC):
            b = banks[2 + c % 2]
            for dc in range(DC):
                k.op('pe', 'matmul', R=[wkv.res, memT.res], W=[b.res], out=b.ap[:, 0:MEM], lhsT=wkv.ap[:, dc, c * P:(c + 1) * P], rhs=memT.ap[:, dc, :],
                     start=(dc == 0), stop=(dc == DC - 1))
            k.op('act', 'copy', R=[b.res], W=[kmT.res], out=kmT.ap[:, c, :], in_=b.ap[:, 0:MEM])
        for mc in range(2):
            for hf in range(2):
                b = banks[4 + (mc * 2 + hf) % 2]
                for dc in range(DC):
                    k.op('pe', 'matmul', R=[wkv.res, memT.res], W=[b.res], out=b.ap, lhsT=memT.ap[:, dc, mc * P:(mc + 1) * P],
                         rhs=wkv.ap[:, dc, D + hf * 512:D + (hf + 1) * 512], start=(dc == 0), stop=(dc == DC - 1))
                k.op('dve', 'tensor_copy', R=[b.res], W=[vmem.res], out=vmem.ap[:, mc, hf * 512:(hf + 1) * 512], in_=b.ap)
        cx.pop()

        load_cast([w_out.ap[:, c, :] for c in range(DC)], [rows(w_out_d, c, 0, D) for c in range(DC)], w_out.res)
        load_cast([wq.ap[:, c, :] for c in range(DC)], [rows(wq_d, c, 0, D) for c in range(DC)], wq.res)
        load_cast([wo.ap[:, c, :] for c in range(DC)], [rows(wo_d, c, 0, D) for c in range(DC)], wo.res)

        NB = 2
        xt_ = [cx.sb(f"xt{i}", [P, D], F32) for i in range(NB)]
        mx_ = [cx.sb(f"mx{i}", [P, D], BF16) for i in range(NB)]
        mT_ = [cx.sb(f"mT{i}", [P, DC, P], BF16) for i in range(NB)]
        x1_ = [cx.sb(f"x1{i}", [P, D], F32) for i in range(NB)]
        x2_ = [cx.sb(f"x2{i}", [P, D], F32) for i in range(NB)]
        xnb = [cx.sb(f"xnb{i}", [P, D], BF16) for i in range(NB)]
        h2T = [cx.sb(f"h2T{i}", [P, DC, P], BF16) for i in range(NB)]
        qT_ = [cx.sb(f"qT{i}", [P, DC, P], BF16) for i in range(NB)]
        pex = [cx.sb(f"pex{i}", [P, NXH, MEM], F32) for i in range(NB)]
        pn_ = [cx.sb(f"pn{i}", [P, NXH, MEM], BF16) for i in range(NB)]
        pT_ = [cx.sb(f"pT{i}", [P, DC, P], BF16) for i in range(NB)]
        oT_ = [cx.sb(f"oT{i}", [P, DC, P], BF16) for i in range(NB)]
        st1 = [cx.sb(f"st1{i}", [P, 16], F32) for i in range(NB)]
        mmr = [0]
        PAIRS = [(2, 3), (4, 5), (6, 7)]

        PAIRS_P = {0: [(2, 3), (4, 5)], 1: [(6, 7)]}
        mmp = {0: 0, 1: 0}

        def pair(par):
            lst = PAIRS_P[par]
            p_ = lst[mmp[par] % len(lst)]
            mmp[par] += 1
            return banks[p_[0]], banks[p_[1]]

        def d1_tile(tt):
            r = tt % NB
            xt, mxt, mT, x1, x2, xn_b, hT2, qT, pe_, pn, pT, oT, st = (xt_[r], mx_[r], mT_[r], x1_[r], x2_[r], xnb[r], h2T[r], qT_[r], pex[r],
                                                                      pn_[r], pT_[r], oT_[r], st1[r])
            ts = slice(tt * P, (tt + 1) * P)
            k.dma('sp', xt.ap, x_d.ap[ts, :], writes=[xt.res])
            k.dma('sp', mxt.ap, mixed_s.ap[ts, :], reads=[mixed_s.res], writes=[mxt.res])
            transpose_T(mxt, g_mixed, mT, tbank=banks[tt % 2])
            ba, bb = pair(tt % 2)
            for hf, b in ((0, ba), (1, bb)):
                for dc in range(DC):
                    k.op('pe', 'matmul', R=[mT.res, w_out.res], W=[b.res], out=b.ap, lhsT=mT.ap[:, dc, :], rhs=w_out.ap[:, dc, hf * 512:(hf + 1) * 512],
                         start=(dc == 0), stop=(dc == DC - 1))
                k.op('dve', 'tensor_tensor', R=[b.res, xt.res], W=[x1.res], out=x1.ap[:, hf * 512:(hf + 1) * 512], in0=b.ap,
                     in1=xt.ap[:, hf * 512:(hf + 1) * 512], op=ALU.add)
            ssb = Buf(st.ap[:, 0:1], st.res)
            rsb = Buf(st.ap[:, 1:2], st.res)
            norm_T(x1, ssb, rsb, xn_b, g_xat, hT2, tbank=banks[tt % 2])
            ba, bb = pair(tt % 2)
            for c in range(DC):
                b = ba if c < 4 else bb
                for dc in range(DC):
                    k.op('pe', 'matmul', R=[wq.res, hT2.res], W=[b.res], out=b.ap[:, (c % 4) * P:(c % 4 + 1) * P], lhsT=wq.ap[:, dc, c * P:(c + 1) * P],
                         rhs=hT2.ap[:, dc, :], start=(dc == 0), stop=(dc == DC - 1))
            for hf, b in ((0, ba), (1, bb)):
                k.op('act', 'copy', R=[b.res], W=[qT.res], out=qT.ap[:, hf * 4:(hf + 1) * 4, :], in_=b.ap.rearrange("p (c t) -> p c t", c=4))
            ba, bb = pair(tt % 2)
            for hh in range(NXH):
                b = ba if hh < 2 else bb
                for c2 in range(2):
                    k.op('pe', 'matmul', R=[qT.res, kmT.res], W=[b.res], out=b.ap[:, (hh % 2) * MEM:(hh % 2 + 1) * MEM], lhsT=qT.ap[:, 2 * hh + c2, :],
                         rhs=kmT.ap[:, 2 * hh + c2, :], start=(c2 == 0), stop=(c2 == 1))
            for hf, b in ((0, ba), (1, bb)):
                k.op('dve', 'tensor_reduce', R=[b.res], W=[st.res], out=st.ap[:, 4 + 2 * hf:6 + 2 * hf], in_=b.ap.rearrange("p (h m) -> p h m", h=2),
                     axis=AX.X, op=ALU.max)
            k.op('dve', 'tensor_scalar', R=[st.res], W=[st.res], out=st.ap[:, 4:8], in0=st.ap[:, 4:8], scalar1=-XSC, scalar2=None, op0=ALU.mult)
            for hh in range(NXH):
                b = ba if hh < 2 else bb
                k.op('act', 'activation', R=[b.res, st.res], W=[pe_.res, st.res], out=pe_.ap[:, hh, :], in_=b.ap[:, (hh % 2) * MEM:(hh % 2 + 1) * MEM],
                     func=AF.Exp, scale=XSC, bias=st.ap[:, 4 + hh:5 + hh], accum_out=st.ap[:, 8 + hh:9 + hh])
            k.op('dve', 'reciprocal', R=[st.res], W=[st.res], out=st.ap[:, 12:16], in_=st.ap[:, 8:12])
            k.op('dve', 'tensor_tensor', R=[pe_.res, st.res], W=[pn.res], out=pn.ap, in0=pe_.ap, in1=bc_last(st.ap[:, 12:16], MEM), op=ALU.mult)
            pb = banks[tt % 2]
            pview = pb.ap.bitcast(BF16).rearrange("p (c t) -> p c t", c=DC)
            for hh in range(NXH):
                for mc in range(2):
                    k.op('pe', 'transpose', R=[pn.res, ident_b.res], W=[pb.res], out=pview[:, hh * 2 + mc, :], in_=pn.ap[:, hh, mc * P:(mc + 1) * P],
                         identity=ident_b.ap)
            k.op('act', 'copy', R=[pb.res], W=[pT.res], out=pT.ap, in_=pview)
            ba, bb = pair(tt % 2)
            for c in range(DC):
                b = ba if c < 4 else bb
                hh = c // 2
                for mc in range(2):
                    k.op('pe', 'matmul', R=[vmem.res, pT.res], W=[b.res], out=b.ap[:, (c % 4) * P:(c % 4 + 1) * P], lhsT=vmem.ap[:, mc, c * P:(c + 1) * P],
                         rhs=pT.ap[:, hh * 2 + mc, :], start=(mc == 0), stop=(mc == 1))
            for hf, b in ((0, ba), (1, bb)):
                k.op('act', 'copy', R=[b.res], W=[oT.res], out=oT.ap[:, hf * 4:(hf + 1) * 4, :], in_=b.ap.rearrange("p (c t) -> p c t", c=4))
            ba, bb = pair(tt % 2)
            for hf, b in ((0, ba), (1, bb)):
                for c in range(DC):
                    k.op('pe', 'matmul', R=[oT.res, wo.res], W=[b.res], out=b.ap, lhsT=oT.ap[:, c, :], rhs=wo.ap[:, c, hf * 512:(hf + 1) * 512],
                         start=(c == 0), stop=(c == DC - 1))
                k.op('dve', 'tensor_tensor', R=[b.res, x1.res], W=[x2.res], out=x2.ap[:, hf * 512:(hf + 1) * 512], in0=b.ap,
                     in1=x1.ap[:, hf * 512:(hf + 1) * 512], op=ALU.add)
            k.dma('pool', x2_s.ap[ts, :], x2.ap, reads=[x2.res], writes=[x2_s.res])

        def zip_play(a_, b_, nsl=8):
            pa, pb2 = -(-len(a_) // nsl) if a_ else 0, -(-len(b_) // nsl) if b_ else 0
            for i_ in range(nsl):
                k.play(a_[i_ * pa:(i_ + 1) * pa])
                k.play(b_[i_ * pb2:(i_ + 1) * pb2])

        cur = k.record(lambda: d1_tile(0))
        half = len(cur) // 2
        k.play(cur[:half])
        for tt in range(NT):
            nxt = k.record(lambda: d1_tile(tt + 1)) if tt + 1 < NT else []
            h2 = len(nxt) // 2
            zip_play(cur[half:], nxt[:h2])
            cur, half = nxt, h2
        cx.pop()

    if 'D' in phases:
        cx.push()
        pwq = cx.sb("pwq", [P, DC, 2 * D], BF16)
        load_cast([pwq.ap[:, c, :] for c in range(DC)], [pwq_d.ap[c * P:(c + 1) * P, :] for c in range(DC)], pwq.res)
        skT = cx.sb("skT", [P, 16, P], BF16)
        load_cast([skT.ap], [skT_d.ap], skT.res)
        g_ffn = make_gbc(3, "g_ffn")
        ffn_bc = cx.sb("ffn_bc", [P, D], F32)
        fin_bc = cx.sb("fin_bc", [P, D], F32)
        gt = grow_d.ap.tensor
        k.dma('sp', ffn_bc.ap, bass.AP(gt, 0, [[0, P], [1, D]]), writes=[ffn_bc.res])
        k.dma('sp', fin_bc.ap, bass.AP(gt, D, [[0, P], [1, D]]), writes=[fin_bc.res])
        iota_i = cx.sb("iota_i", [P, 16], I32)
        k.op('pool', 'iota', W=[iota_i.res], out=iota_i.ap, pattern=[[1, 16]], base=0, channel_multiplier=0)
        iota16 = cx.sb("iota16", [P, 16], F32)
        k.op('dve', 'tensor_copy', R=[iota_i.res], W=[iota16.res], out=iota16.ap, in_=iota_i.ap)
        thr16 = cx.sb("thr16", [P, 16], F32)
        k.op('dve', 'tensor_scalar', R=[iota16.res], W=[thr16.res], out=thr16.ap, in0=iota16.ap, scalar1=16.0, scalar2=15.5,
             op0=ALU.mult, op1=ALU.add)
        junk2 = cx.sb("junk2", [P, D], F32)

        NB = 1
        x2t = [cx.sb(f"x2t{i}", [P, D], F32) for i in range(2)]
        h3 = [cx.sb(f"h3{i}", [P, D], F32) for i in range(2)]
        xn3 = [cx.sb(f"xn3{i}", [P, D], BF16) for i in range(NB)]
        h3T = [cx.sb(f"h3T{i}", [P, DC, P], BF16) for i in range(NB)]
        pqT = [cx.sb(f"pqT{i}", [P, 16, P], BF16) for i in range(NB)]
        ssb_ = [cx.sb(f"ssb{i}", [P, 16, P], F32) for i in range(NB)]
        s2b_ = cx.sb("s2b", [P, 16, P], F32)
        cand2 = Buf(s2b_.ap.rearrange("p a b -> p (a b)").rearrange("p (h c) -> p h c", h=PH), s2b_.res)
        mxs = [cx.sb(f"mxs{i}", [P, 16, 16], F32) for i in range(NB)]
        ixs = [cx.sb(f"ixs{i}", [P, 16, 16], U32) for i in range(NB)]
        ixf = [cx.sb(f"ixf{i}", [P, 16, 16], F32) for i in range(NB)]
        cand = [cx.sb(f"cand{i}", [P, PH, 256], F32) for i in range(NB)]
        best = [cx.sb(f"best{i}", [P, PH, 16], F32) for i in range(NB)]
        pos = [cx.sb(f"pos{i}", [P, PH, 16], U32) for i in range(NB)]
        posf = [cx.sb(f"posf{i}", [P, PH, 16], F32) for i in range(NB)]
        paf = [cx.sb(f"paf{i}", [P, PH, 16], F32) for i in range(NB)]
        pbf = [cx.sb(f"pbf{i}", [P, PH, 16], F32) for i in range(NB)]
        oh = cx.sb("oh", [P, PH, 16, 16], F32)
        oh2 = cx.sb("oh2", [P, PH, 16, 16], F32)
        i1s = [cx.sb(f"i1s{i}", [P, PH, 16], F32) for i in range(NB)]
        i2s = [cx.sb(f"i2s{i}", [P, PH, 16], F32) for i in range(NB)]
        eidf = [cx.sb(f"eidf{i}", [P, PH * 16], F32) for i in range(NB)]
        eid = [cx.sb(f"eid{i}", [P, PH * 16], U32) for i in range(2)]
        gate = [cx.sb(f"gate{i}", [P, PH, 16], F32) for i in range(2)]
        gst_ = [cx.sb(f"gstat{i}", [P, 16], F32) for i in range(NB)]
        yacc = [cx.sb(f"yacc{i}", [P, D], F32) for i in range(2)]
        st3 = [cx.sb(f"st3{i}", [P, 4], F32) for i in range(2)]
        NSL = 12
        GK = 8
        uvg = [cx.sb(f"uvg{i}", [P, 2 * D], BF16) for i in range(NSL)]
        NGR = PH * 16 // GK
        dgs = [cx.sb(f"dg{i}", [P, GK, P], BF16) for i in range(2)]
        sdg = [cx.sb(f"sdg{i}", [P, GK], F32) for i in range(2)]
        avg = [cx.sb(f"avg{i}", [P, GK], F32) for i in range(2)]
        trot = [0]
        gsl = [0, 0]

        def front_part(tt):
            r = 0
            ts = slice(tt * P, (tt + 1) * P)
            x2, h3_, xn_b, hT3, pq, ssb, mx, ix, ixf_ = x2t[tt % 2], h3[tt % 2], xn3[r], h3T[r], pqT[r], ssb_[r], mxs[r], ixs[r], ixf[r]
            cd, bst, ps_, psf, pa, pb_ = cand[r], best[r], pos[r], posf[r], paf[r], pbf[r]
            st = st3[tt % 2]
            k.dma('sp', x2.ap, x2_s.ap[ts, :], reads=[x2_s.res], writes=[x2.res])
            k.op('act', 'activation', R=[x2.res], W=[junk2.res, st.res], out=junk2.ap, in_=x2.ap, func=AF.Square, accum_out=st.ap[:, 0:1])
            k.op('act', 'activation', R=[st.res], W=[st.res], out=st.ap[:, 1:2], in_=st.ap[:, 0:1], func=AF.Ln, scale=1.0 / D, bias=EPS)
            k.op('act', 'activation', R=[st.res], W=[st.res], out=st.ap[:, 1:2], in_=st.ap[:, 1:2], func=AF.Exp, scale=-0.5)
            k.op('dve', 'tensor_scalar', R=[x2.res, st.res], W=[xn_b.res], out=xn_b.ap, in0=x2.ap, scalar1=st.ap[:, 1:2], scalar2=None, op0=ALU.mult)
            k.op('dve', 'scalar_tensor_tensor', R=[x2.res, st.res, ffn_bc.res], W=[h3_.res], out=h3_.ap, in0=x2.ap, scalar=st.ap[:, 1:2],
                 in1=ffn_bc.ap, op0=ALU.mult, op1=ALU.mult)
            pb = banks[trot[0] % 2]
            trot[0] += 1
            pview = pb.ap.bitcast(BF16).rearrange("p (c t) -> p c t", c=DC)
            for c in range(DC):
                k.op('pe', 'transpose', R=[xn_b.res, ident_b.res], W=[pb.res], out=pview[:, c, :], in_=xn_b.ap[:, c * P:(c + 1) * P], identity=ident_b.ap)
            k.op('dve', 'tensor_tensor', R=[pb.res, g_ffn.res], W=[hT3.res], out=hT3.ap, in0=pview, in1=g_ffn.ap, op=ALU.mult)
            for bq in range(4):
                b = banks[2 + bq]
                for cc in range(4):
                    c = bq * 4 + cc
                    for dc in range(DC):
                        k.op('pe', 'matmul', R=[pwq.res, hT3.res], W=[b.res], out=b.ap[:, cc * P:(cc + 1) * P], lhsT=pwq.ap[:, dc, c * P:(c + 1) * P],
                             rhs=hT3.ap[:, dc, :], start=(dc == 0), stop=(dc == DC - 1))
                k.op('act', 'copy', R=[b.res], W=[pq.res], out=pq.ap[:, bq * 4:(bq + 1) * 4, :], in_=b.ap.rearrange("p (c t) -> p c t", c=4))
            for bq in range(4):
                b = banks[2 + (bq + 2) % 4 + 0] if False else banks[6 + bq % 2] if False else banks[2 + bq]
                for cc in range(4):
                    hp = bq * 4 + cc
                    k.op('pe', 'matmul', R=[pq.res, skT.res], W=[b.res], out=b.ap[:, cc * P:(cc + 1) * P], lhsT=pq.ap[:, hp, :], rhs=skT.ap[:, hp, :],
                         start=True, stop=True)
                k.op('act', 'copy', R=[b.res], W=[ssb.res], out=ssb.ap[:, bq * 4:(bq + 1) * 4, :], in_=b.ap.rearrange("p (c t) -> p c t", c=4))
            for hp in range(16):
                k.op('dve', 'max', R=[ssb.res], W=[mx.res], out=mx.ap[:, hp, 0:8], in_=ssb.ap[:, hp, :])
                k.op('dve', 'match_replace', R=[mx.res, ssb.res], W=[s2b_.res], out=s2b_.ap[:, hp, :], in_to_replace=mx.ap[:, hp, 0:8],
                     in_values=ssb.ap[:, hp, :], imm_value=NEG)
                k.op('dve', 'max', R=[s2b_.res], W=[mx.res], out=mx.ap[:, hp, 8:16], in_=s2b_.ap[:, hp, :])
                k.op('dve', 'max_index', R=[mx.res, ssb.res], W=[ix.res], out=ix.ap[:, hp, 0:8], in_max=mx.ap[:, hp, 0:8], in_values=ssb.ap[:, hp, :])
                k.op('dve', 'max_index', R=[mx.res, ssb.res], W=[ix.res], out=ix.ap[:, hp, 8:16], in_max=mx.ap[:, hp, 8:16], in_values=ssb.ap[:, hp, :])
            k.op('dve', 'tensor_copy', R=[ix.res], W=[ixf_.res], out=ixf_.ap, in_=ix.ap)
            mx4 = mx.ap.rearrange("p (h two) k -> p h two k", two=2)
            cd4 = cd.ap.rearrange("p h (a b) -> p h a b", a=16)
            k.op('dve', 'tensor_tensor', R=[mx.res], W=[cd.res], out=cd4, in0=bc_last(mx4[:, :, 0, :], 16), in1=bc_mid(mx4[:, :, 1, :], 2, 16), op=ALU.add)
            for hh in range(PH):
                k.op('dve', 'max', R=[cd.res], W=[bst.res], out=bst.ap[:, hh, 0:8], in_=cd.ap[:, hh, :])
                k.op('dve', 'match_replace', R=[bst.res, cd.res], W=[cand2.res], out=cand2.ap[:, hh, :], in_to_replace=bst.ap[:, hh, 0:8],
                     in_values=cd.ap[:, hh, :], imm_value=NEG)
                k.op('dve', 'max', R=[cand2.res], W=[bst.res], out=bst.ap[:, hh, 8:16], in_=cand2.ap[:, hh, :])
                k.op('dve', 'max_index', R=[bst.res, cd.res], W=[ps_.res], out=ps_.ap[:, hh, 0:8], in_max=bst.ap[:, hh, 0:8], in_values=cd.ap[:, hh, :])
                k.op('dve', 'max_index', R=[bst.res, cd.res], W=[ps_.res], out=ps_.ap[:, hh, 8:16], in_max=bst.ap[:, hh, 8:16], in_values=cd.ap[:, hh, :])
            gs_, gt_ = gst_[r], gate[tt % 2]
            k.op('dve', 'tensor_tensor', R=[bst.res], W=[gt_.res], out=gt_.ap, in0=bst.ap, in1=bc_last(bst.ap[:, :, 0], 16), op=ALU.subtract)
            k.op('act', 'activation', R=[gt_.res], W=[gt_.res], out=gt_.ap, in_=gt_.ap, func=AF.Exp)
            k.op('dve', 'tensor_reduce', R=[gt_.res], W=[gs_.res], out=gs_.ap[:, 0:8], in_=gt_.ap, axis=AX.X, op=ALU.add)
            k.op('dve', 'reciprocal', R=[gs_.res], W=[gs_.res], out=gs_.ap[:, 8:16], in_=gs_.ap[:, 0:8])
            k.op('dve', 'tensor_tensor', R=[gt_.res, gs_.res], W=[gt_.res], out=gt_.ap, in0=gt_.ap, in1=bc_last(gs_.ap[:, 8:16], 16), op=ALU.mult)
            k.op('dve', 'tensor_copy', R=[ps_.res], W=[psf.res], out=psf.ap, in_=ps_.ap)
            thr_bc = bc_mid(bc_mid(thr16.ap, 1, 16), 1, PH)
            iota_bc = bc_mid(bc_mid(iota16.ap, 1, 16), 1, PH)
            k.op('dve', 'tensor_tensor', R=[psf.res, thr16.res], W=[oh.res], out=oh.ap, in0=bc_last(psf.ap, 16), in1=thr_bc, op=ALU.is_ge)
            k.op('dve', 'tensor_reduce', R=[oh.res], W=[pa.res], out=pa.ap, in_=oh.ap, axis=AX.X, op=ALU.add)
            k.op('dve', 'scalar_tensor_tensor', R=[pa.res, psf.res], W=[pb_.res], out=pb_.ap, in0=pa.ap, scalar=-16.0, in1=psf.ap, op0=ALU.mult, op1=ALU.add)
            ixf4 = ixf_.ap.rearrange("p (h two) k -> p h two k", two=2)
            for which, pidx, dsti in ((0, pa, i1s[r]), (1, pb_, i2s[r])):
                k.op('dve', 'tensor_tensor', R=[pidx.res, iota16.res], W=[oh.res], out=oh.ap, in0=bc_last(pidx.ap, 16), in1=iota_bc, op=ALU.is_equal)
                k.op('dve', 'tensor_tensor', R=[oh.res, ixf_.res], W=[oh2.res], out=oh2.ap, in0=oh.ap, in1=bc_mid(ixf4[:, :, which, :], 2, 16), op=ALU.mult)
                k.op('dve', 'tensor_reduce', R=[oh2.res], W=[dsti.res], out=dsti.ap, in_=oh2.ap, axis=AX.X, op=ALU.add)
            ef, ei = eidf[r], eid[tt % 2]
            k.op('dve', 'scalar_tensor_tensor', R=[i1s[r].res, i2s[r].res], W=[ef.res], out=ef.ap, in0=i1s[r].ap.rearrange("p h k -> p (h k)"),
                 scalar=float(PK), in1=i2s[r].ap.rearrange("p h k -> p (h k)"), op0=ALU.mult, op1=ALU.add)
            k.op('dve', 'tensor_copy', R=[ef.res], W=[ei.res], out=ei.ap, in_=ef.ap)

        def expert_group(tt, kg):
            r = 0
            h3_, ei, gt_ = h3[tt % 2], eid[tt % 2], gate[tt % 2]
            gflat = gt_.ap.rearrange("p h k -> p (h k)")
            sd, av = sdg[kg % 2], avg[kg % 2]
            slots = []
            for kk in range(kg * GK, (kg + 1) * GK):
                u_ = uvg[gsl[0] % NSL]
                gsl[0] += 1
                slots.append(u_)
                k.dma('pool', u_.ap, uv_s.ap, reads=[ei.res, uv_s.res], writes=[u_.res],
                      indirect=bass.IndirectOffsetOnAxis(ap=ei.ap[:, kk:kk + 1], axis=0))
                k.op('dve', 'scalar_tensor_tensor', R=[u_.res, h3_.res], W=[junk2.res, sd.res], out=junk2.ap, in0=u_.ap[:, 0:D], scalar=1.0,
                     in1=h3_.ap, op0=ALU.mult, op1=ALU.mult, accum_out=sd.ap[:, kk - kg * GK:kk - kg * GK + 1])
            k.op('act', 'activation', R=[sd.res], W=[av.res], out=av.ap, in_=sd.ap, func=AF.Gelu)
            k.op('dve', 'tensor_tensor', R=[av.res, gt_.res], W=[av.res], out=av.ap, in0=av.ap, in1=gflat[:, kg * GK:(kg + 1) * GK], op=ALU.mult)
            dg = dgs[kg % 2]
            k.op('dve', 'tensor_tensor', R=[ident_b.res, av.res], W=[dg.res], out=dg.ap, in0=bc_mid(ident_b.ap, 1, GK), in1=bc_last(av.ap, P), op=ALU.mult)
            for i_, u_ in enumerate(slots):
                first, last = (kg == 0 and i_ == 0), (kg == NGR - 1 and i_ == GK - 1)
                for hf in range(2):
                    yb = banks[6 + hf]
                    k.op('pe', 'matmul', R=[dg.res, u_.res], W=[yb.res], out=yb.ap, lhsT=dg.ap[:, i_, :], rhs=u_.ap[:, D + hf * 512:D + (hf + 1) * 512],
                         start=first, stop=last)

        def tail_part(tt):
            r = 0
            ts = slice(tt * P, (tt + 1) * P)
            x2, ya, st = x2t[tt % 2], yacc[tt % 2], st3[tt % 2]
            for hf in range(2):
                yb = banks[6 + hf]
                k.op('dve', 'tensor_tensor', R=[yb.res, x2.res], W=[ya.res], out=ya.ap[:, hf * 512:(hf + 1) * 512], in0=yb.ap,
                     in1=x2.ap[:, hf * 512:(hf + 1) * 512], op=ALU.add)
            k.op('act', 'activation', R=[ya.res], W=[junk2.res, st.res], out=junk2.ap, in_=ya.ap, func=AF.Square, accum_out=st.ap[:, 2:3])
            k.op('act', 'activation', R=[st.res], W=[st.res], out=st.ap[:, 3:4], in_=st.ap[:, 2:3], func=AF.Ln, scale=1.0 / D, bias=EPS)
            k.op('act', 'activation', R=[st.res], W=[st.res], out=st.ap[:, 3:4], in_=st.ap[:, 3:4], func=AF.Exp, scale=-0.5)
            k.op('dve', 'scalar_tensor_tensor', R=[ya.res, st.res, fin_bc.res], W=[ya.res], out=ya.ap, in0=ya.ap, scalar=st.ap[:, 3:4], in1=fin_bc.ap,
                 op0=ALU.mult, op1=ALU.mult)
            o_ = ya
            k.dma('sp', out_d.ap[ts, :], o_.ap, reads=[o_.res], writes=[out_d.res])

        k.play(k.record(lambda: front_part(0)))
        for tt in range(NT):
            nxt = k.record(lambda: front_part(tt + 1)) if tt + 1 < NT else []
            per = -(-len(nxt) // NGR) if nxt else 0
            for kg in range(NGR):
                k.play(k.record(lambda: expert_group(tt, kg)))
                k.play(nxt[kg * per:(kg + 1) * per])
            k.play(k.record(lambda: tail_part(tt)))
        cx.pop()

    finals = []
    for b in (qkm_s, sbq_s, sbk_s, vm_s, og_s, vs_s, gates_s, mixed_s, x2_s, out_d):
        if b is out_d or b.res.name in debug:
            finals.extend(b.res.w)
    k.emit(finals)
    return nc, k


def prep_shared(inp):
    f = lambda a: np.ascontiguousarray(np.asarray(a, dtype=np.float32))
    g = lambda v: f(v).reshape(DC, P).T
    mixed_g = np.concatenate([f(inp['mlstm_norm_g'][0]), f(inp['sb_norm_g'][0])])
    gvec = np.stack([g(inp['mix_norm_g'][0]), g(inp['xattn_norm_g'][0]), g(inp['mem_norm_g'][0]),
                     g(inp['ffn_norm_g'][0]), g(mixed_g)], axis=1)
    sh = {
        'w_in': f(inp['w_in'][0]), 'w_out': f(inp['w_out'][0]), 'xattn_wq': f(inp['xattn_wq'][0]),
        'xattn_wkv': f(inp['xattn_wkv'][0]), 'xattn_wo': f(inp['xattn_wo'][0]), 'peer_wq': f(inp['peer_wq'][0]),
        'subkeysT': f(np.transpose(f(inp['peer_subkeys'][0]).reshape(16, P, P), (2, 0, 1))),
        'peer_u': f(inp['peer_u'][0]), 'peer_v': f(inp['peer_v'][0]),
        'gvec': f(gvec),
        'grow': f(np.stack([f(inp['ffn_norm_g'][0]), f(inp['final_norm_g'])])),
        'conv_wT': f(np.transpose(f(inp['conv_w'][0]).reshape(4, DC, P), (2, 1, 0))),
        'conv_bT': g(inp['conv_b'][0]),
        'gate_b': f(np.stack([f(inp['igate_b'][0]), f(inp['fgate_b'][0])], axis=1)),
    }
    return sh


_CACHE = {}


def kernel(**inputs):
    x = np.asarray(inputs['x'], dtype=np.float32)
    mem = np.asarray(inputs['mem'], dtype=np.float32)
    B, S, _ = x.shape
    sh = prep_shared(inputs)
    if S not in _CACHE:
        _CACHE[S] = build(S)[0]
    nc = _CACHE[S]
    in_maps = []
    for b in range(B):
        m = dict(sh)
        m['x'] = np.ascontiguousarray(x[b])
        m['mem'] = np.ascontiguousarray(mem[b])
        in_maps.append(m)
    res = run_bass_kernel_spmd(nc, in_maps, core_ids=list(range(B)))
    return np.stack([np.asarray(r['out'], dtype=np.float32) for r in res.results], axis=0)
```

```python
import contextlib
import os
import numpy as np
import concourse.bass as bass
import concourse.mybir as mybir
from concourse.bass_utils import run_bass_kernel_spmd

F32 = mybir.dt.float32
BF16 = mybir.dt.bfloat16
U32 = mybir.dt.uint32
I32 = mybir.dt.int32
F32R = mybir.dt.float32r
ALU = mybir.AluOpType
AF = mybir.ActivationFunctionType
AX = mybir.AxisListType

P = 128
D = 1024
DC = D // P
NH_M, HD_M = 4, 128
NH_S, HD_S = 8, 64
PROJ = 3592
C_QKM, C_VM, C_OM, C_I, C_F, C_QS, C_KS, C_VS = 0, 1024, 1536, 2048, 2052, 2056, 2568, 3080
MEM = 256
NXH, XHD = 4, 256
PH, PK, PTOP = 8, 128, 16
NEXP = PK * PK
EPS = 1e-6
NEG = -1.0e30


class Res:
    def __init__(self, name, dram=False):
        self.name = name
        self.w = []
        self.r = []
        self.dsem = None
        self.dcnt = 0
        self.dram = dram


class Buf:
    def __init__(self, ap, res):
        self.ap = ap
        self.res = res


class KB:
    ENG = ('sp', 'pe', 'dve', 'act', 'pool')

    def __init__(self, nc):
        self.nc = nc
        self.sem = {e: nc.alloc_semaphore(name='s_' + e) for e in self.ENG}
        self.cnt = {e: 0 for e in self.ENG}
        self.waited = {e: {} for e in self.ENG}
        self.prog = {e: [] for e in self.ENG}
        self.nsem = len(self.ENG)
        self.ninst = 0
        self.owners = []
        self.marks = []
        self.rec = None

    def _wait(self, e, deps):
        for d in deps:
            if d is None:
                continue
            s, v = d
            key = id(s)
            if self.waited[e].get(key, 0) >= v:
                continue
            self.waited[e][key] = v
            self.prog[e].append(lambda eng, s=s, v=v: eng.wait_ge(s, v))

    @staticmethod
    def _deps(reads, writes, dma_write=False):
        deps = []
        for r in reads:
            deps.extend(r.w)
        for w in writes:
            if not (dma_write and w.dram):
                deps.extend(w.w)
            deps.extend(w.r)
        return deps

    def op(self, e, meth, R=(), W=(), **kw):
        if self.rec is not None:
            self.rec.append(('op', (e, meth), dict(R=R, W=W, **kw)))
            return None
        deps = self._deps(R, W)
        if e == 'pe':
            deps = [d for d in deps if d is not None and d[0] is not self.sem['pe']]
        self._wait(e, deps)
        self.cnt[e] += 1
        self.ninst += 1
        sem = self.sem[e]
        ev = (sem, self.cnt[e])
        self.prog[e].append(lambda eng, meth=meth, kw=kw, sem=sem: getattr(eng, meth)(**kw).then_inc(sem, 1))
        for r in R:
            r.r.append(ev)
        for w in W:
            w.w = [ev]
            w.r = []
        return ev

    def dma(self, q, out, in_, reads=(), writes=(), indirect=None, **kw):
        if self.rec is not None:
            self.rec.append(('dma', (q, out, in_), dict(reads=reads, writes=writes, indirect=indirect, **kw)))
            return None
        self._wait(q, self._deps(reads, writes, dma_write=True))
        (dst,) = writes
        owner = dst if not dst.dram else [r for r in reads if not r.dram][0]
        if owner.dsem is None:
            owner.dsem = self.nc.alloc_semaphore(name='d_' + owner.name)
            self.nsem += 1
            self.owners.append(owner)
        owner.dcnt += 16
        self.ninst += 1
        ev = (owner.dsem, owner.dcnt)
        ds = owner.dsem
        if indirect is None:
            self.prog[q].append(lambda eng, out=out, in_=in_, kw=kw, ds=ds:
                                eng.dma_start(out=out, in_=in_, **kw).then_inc(ds, 16))
        else:
            self.prog[q].append(lambda eng, out=out, in_=in_, io=indirect, kw=kw, ds=ds:
                                eng.indirect_dma_start(out=out, out_offset=None, in_=in_, in_offset=io,
                                                       **kw).then_inc(ds, 16))
        for r in reads:
            r.r.append(ev)
        if dst.dram:
            dst.w = [x for x in dst.w if x[0] is not ds] + [ev]
        else:
            dst.w = [ev]
        dst.r = []
        return ev

    def record(self, fn):
        assert self.rec is None
        self.rec = []
        fn()
        out, self.rec = self.rec, None
        return out

    def play(self, items):
        for kind, a, kw in items:
            if kind == 'op':
                self.op(*a, **kw)
            else:
                self.dma(*a, **kw)

    def barrier(self):
        evs = [(self.sem[e], self.cnt[e]) for e in self.ENG if self.cnt[e] > 0]
        evs += [(o.dsem, o.dcnt) for o in self.owners]
        for e in self.ENG:
            self._wait(e, evs)

    def emit(self, final_events):
        self._wait('sp', final_events)
        prog = self.prog
        with self.nc.Block() as block:
            @block.sync
            def _(eng):
                for f in prog['sp']:
                    f(eng)

            @block.tensor
            def _(eng):
                for f in prog['pe']:
                    f(eng)

            @block.vector
            def _(eng):
                for f in prog['dve']:
                    f(eng)

            @block.scalar
            def _(eng):
                for f in prog['act']:
                    f(eng)

            @block.gpsimd
            def _(eng):
                for f in prog['pool']:
                    f(eng)


class Ctx:
    def __init__(self, nc, k):
        self.nc = nc
        self.k = k
        self.n = 0
        self.stack = [contextlib.ExitStack()]

    def push(self):
        self.stack.append(contextlib.ExitStack())

    def pop(self):
        self.k.marks.append(dict(self.k.cnt))
        self.k.barrier()
        self.stack.pop().close()

    def sb(self, name, shape, dt):
        self.n += 1
        t = self.stack[-1].enter_context(self.nc.sbuf_tensor(f"{name}_{self.n}", list(shape), dt))
        return Buf(t[:] if not hasattr(t, 'ap') else t.ap(), Res(f"{name}_{self.n}"))

    def dram(self, name, shape, dt, kind="Internal"):
        t = self.nc.dram_tensor(name, list(shape), dt, kind=kind)
        return Buf(t.ap(), Res(name, dram=True))


def bc_last(ap, n):
    return ap.unsqueeze(ap.ndim).to_broadcast(list(ap.shape) + [n])


def bc_mid(ap, axis, n):
    shp = list(ap.shape)
    shp.insert(axis, n)
    return ap.unsqueeze(axis).to_broadcast(shp)


def build(S, phases=('A', 'B', 'C', 'D'), debug=()):
    NT = S // P
    NG = S // 512
    assert S % 512 == 0
    nc = bass.Bass("TRN2", target_bir_lowering=False)
    k = KB(nc)
    cx = Ctx(nc, k)

    def dkind(name):
        return "ExternalOutput" if name in debug else "Internal"

    def din(name, shape, dt=F32):
        return Buf(nc.dram_tensor(name, list(shape), dt, kind="ExternalInput").ap(), Res(name, dram=True))

    x_d = din("x", [S, D])
    mem_d = din("mem", [MEM, D])
    w_in_d = din("w_in", [D, PROJ])
    w_out_d = din("w_out", [D, D])
    wq_d = din("xattn_wq", [D, D])
    wkv_d = din("xattn_wkv", [D, 2 * D])
    wo_d = din("xattn_wo", [D, D])
    pwq_d = din("peer_wq", [D, 2 * D])
    skT_d = din("subkeysT", [P, 16, P])
    pu_d = din("peer_u", [NEXP, D])
    pv_d = din("peer_v", [NEXP, D])
    gvec_d = din("gvec", [P, 5, DC])
    grow_d = din("grow", [2, D])
    convw_d = din("conv_wT", [P, DC, 4])
    convb_d = din("conv_bT", [P, DC])
    gateb_d = din("gate_b", [4, 2])
    out_d = Buf(nc.dram_tensor("out", [S, D], F32, kind="ExternalOutput").ap(), Res("out", dram=True))

    qkm_s = cx.dram("qkm_s", [DC, P, S], BF16, kind=dkind("qkm_s"))
    sbq_s = cx.dram("sbq_s", [NH_S * HD_S, S], BF16, kind=dkind("sbq_s"))
    sbk_s = cx.dram("sbk_s", [NH_S * HD_S, S], BF16, kind=dkind("sbk_s"))
    vm_s = cx.dram("vm_s", [S, NH_M * HD_M], BF16, kind=dkind("vm_s"))
    og_s = cx.dram("og_s", [S, NH_M * HD_M], BF16, kind=dkind("og_s"))
    vs_s = cx.dram("vs_s", [S, NH_S * HD_S], BF16, kind=dkind("vs_s"))
    gates_s = cx.dram("gates_s", [4, 2, S], F32, kind=dkind("gates_s"))
    mixed_s = cx.dram("mixed_s", [S, D], BF16, kind=dkind("mixed_s"))
    x2_s = cx.dram("x2_s", [S, D], F32, kind=dkind("x2_s"))
    uv_s = cx.dram("uv_s", [NEXP, 2 * D], BF16)

    banks = []
    for i in range(8):
        t = nc.alloc_psum_tensor(f"bank{i}", [P, 512], F32)
        banks.append(Buf(t.ap(), Res(f"bank{i}")))

    ident_b = cx.sb("ident_b", [P, P], BF16)
    ident_f = cx.sb("ident_f", [P, P], F32)
    for idt in (ident_b, ident_f):
        k.op('pool', 'memset', W=[idt.res], ap=idt.ap, constant=1.0)
        k.op('pool', 'affine_select', R=[idt.res], W=[idt.res], out=idt.ap, in_=idt.ap, pattern=[[-1, P]],
             compare_op=ALU.is_equal, fill=0.0, base=0, channel_multiplier=1)
    gvec = cx.sb("gvec", [P, 5, DC], F32)
    k.dma('sp', gvec.ap, gvec_d.ap, writes=[gvec.res])
    convw = cx.sb("convw", [P, DC, 4], F32)
    k.dma('sp', convw.ap, convw_d.ap, writes=[convw.res])
    convb = cx.sb("convb", [P, DC], F32)
    k.dma('sp', convb.ap, convb_d.ap, writes=[convb.res])
    gateb = cx.sb("gateb", [4, 2], F32)
    k.dma('sp', gateb.ap, gateb_d.ap, writes=[gateb.res])

    STG = 2048
    stage = [cx.sb(f"stage{i}", [P, STG], F32) for i in range(2)]
    stage_i = [0]

    def load_cast(dst_aps, src_aps, dst_res, cast_engs=('dve', 'pool')):
        for dst, src in zip(dst_aps, src_aps):
            st = stage[stage_i[0] % 2]
            n = int(np.prod(src.shape[1:]))
            sview = st.ap[:, 0:n]
            if len(src.shape) == 3:
                sview = sview.rearrange("p (a b) -> p a b", a=src.shape[1])
            k.dma('sp', sview, src, writes=[st.res])
            eng = cast_engs[stage_i[0] % len(cast_engs)]
            k.op(eng, 'tensor_copy', R=[st.res], W=[dst_res], out=dst, in_=sview)
            stage_i[0] += 1

    def make_gbc(idx, name):
        g = cx.sb(name, [P, DC, P], F32)
        k.op('dve', 'tensor_copy', R=[gvec.res], W=[g.res], out=g.ap, in_=bc_last(gvec.ap[:, idx, :], P))
        return g

    def rms_stats(ss, rstd, n_cols):
        k.op('act', 'activation', R=[ss.res], W=[rstd.res], out=rstd.ap[:, 0:n_cols], in_=ss.ap[:, 0:n_cols],
             func=AF.Ln, scale=1.0 / D, bias=EPS)
        k.op('act', 'activation', R=[rstd.res], W=[rstd.res], out=rstd.ap[:, 0:n_cols], in_=rstd.ap[:, 0:n_cols],
             func=AF.Exp, scale=-0.5)

    def emit_T():
        RT = 2
        tin_u = [cx.sb(f"tin_u{i}", [P, RT, D], F32) for i in range(2)]
        tin_v = [cx.sb(f"tin_v{i}", [P, RT, D], F32) for i in range(2)]
        tout = [cx.sb(f"tout{i}", [P, RT, 2 * D], BF16) for i in range(2)]
        nchunk = NEXP // (P * RT)
        for c in range(nchunk):
            rs_ = slice(c * P * RT, (c + 1) * P * RT)
            tu, tv, to = tin_u[c % 2], tin_v[c % 2], tout[c % 2]
            k.dma('sp', tu.ap, pu_d.ap[rs_, :].rearrange("(p r) d -> p r d", r=RT), writes=[tu.res])
            k.dma('sp', tv.ap, pv_d.ap[rs_, :].rearrange("(p r) d -> p r d", r=RT), writes=[tv.res])
            k.op('dve', 'tensor_copy', R=[tu.res], W=[to.res], out=to.ap[:, :, 0:D], in_=tu.ap)
            k.op('act', 'copy', R=[tv.res], W=[to.res], out=to.ap[:, :, D:2 * D], in_=tv.ap)
            k.dma('pool', uv_s.ap[rs_, :].rearrange("(p r) d -> p r d", r=RT), to.ap, reads=[to.res], writes=[uv_s.res])

    defer_T = ('C' in phases) and ('D' in phases)
    if 'D' in phases and not defer_T:
        cx.push()
        emit_T()
        cx.pop()

    if 'A' in phases:
        cx.push()
        gst = [cx.sb(f"gst{i}", [4, 2, 512], F32) for i in range(2)]
        w_in = cx.sb("w_in", [P, DC, PROJ], BF16)
        load_cast([w_in.ap[:, c, a:min(a + STG, PROJ)] for c in range(DC) for a in range(0, PROJ, STG)],
                  [w_in_d.ap[c * P:(c + 1) * P, a:min(a + STG, PROJ)] for c in range(DC) for a in range(0, PROJ, STG)], w_in.res)
        g_mix = make_gbc(0, "g_mix")
        xg = [cx.sb(f"xg{i}", [P, 4, D], F32) for i in range(2)]
        xn = cx.sb("xn", [P, 4, D], BF16)
        junk = cx.sb("junk", [P, D], F32)
        ssq = [cx.sb(f"ssq{i}", [P, 4], F32) for i in range(2)]
        rstd = [cx.sb(f"rstd{i}", [P, 4], F32) for i in range(2)]
        hT = [cx.sb(f"hT{i}", [P, DC, 512], BF16) for i in range(2)]
        pre = cx.sb("pre", [P, DC, 3 + 512], F32)
        cacc = [cx.sb(f"cacc{i}", [P, 512], F32) for i in range(2)]
        qkm_o = [cx.sb(f"qkm_o{i}", [P, 512], BF16) for i in range(3)]
        sb_o = [cx.sb(f"sb_o{i}", [P, 512], BF16) for i in range(3)]
        tm_o = [cx.sb(f"tm_o{i}", [P, 512], BF16) for i in range(4)]
        k.op('pool', 'memset', W=[pre.res], ap=pre.ap[:, :, 0:3], constant=0.0)
        rot = {'mm': 0, 'q': 0, 's': 0, 't': 0}
        MMB = [2, 3, 4, 5, 6, 7]

        def mm_bank():
            b = banks[MMB[rot['mm'] % len(MMB)]]
            rot['mm'] += 1
            return b

        def load_x(g):
            xb = xg[g % 2]
            k.dma('sp', xb.ap, x_d.ap[g * 512:(g + 1) * 512, :].rearrange("(a p) d -> p a d", p=P), writes=[xb.res])

        def a_stage1(g):
            xb, ss, rs, hTg = xg[g % 2], ssq[g % 2], rstd[g % 2], hT[g % 2]
            gs = slice(g * 512, (g + 1) * 512)
            for tl in range(4):
                k.op('act', 'activation', R=[xb.res], W=[junk.res, ss.res], out=junk.ap, in_=xb.ap[:, tl, :],
                     func=AF.Square, accum_out=ss.ap[:, tl:tl + 1])
            rms_stats(ss, rs, 4)
            for tl in range(4):
                k.op('dve', 'tensor_scalar', R=[xb.res, rs.res], W=[xn.res], out=xn.ap[:, tl, :], in0=xb.ap[:, tl, :],
                     scalar1=rs.ap[:, tl:tl + 1], scalar2=None, op0=ALU.mult)
            for tl in range(4):
                pb = banks[tl % 2]
                pview = pb.ap.bitcast(BF16).rearrange("p (c t) -> p c t", c=DC)
                for c in range(DC):
                    k.op('pe', 'transpose', R=[xn.res, ident_b.res], W=[pb.res], out=pview[:, c, :],
                         in_=xn.ap[:, tl, c * P:(c + 1) * P], identity=ident_b.ap)
                k.op('dve', 'tensor_tensor', R=[pb.res, g_mix.res], W=[hTg.res], out=hTg.ap[:, :, tl * P:(tl + 1) * P],
                     in0=pview, in1=g_mix.ap, op=ALU.mult)


        def a_stage2(g):
            hTg = hT[g % 2]
            gs = slice(g * 512, (g + 1) * 512)
            def fm_matmul(col0, M):
                b = mm_bank()
                for c in range(DC):
                    k.op('pe', 'matmul', R=[w_in.res, hTg.res], W=[b.res], out=b.ap[0:M, :], lhsT=w_in.ap[:, c, col0:col0 + M],
                         rhs=hTg.ap[:, c, :], start=(c == 0), stop=(c == DC - 1))
                return b

            for c in range(DC):
                b = fm_matmul(C_QKM + c * P, P)
                k.op('act', 'copy', R=[b.res], W=[pre.res], out=pre.ap[:, c, 3:515], in_=b.ap)
            for c in range(DC):
                ca = cacc[c % 2]
                k.op('dve', 'tensor_scalar', R=[pre.res, convw.res, convb.res], W=[ca.res], out=ca.ap, in0=pre.ap[:, c, 0:512],
                     scalar1=convw.ap[:, c, 0:1], scalar2=convb.ap[:, c:c + 1], op0=ALU.mult, op1=ALU.add)
                for j in range(1, 4):
                    k.op('dve', 'scalar_tensor_tensor', R=[pre.res, convw.res, ca.res], W=[ca.res], out=ca.ap,
                         in0=pre.ap[:, c, j:j + 512], scalar=convw.ap[:, c, j:j + 1], in1=ca.ap, op0=ALU.mult, op1=ALU.add)
                qo = qkm_o[rot['q'] % 3]
                rot['q'] += 1
                k.op('act', 'activation', R=[ca.res], W=[qo.res], out=qo.ap, in_=ca.ap, func=AF.Silu)
                k.dma('pool', qkm_s.ap[c, :, gs], qo.ap, reads=[qo.res], writes=[qkm_s.res])
            k.op('dve', 'tensor_copy', R=[pre.res], W=[pre.res], out=pre.ap[:, :, 0:3], in_=pre.ap[:, :, 512:515])
            for col0, dst in ((C_QS, sbq_s), (C_KS, sbk_s)):
                for c in range(4):
                    b = fm_matmul(col0 + c * P, P)
                    so = sb_o[rot['s'] % 3]
                    rot['s'] += 1
                    k.op('act', 'copy', R=[b.res], W=[so.res], out=so.ap, in_=b.ap)
                    k.dma('pool', dst.ap[c * P:(c + 1) * P, gs], so.ap, reads=[so.res], writes=[dst.res])
            gsb = gst[g % 2]
            for gi, col0 in ((0, C_I), (1, C_F)):
                b = fm_matmul(col0, 4)
                k.op('act', 'activation', R=[b.res, gateb.res], W=[gsb.res], out=gsb.ap[:, gi, :], in_=b.ap[0:4, :],
                     func=AF.Identity, bias=gateb.ap[:, gi:gi + 1])
            k.dma('pool', gates_s.ap[:, :, gs], gsb.ap, reads=[gsb.res], writes=[gates_s.res])
            for tl in range(4):
                tt = g * 4 + tl
                for col0, dst, fn in ((C_VM, vm_s, None), (C_OM, og_s, AF.Sigmoid), (C_VS, vs_s, None)):
                    b = mm_bank()
                    for c in range(DC):
                        k.op('pe', 'matmul', R=[w_in.res, hTg.res], W=[b.res], out=b.ap, lhsT=hTg.ap[:, c, tl * P:(tl + 1) * P],
                             rhs=w_in.ap[:, c, col0:col0 + 512], start=(c == 0), stop=(c == DC - 1))
                    to = tm_o[rot['t'] % 4]
                    rot['t'] += 1
                    if fn is None:
                        k.op('dve', 'tensor_copy', R=[b.res], W=[to.res], out=to.ap, in_=b.ap)
                    else:
                        k.op('act', 'activation', R=[b.res], W=[to.res], out=to.ap, in_=b.ap, func=fn)
                    k.dma('pool', dst.ap[tt * P:(tt + 1) * P, :], to.ap, reads=[to.res], writes=[dst.res])

        load_x(0)
        if NG > 1:
            load_x(1)
        k.play(k.record(lambda: a_stage1(0)))
        for g in range(NG):
            if g + 2 < NG:
                load_x(g + 2)
            if g + 1 < NG:
                k.play(k.record(lambda: a_stage1(g + 1)))
            k.play(k.record(lambda: a_stage2(g)))
        cx.pop()

    if 'B' in phases:
        cx.push()
        KSC = float(HD_M) ** -0.5
        g4 = cx.sb("g4", [4, 2, S], F32)
        k.dma('sp', g4.ap, gates_s.ap, reads=[gates_s.res], writes=[g4.res])
        t2 = cx.sb("t2", [4, S], F32)
        k.op('act', 'activation', R=[g4.res], W=[t2.res], out=t2.ap, in_=g4.ap[:, 1, :], func=AF.Exp, scale=-1.0)
        k.op('act', 'activation', R=[t2.res], W=[t2.res], out=t2.ap, in_=t2.ap, func=AF.Ln, bias=1.0)
        ones4 = cx.sb("ones4", [4, S], F32)
        k.op('pool', 'memset', W=[ones4.res], ap=ones4.ap, constant=1.0)
        csum = cx.sb("csum", [4, S], F32)
        k.op('dve', 'tensor_tensor_scan', R=[ones4.res, t2.res], W=[csum.res], out=csum.ap, data0=ones4.ap, data1=t2.ap,
             initial=0.0, op0=ALU.mult, op1=ALU.add)
        mi = cx.sb("mi", [4, 1], F32)
        k.op('dve', 'tensor_reduce', R=[g4.res], W=[mi.res], out=mi.ap, in_=g4.ap[:, 0, :], axis=AX.X, op=ALU.max)
        crow = cx.sb("crow", [4, S], F32)
        k.op('dve', 'scalar_tensor_tensor', R=[g4.res, mi.res, csum.res], W=[crow.res], out=crow.ap, in0=g4.ap[:, 0, :],
             scalar=mi.ap[:, 0:1], in1=csum.ap, op0=ALU.subtract, op1=ALU.add)
        brow = cx.sb("brow", [4, S], F32)
        k.op('dve', 'tensor_scalar', R=[csum.res], W=[brow.res], out=brow.ap, in0=csum.ap, scalar1=-1.0, scalar2=None, op0=ALU.mult)
        em = cx.sb("em", [4, 1], F32)
        k.op('act', 'activation', R=[mi.res], W=[em.res], out=em.ap, in_=mi.ap, func=AF.Exp, scale=-1.0)
        sel = cx.sb("sel", [4, 4, P], F32)
        k.op('dve', 'tensor_copy', R=[ident_f.res], W=[sel.res], out=sel.ap, in_=bc_last(ident_f.ap[0:4, 0:4], P))
        dgem = cx.sb("dgem", [4, 4], F32)
        k.op('dve', 'tensor_scalar', R=[ident_f.res, em.res], W=[dgem.res], out=dgem.ap, in0=ident_f.ap[0:4, 0:4],
             scalar1=em.ap[:, 0:1], scalar2=None, op0=ALU.mult)
        ones4p = cx.sb("ones4p", [4, P], F32)
        k.op('pool', 'memset', W=[ones4p.res], ap=ones4p.ap, constant=1.0)
        ccol = cx.sb("ccol", [P, NT, 4], F32)
        embc = cx.sb("embc", [P, 4], F32)
        b2 = banks[2]
        for blk in range(NT):
            k.op('pe', 'matmul', R=[crow.res, ident_f.res], W=[b2.res], out=b2.ap[:, blk * 4:(blk + 1) * 4],
                 lhsT=crow.ap[:, blk * P:(blk + 1) * P], rhs=ident_f.ap[0:4, 0:4], start=True, stop=True)
        k.op('dve', 'tensor_copy', R=[b2.res], W=[ccol.res], out=ccol.ap, in_=b2.ap[:, 0:NT * 4].rearrange("p (n h) -> p n h", h=4))
        b3 = banks[3]
        k.op('pe', 'matmul', R=[ones4p.res, dgem.res], W=[b3.res], out=b3.ap[:, 0:4], lhsT=ones4p.ap, rhs=dgem.ap, start=True, stop=True)
        k.op('dve', 'tensor_copy', R=[b3.res], W=[embc.res], out=embc.ap, in_=b3.ap[:, 0:4])

        qTb = cx.sb("qTb", [P, S], BF16)
        kTb = cx.sb("kTb", [P, S], BF16)
        v1 = cx.sb("v1", [P, NT, HD_M + 1], BF16)
        k.op('pool', 'memset', W=[v1.res], ap=v1.ap, constant=1.0)
        Bbc = cx.sb("Bbc", [P, S], F32)
        Dt = [cx.sb(f"Dt{i}", [P, 512], F32) for i in range(2)]
        At = [cx.sb(f"At{i}", [P, 512], BF16) for i in range(2)]
        ogt = [cx.sb(f"ogt{i}", [P, 4, HD_M], BF16) for i in range(2)]
        hm = [cx.sb(f"hm{i}", [P, 4, HD_M], F32) for i in range(2)]
        hsq = cx.sb("hsq", [P, HD_M], F32)
        dn = [cx.sb(f"dn{i}", [P, 4], F32) for i in range(2)]
        hss = [cx.sb(f"hss{i}", [P, 4], F32) for i in range(2)]
        hrs = [cx.sb(f"hrs{i}", [P, 4], F32) for i in range(2)]
        mo = [cx.sb(f"mo{i}", [P, 4, HD_M], BF16) for i in range(2)]
        it = 0
        ch = 0
        for h in range(int(os.environ.get('NHB', NH_M))):
            k.dma('sp', qTb.ap, qkm_s.ap[h], reads=[qkm_s.res], writes=[qTb.res])
            k.dma('sp', kTb.ap, qkm_s.ap[4 + h], reads=[qkm_s.res], writes=[kTb.res])
            with nc.allow_non_contiguous_dma(reason="per-head v slices, 256B runs"):
                k.dma('sp', v1.ap[:, :, 0:HD_M], vm_s.ap[:, h * HD_M:(h + 1) * HD_M].rearrange("(n p) d -> p n d", p=P),
                      reads=[vm_s.res], writes=[v1.res])
            for c5 in range(S // 512):
                bb = banks[2 + c5 % 2]
                k.op('pe', 'matmul', R=[sel.res, brow.res], W=[bb.res], out=bb.ap, lhsT=sel.ap[:, h, :],
                     rhs=brow.ap[:, c5 * 512:(c5 + 1) * 512], start=True, stop=True)
                k.op('act', 'copy', R=[bb.res], W=[Bbc.res], out=Bbc.ap[:, c5 * 512:(c5 + 1) * 512], in_=bb.ap)
            for q in range(NG):
                og = ogt[ch % 2]
                with nc.allow_non_contiguous_dma(reason="per-head o-gate slices"):
                    k.dma('sp', og.ap, og_s.ap[q * 512:(q + 1) * 512, h * HD_M:(h + 1) * HD_M].rearrange("(a p) d -> p a d", p=P),
                          reads=[og_s.res], writes=[og.res])
                accs = [banks[4 + i] for i in range(4)]
                steps = []
                for j in range(4 * q + 4):
                    t0 = max(P * j, 512 * q)
                    steps.append(dict(j=j, t0=t0, n=512 * (q + 1) - t0, zb=banks[it % 2], dt_=Dt[it % 2], at_=At[it % 2]))
                    it += 1

                def bstage1(st):
                    j, t0, n, zb, dt_, at_ = st['j'], st['t0'], st['n'], st['zb'], st['dt_'], st['at_']
                    k.op('pe', 'matmul', R=[kTb.res, qTb.res], W=[zb.res], out=zb.ap[:, 0:n], lhsT=kTb.ap[:, j * P:(j + 1) * P],
                         rhs=qTb.ap[:, t0:t0 + n], start=True, stop=True)
                    k.op('act', 'activation', R=[Bbc.res, ccol.res], W=[dt_.res], out=dt_.ap[:, 0:n], in_=Bbc.ap[:, t0:t0 + n],
                         func=AF.Exp, bias=ccol.ap[:, j, h:h + 1])
                    if j >= 4 * q:
                        k.op('pool', 'affine_select', R=[dt_.res], W=[dt_.res], out=dt_.ap[:, 0:P], in_=dt_.ap[:, 0:P],
                             pattern=[[1, P]], compare_op=ALU.is_ge, fill=0.0, base=0, channel_multiplier=-1)
                    k.op('dve', 'scalar_tensor_tensor', R=[zb.res, dt_.res], W=[at_.res], out=at_.ap[:, 0:n], in0=zb.ap[:, 0:n],
                         scalar=KSC, in1=dt_.ap[:, 0:n], op0=ALU.mult, op1=ALU.mult)

                def bstage2(st):
                    j, t0, at_ = st['j'], st['t0'], st['at_']
                    for i in range(max(j, 4 * q), 4 * q + 4):
                        o = i * P - t0
                        acc = accs[i - 4 * q]
                        k.op('pe', 'matmul', R=[at_.res, v1.res], W=[acc.res], out=acc.ap[:, 0:HD_M + 1], lhsT=at_.ap[:, o:o + P],
                             rhs=v1.ap[:, j, :], start=(j == 0), stop=(j == i))

                ns = len(steps)
                for x_ in range(ns + 1):
                    if x_ < ns:
                        bstage1(steps[x_])
                    if 0 <= x_ - 1 < ns:
                        bstage2(steps[x_ - 1])
                d_, ss_, rs_, hm_, mo_ = dn[ch % 2], hss[ch % 2], hrs[ch % 2], hm[ch % 2], mo[ch % 2]
                for i in range(4):
                    acc = accs[i]
                    k.op('act', 'activation', R=[acc.res], W=[d_.res], out=d_.ap[:, i:i + 1], in_=acc.ap[:, HD_M:HD_M + 1], func=AF.Abs)
                k.op('dve', 'tensor_scalar', R=[d_.res, embc.res], W=[d_.res], out=d_.ap, in0=d_.ap, scalar1=embc.ap[:, h:h + 1],
                     scalar2=None, op0=ALU.max)
                k.op('dve', 'reciprocal', R=[d_.res], W=[d_.res], out=d_.ap, in_=d_.ap)
                for i in range(4):
                    acc = accs[i]
                    k.op('dve', 'scalar_tensor_tensor', R=[acc.res, d_.res, og.res], W=[hm_.res], out=hm_.ap[:, i, :], in0=acc.ap[:, 0:HD_M],
                         scalar=d_.ap[:, i:i + 1], in1=og.ap[:, i, :], op0=ALU.mult, op1=ALU.mult)
                    k.op('dve', 'scalar_tensor_tensor', R=[hm_.res], W=[hsq.res, ss_.res], out=hsq.ap, in0=hm_.ap[:, i, :], scalar=1.0, in1=hm_.ap[:, i, :],
                         op0=ALU.mult, op1=ALU.mult, accum_out=ss_.ap[:, i:i + 1])
                k.op('act', 'activation', R=[ss_.res], W=[rs_.res], out=rs_.ap, in_=ss_.ap, func=AF.Ln, scale=1.0 / HD_M, bias=EPS)
                k.op('act', 'activation', R=[rs_.res], W=[rs_.res], out=rs_.ap, in_=rs_.ap, func=AF.Exp, scale=-0.5)
                k.op('dve', 'tensor_tensor', R=[hm_.res, rs_.res], W=[mo_.res], out=mo_.ap, in0=hm_.ap, in1=bc_last(rs_.ap, HD_M), op=ALU.mult)
                with nc.allow_non_contiguous_dma(reason="per-head mixed slices"):
                    k.dma('pool', mixed_s.ap[q * 512:(q + 1) * 512, h * HD_M:(h + 1) * HD_M].rearrange("(a p) d -> p a d", p=P), mo_.ap,
                          reads=[mo_.res], writes=[mixed_s.res])
                ch += 1
        cx.pop()

    if 'C' in phases:
        cx.push()
        SSC = float(HD_S) ** -0.5
        tri = cx.sb("tri", [P, P], F32)
        k.op('pool', 'memset', W=[tri.res], ap=tri.ap, constant=1.0)
        k.op('pool', 'affine_select', R=[tri.res], W=[tri.res], out=tri.ap, in_=tri.ap, pattern=[[-1, P]],
             compare_op=ALU.is_ge, fill=0.0, base=0, channel_multiplier=1)
        onesf = cx.sb("onesf", [P, P], F32)
        k.op('pool', 'memset', W=[onesf.res], ap=onesf.ap, constant=1.0)
        sq_ = cx.sb("sq_", [HD_S, S], BF16)
        sk_ = cx.sb("sk_", [HD_S, S], BF16)
        skn = cx.sb("skn", [HD_S, S], BF16)
        sv_ = cx.sb("sv_", [P, NT, HD_S], BF16)
        e1 = [cx.sb(f"e1{i}", [P, 512], F32) for i in range(2)]
        sA = [cx.sb(f"sA{i}", [P, 512], BF16) for i in range(2)]
        raccs = [cx.sb(f"racc{i}", [P, 512], F32) for i in range(2)]
        os_ = [cx.sb(f"os{i}", [P, 4, HD_S], F32) for i in range(2)]
        osq = cx.sb("osq", [P, HD_S], F32)
        sss = [cx.sb(f"sss{i}", [P, 4], F32) for i in range(2)]
        srs = [cx.sb(f"srs{i}", [P, 4], F32) for i in range(2)]
        smo = [cx.sb(f"smo{i}", [P, 4, HD_S], BF16) for i in range(2)]
        it = 0
        ch = 0
        tlist = k.record(emit_T) if defer_T else []
        tsteps = NH_S * sum(4 * q + 6 for q in range(NG))
        tper = -(-len(tlist) // tsteps) if tlist else 0
        tpos = [0]
        for h in range(NH_S):
            k.dma('sp', sq_.ap, sbq_s.ap[h * HD_S:(h + 1) * HD_S, :], reads=[sbq_s.res], writes=[sq_.res])
            k.dma('sp', sk_.ap, sbk_s.ap[h * HD_S:(h + 1) * HD_S, :], reads=[sbk_s.res], writes=[sk_.res])
            with nc.allow_non_contiguous_dma(reason="per-head v slices, 128B runs"):
                k.dma('sp', sv_.ap, vs_s.ap[:, h * HD_S:(h + 1) * HD_S].rearrange("(n p) d -> p n d", p=P),
                      reads=[vs_s.res], writes=[sv_.res])
            k.op('dve', 'tensor_scalar', R=[sk_.res], W=[skn.res], out=skn.ap, in0=sk_.ap, scalar1=-SSC, scalar2=None, op0=ALU.mult)
            for q in range(NG):
                accs = [banks[4 + i] for i in range(4)]
                for rb in raccs:
                    k.op('pool', 'memset', W=[rb.res], ap=rb.ap, constant=0.0)
                steps = []
                for j in range(4 * q + 3, -1, -1):
                    c0 = max(P * j - 512 * q, 0)
                    steps.append(dict(j=j, c0=c0, t0=512 * q + c0, n=512 - c0, diag=(j >= 4 * q), first=(j == 4 * q + 3),
                                      zb=banks[it % 2], cp=banks[2 + it % 2], e_=e1[it % 2], a_=sA[it % 2],
                                      rc=raccs[len(steps) % 2], rn=raccs[(len(steps) + 1) % 2]))
                    it += 1

                def stage1(st):
                    j, t0, n, zb, e_ = st['j'], st['t0'], st['n'], st['zb'], st['e_']
                    k.op('pe', 'matmul', R=[sk_.res, sq_.res], W=[zb.res], out=zb.ap[:, 0:n], lhsT=sk_.ap[:, j * P:(j + 1) * P],
                         rhs=sq_.ap[:, t0:t0 + n], start=True, stop=True)
                    k.op('act', 'activation', R=[zb.res], W=[e_.res], out=e_.ap[:, 0:n], in_=zb.ap[:, 0:n], func=AF.Exp, scale=SSC)
                    k.op('act', 'activation', R=[e_.res], W=[e_.res], out=e_.ap[:, 0:n], in_=e_.ap[:, 0:n], func=AF.Ln, bias=1.0)
                    if st['diag']:
                        k.op('pool', 'affine_select', R=[e_.res], W=[e_.res], out=e_.ap[:, 0:P], in_=e_.ap[:, 0:P], pattern=[[1, P]],
                             compare_op=ALU.is_ge, fill=0.0, base=-1, channel_multiplier=-1)

                def stage2(st):
                    j, c0, t0, n, cp, e_, a_ = st['j'], st['c0'], st['t0'], st['n'], st['cp'], st['e_'], st['a_']
                    k.op('pe', 'matmul', R=[tri.res, e_.res], W=[cp.res], out=cp.ap[:, 0:n], lhsT=tri.ap,
                         rhs=e_.ap[:, 0:n], start=True, stop=False)
                    rc, rn = st['rc'], st['rn']
                    if not st['first']:
                        k.op('pe', 'matmul', R=[onesf.res, rc.res], W=[cp.res], out=cp.ap[:, 0:n], lhsT=onesf.ap,
                             rhs=rc.ap[:, c0:512], start=False, stop=False)
                    k.op('pe', 'matmul', R=[skn.res, sq_.res], W=[cp.res], out=cp.ap[:, 0:n], lhsT=skn.ap[:, j * P:(j + 1) * P],
                         rhs=sq_.ap[:, t0:t0 + n], start=False, stop=True)
                    if j > 0:
                        k.op('pool', 'tensor_tensor', R=[rc.res, e_.res], W=[rn.res], out=rn.ap[:, c0:512], in0=rc.ap[:, c0:512],
                             in1=e_.ap[:, 0:n], op=ALU.add)
                    k.op('act', 'activation', R=[cp.res], W=[a_.res], out=a_.ap[:, 0:n], in_=cp.ap[:, 0:n], func=AF.Exp, scale=-1.0)
                    if st['diag']:
                        k.op('pool', 'affine_select', R=[a_.res], W=[a_.res], out=a_.ap[:, 0:P], in_=a_.ap[:, 0:P], pattern=[[1, P]],
                             compare_op=ALU.is_ge, fill=0.0, base=-1, channel_multiplier=-1)

                def stage3(st):
                    j, t0, a_ = st['j'], st['t0'], st['a_']
                    for i in range(max(j, 4 * q), 4 * q + 4):
                        o = i * P - t0
                        acc = accs[i - 4 * q]
                        k.op('pe', 'matmul', R=[a_.res, sv_.res], W=[acc.res], out=acc.ap[:, 0:HD_S], lhsT=a_.ap[:, o:o + P],
                             rhs=sv_.ap[:, j, :], start=(j == i), stop=(j == 0))

                ns = len(steps)
                for x_ in range(ns + 2):
                    if x_ < ns:
                        stage1(steps[x_])
                    if 0 <= x_ - 1 < ns:
                        stage2(steps[x_ - 1])
                    if 0 <= x_ - 2 < ns:
                        stage3(steps[x_ - 2])
                    if tper:
                        k.play(tlist[tpos[0]:tpos[0] + tper])
                        tpos[0] += tper
                o_, ss_, rs_, mo_ = os_[ch % 2], sss[ch % 2], srs[ch % 2], smo[ch % 2]
                for i in range(4):
                    k.op('act', 'copy', R=[accs[i].res], W=[o_.res], out=o_.ap[:, i, :], in_=accs[i].ap[:, 0:HD_S])
                for i in range(4):
                    k.op('dve', 'scalar_tensor_tensor', R=[o_.res], W=[osq.res, ss_.res], out=osq.ap, in0=o_.ap[:, i, :], scalar=1.0, in1=o_.ap[:, i, :],
                         op0=ALU.mult, op1=ALU.mult, accum_out=ss_.ap[:, i:i + 1])
                k.op('act', 'activation', R=[ss_.res], W=[rs_.res], out=rs_.ap, in_=ss_.ap, func=AF.Ln, scale=1.0 / HD_S, bias=EPS)
                k.op('act', 'activation', R=[rs_.res], W=[rs_.res], out=rs_.ap, in_=rs_.ap, func=AF.Exp, scale=-0.5)
                k.op('dve', 'tensor_tensor', R=[o_.res, rs_.res], W=[mo_.res], out=mo_.ap, in0=o_.ap, in1=bc_last(rs_.ap, HD_S), op=ALU.mult)
                with nc.allow_non_contiguous_dma(reason="per-head mixed slices"):
                    k.dma('pool', mixed_s.ap[q * 512:(q + 1) * 512, 512 + h * HD_S:512 + (h + 1) * HD_S].rearrange("(a p) d -> p a d", p=P),
                          mo_.ap, reads=[mo_.res], writes=[mixed_s.res])
                ch += 1
        k.play(tlist[tpos[0]:])
        cx.pop()

    if 'D' in phases:
        cx.push()
        XSC = float(XHD) ** -0.5
        w_out = cx.sb("w_out", [P, DC, D], BF16)
        wq = cx.sb("wq", [P, DC, D], BF16)
        wo = cx.sb("wo", [P, DC, D], BF16)
        kmT = cx.sb("kmT", [P, DC, MEM], BF16)
        vmem = cx.sb("vmem", [P, 2, D], BF16)
        g_mixed = make_gbc(4, "g_mixed")
        g_xat = make_gbc(1, "g_xat")
        junk1 = cx.sb("junk1", [P, D], F32)

        def rows(wd, c, a, n):
            return wd.ap[c * P:(c + 1) * P, a:a + n]

        def norm_T(src, ss, rs, xn_b, gbc, dstT, ncol_off=0, tbank=None):
            k.op('act', 'activation', R=[src.res], W=[junk1.res, ss.res], out=junk1.ap, in_=src.ap, func=AF.Square, accum_out=ss.ap[:, 0:1])
            rms_stats(ss, rs, 1)
            k.op('dve', 'tensor_scalar', R=[src.res, rs.res], W=[xn_b.res], out=xn_b.ap, in0=src.ap, scalar1=rs.ap[:, 0:1], scalar2=None, op0=ALU.mult)
            transpose_T(xn_b, gbc, dstT, ncol_off, tbank)

        trot = [0]

        def transpose_T(src_b, gbc, dstT, ncol_off=0, tbank=None):
            pb = tbank if tbank is not None else banks[trot[0] % 2]
            trot[0] += 1
            pview = pb.ap.bitcast(BF16).rearrange("p (c t) -> p c t", c=DC)
            for c in range(DC):
                k.op('pe', 'transpose', R=[src_b.res, ident_b.res], W=[pb.res], out=pview[:, c, :], in_=src_b.ap[:, c * P:(c + 1) * P],
                     identity=ident_b.ap)
            k.op('dve', 'tensor_tensor', R=[pb.res, gbc.res], W=[dstT.res], out=dstT.ap[:, :, ncol_off:ncol_off + P], in0=pview, in1=gbc.ap,
                 op=ALU.mult)

        cx.push()
        wkv = cx.sb("wkv", [P, DC, 2 * D], BF16)
        load_cast([wkv.ap[:, c, :] for c in range(DC)], [rows(wkv_d, c, 0, 2 * D) for c in range(DC)], wkv.res)
        g_mem = make_gbc(2, "g_mem")
        memt = cx.sb("memt", [P, D], F32)
        memn = cx.sb("memn", [P, D], BF16)
        memT = cx.sb("memT", [P, DC, MEM], BF16)
        mss = cx.sb("mss", [P, 1], F32)
        mrs = cx.sb("mrs", [P, 1], F32)
        for mc in range(2):
            k.dma('sp', memt.ap, mem_d.ap[mc * P:(mc + 1) * P, :], writes=[memt.res])
            norm_T(memt, mss, mrs, memn, g_mem, memT, ncol_off=mc * P)
        for c in range(DC):
            b = banks[2 + c % 2]
            for dc in range(DC):
                k.op('pe', 'matmul', R=[wkv.res, memT.res], W=[b.res], out=b.ap[:, 0:MEM], lhsT=wkv.ap[:, dc, c * P:(c + 1) * P], rhs=memT.ap[:, dc, :],
                     start=(dc == 0), stop=(dc == DC - 1))
            k.op('act', 'copy', R=[b.res], W=[kmT.res], out=kmT.ap[:, c, :], in_=b.ap[:, 0:MEM])
        for mc in range(2):
            for hf in range(2):
                b = banks[4 + (mc * 2 + hf) % 2]
                for dc in range(DC):
                    k.op('pe', 'matmul', R=[wkv.res, memT.res], W=[b.res], out=b.ap, lhsT=memT.ap[:, dc, mc * P:(mc + 1) * P],
                         rhs=wkv.ap[:, dc, D + hf * 512:D + (hf + 1) * 512], start=(dc == 0), stop=(dc == DC - 1))
                k.op('dve', 'tensor_copy', R=[b.res], W=[vmem.res], out=vmem.ap[:, mc, hf * 512:(hf + 1) * 512], in_=b.ap)
        cx.pop()

        load_cast([w_out.ap[:, c, :] for c in range(DC)], [rows(w_out_d, c, 0, D) for c in range(DC)], w_out.res)
        load_cast([wq.ap[:, c, :] for c in range(DC)], [rows(wq_d, c, 0, D) for c in range(DC)], wq.res)
        load_cast([wo.ap[:, c, :] for c in range(DC)], [rows(wo_d, c, 0, D) for c in range(DC)], wo.res)

        NB = 2
        xt_ = [cx.sb(f"xt{i}", [P, D], F32) for i in range(NB)]
        mx_ = [cx.sb(f"mx{i}", [P, D], BF16) for i in range(NB)]
        mT_ = [cx.sb(f"mT{i}", [P, DC, P], BF16) for i in range(NB)]
        x1_ = [cx.sb(f"x1{i}", [P, D], F32) for i in range(NB)]
        x2_ = [cx.sb(f"x2{i}", [P, D], F32) for i in range(NB)]
        xnb = [cx.sb(f"xnb{i}", [P, D], BF16) for i in range(NB)]
        h2T = [cx.sb(f"h2T{i}", [P, DC, P], BF16) for i in range(NB)]
        qT_ = [cx.sb(f"qT{i}", [P, DC, P], BF16) for i in range(NB)]
        pex = [cx.sb(f"pex{i}", [P, NXH, MEM], F32) for i in range(NB)]
        pn_ = [cx.sb(f"pn{i}", [P, NXH, MEM], BF16) for i in range(NB)]
        pT_ = [cx.sb(f"pT{i}", [P, DC, P], BF16) for i in range(NB)]
        oT_ = [cx.sb(f"oT{i}", [P, DC, P], BF16) for i in range(NB)]
        st1 = [cx.sb(f"st1{i}", [P, 16], F32) for i in range(NB)]
        mmr = [0]
        PAIRS = [(2, 3), (4, 5), (6, 7)]

        PAIRS_P = {0: [(2, 3), (4, 5)], 1: [(6, 7)]}
        mmp = {0: 0, 1: 0}

        def pair(par):
            lst = PAIRS_P[par]
            p_ = lst[mmp[par] % len(lst)]
            mmp[par] += 1
            return banks[p_[0]], banks[p_[1]]

        def d1_tile(tt):
            r = tt % NB
            xt, mxt, mT, x1, x2, xn_b, hT2, qT, pe_, pn, pT, oT, st = (xt_[r], mx_[r], mT_[r], x1_[r], x2_[r], xnb[r], h2T[r], qT_[r], pex[r],
                                                                      pn_[r], pT_[r], oT_[r], st1[r])
            ts = slice(tt * P, (tt + 1) * P)
            k.dma('sp', xt.ap, x_d.ap[ts, :], writes=[xt.res])
            k.dma('sp', mxt.ap, mixed_s.ap[ts, :], reads=[mixed_s.res], writes=[mxt.res])
            transpose_T(mxt, g_mixed, mT, tbank=banks[tt % 2])
            ba, bb = pair(tt % 2)
            for hf, b in ((0, ba), (1, bb)):
                for dc in range(DC):
                    k.op('pe', 'matmul', R=[mT.res, w_out.res], W=[b.res], out=b.ap, lhsT=mT.ap[:, dc, :], rhs=w_out.ap[:, dc, hf * 512:(hf + 1) * 512],
                         start=(dc == 0), stop=(dc == DC - 1))
                k.op('dve', 'tensor_tensor', R=[b.res, xt.res], W=[x1.res], out=x1.ap[:, hf * 512:(hf + 1) * 512], in0=b.ap,
                     in1=xt.ap[:, hf * 512:(hf + 1) * 512], op=ALU.add)
            ssb = Buf(st.ap[:, 0:1], st.res)
            rsb = Buf(st.ap[:, 1:2], st.res)
            norm_T(x1, ssb, rsb, xn_b, g_xat, hT2, tbank=banks[tt % 2])
            ba, bb = pair(tt % 2)
            for c in range(DC):
                b = ba if c < 4 else bb
                for dc in range(DC):
                    k.op('pe', 'matmul', R=[wq.res, hT2.res], W=[b.res], out=b.ap[:, (c % 4) * P:(c % 4 + 1) * P], lhsT=wq.ap[:, dc, c * P:(c + 1) * P],
                         rhs=hT2.ap[:, dc, :], start=(dc == 0), stop=(dc == DC - 1))
            for hf, b in ((0, ba), (1, bb)):
                k.op('act', 'copy', R=[b.res], W=[qT.res], out=qT.ap[:, hf * 4:(hf + 1) * 4, :], in_=b.ap.rearrange("p (c t) -> p c t", c=4))
            ba, bb = pair(tt % 2)
            for hh in range(NXH):
                b = ba if hh < 2 else bb
                for c2 in range(2):
                    k.op('pe', 'matmul', R=[qT.res, kmT.res], W=[b.res], out=b.ap[:, (hh % 2) * MEM:(hh % 2 + 1) * MEM], lhsT=qT.ap[:, 2 * hh + c2, :],
                         rhs=kmT.ap[:, 2 * hh + c2, :], start=(c2 == 0), stop=(c2 == 1))
            for hf, b in ((0, ba), (1, bb)):
                k.op('dve', 'tensor_reduce', R=[b.res], W=[st.res], out=st.ap[:, 4 + 2 * hf:6 + 2 * hf], in_=b.ap.rearrange("p (h m) -> p h m", h=2),
                     axis=AX.X, op=ALU.max)
            k.op('dve', 'tensor_scalar', R=[st.res], W=[st.res], out=st.ap[:, 4:8], in0=st.ap[:, 4:8], scalar1=-XSC, scalar2=None, op0=ALU.mult)
            for hh in range(NXH):
                b = ba if hh < 2 else bb
                k.op('act', 'activation', R=[b.res, st.res], W=[pe_.res, st.res], out=pe_.ap[:, hh, :], in_=b.ap[:, (hh % 2) * MEM:(hh % 2 + 1) * MEM],
                     func=AF.Exp, scale=XSC, bias=st.ap[:, 4 + hh:5 + hh], accum_out=st.ap[:, 8 + hh:9 + hh])
            k.op('dve', 'reciprocal', R=[st.res], W=[st.res], out=st.ap[:, 12:16], in_=st.ap[:, 8:12])
            k.op('dve', 'tensor_tensor', R=[pe_.res, st.res], W=[pn.res], out=pn.ap, in0=pe_.ap, in1=bc_last(st.ap[:, 12:16], MEM), op=ALU.mult)
            pb = banks[tt % 2]
            pview = pb.ap.bitcast(BF16).rearrange("p (c t) -> p c t", c=DC)
            for hh in range(NXH):
                for mc in range(2):
                    k.op('pe', 'transpose', R=[pn.res, ident_b.res], W=[pb.res], out=pview[:, hh * 2 + mc, :], in_=pn.ap[:, hh, mc * P:(mc + 1) * P],
                         identity=ident_b.ap)
            k.op('act', 'copy', R=[pb.res], W=[pT.res], out=pT.ap, in_=pview)
            ba, bb = pair(tt % 2)
            for c in range(DC):
                b = ba if c < 4 else bb
                hh = c // 2
                for mc in range(2):
                    k.op('pe', 'matmul', R=[vmem.res, pT.res], W=[b.res], out=b.ap[:, (c % 4) * P:(c % 4 + 1) * P], lhsT=vmem.ap[:, mc, c * P:(c + 1) * P],
                         rhs=pT.ap[:, hh * 2 + mc, :], start=(mc == 0), stop=(mc == 1))
            for hf, b in ((0, ba), (1, bb)):
                k.op('act', 'copy', R=[b.res], W=[oT.res], out=oT.ap[:, hf * 4:(hf + 1) * 4, :], in_=b.ap.rearrange("p (c t) -> p c t", c=4))
            ba, bb = pair(tt % 2)
            for hf, b in ((0, ba), (1, bb)):
                for c in range(DC):
                    k.op('pe', 'matmul', R=[oT.res, wo.res], W=[b.res], out=b.ap, lhsT=oT.ap[:, c, :], rhs=wo.ap[:, c, hf * 512:(hf + 1) * 512],
                         start=(c == 0), stop=(c == DC - 1))
                k.op('dve', 'tensor_tensor', R=[b.res, x1.res], W=[x2.res], out=x2.ap[:, hf * 512:(hf + 1) * 512], in0=b.ap,
                     in1=x1.ap[:, hf * 512:(hf + 1) * 512], op=ALU.add)
            k.dma('pool', x2_s.ap[ts, :], x2.ap, reads=[x2.res], writes=[x2_s.res])

        def zip_play(a_, b_, nsl=8):
            pa, pb2 = -(-len(a_) // nsl) if a_ else 0, -(-len(b_) // nsl) if b_ else 0
            for i_ in range(nsl):
                k.play(a_[i_ * pa:(i_ + 1) * pa])
                k.play(b_[i_ * pb2:(i_ + 1) * pb2])

        cur = k.record(lambda: d1_tile(0))
        half = len(cur) // 2
        k.play(cur[:half])
        for tt in range(NT):
            nxt = k.record(lambda: d1_tile(tt + 1)) if tt + 1 < NT else []
            h2 = len(nxt) // 2
            zip_play(cur[half:], nxt[:h2])
            cur, half = nxt, h2
        cx.pop()

    if 'D' in phases:
        cx.push()
        pwq = cx.sb("pwq", [P, DC, 2 * D], BF16)
        load_cast([pwq.ap[:, c, :] for c in range(DC)], [pwq_d.ap[c * P:(c + 1) * P, :] for c in range(DC)], pwq.res)
        skT = cx.sb("skT", [P, 16, P], BF16)
        load_cast([skT.ap], [skT_d.ap], skT.res)
        g_ffn = make_gbc(3, "g_ffn")
        ffn_bc = cx.sb("ffn_bc", [P, D], F32)
        fin_bc = cx.sb("fin_bc", [P, D], F32)
        gt = grow_d.ap.tensor
        k.dma('sp', ffn_bc.ap, bass.AP(gt, 0, [[0, P], [1, D]]), writes=[ffn_bc.res])
        k.dma('sp', fin_bc.ap, bass.AP(gt, D, [[0, P], [1, D]]), writes=[fin_bc.res])
        iota_i = cx.sb("iota_i", [P, 16], I32)
        k.op('pool', 'iota', W=[iota_i.res], out=iota_i.ap, pattern=[[1, 16]], base=0, channel_multiplier=0)
        iota16 = cx.sb("iota16", [P, 16], F32)
        k.op('dve', 'tensor_copy', R=[iota_i.res], W=[iota16.res], out=iota16.ap, in_=iota_i.ap)
        thr16 = cx.sb("thr16", [P, 16], F32)
        k.op('dve', 'tensor_scalar', R=[iota16.res], W=[thr16.res], out=thr16.ap, in0=iota16.ap, scalar1=16.0, scalar2=15.5,
             op0=ALU.mult, op1=ALU.add)
        junk2 = cx.sb("junk2", [P, D], F32)

        NB = 1
        x2t = [cx.sb(f"x2t{i}", [P, D], F32) for i in range(2)]
        h3 = [cx.sb(f"h3{i}", [P, D], F32) for i in range(2)]
        xn3 = [cx.sb(f"xn3{i}", [P, D], BF16) for i in range(NB)]
        h3T = [cx.sb(f"h3T{i}", [P, DC, P], BF16) for i in range(NB)]
        pqT = [cx.sb(f"pqT{i}", [P, 16, P], BF16) for i in range(NB)]
        ssb_ = [cx.sb(f"ssb{i}", [P, 16, P], F32) for i in range(NB)]
        s2b_ = cx.sb("s2b", [P, 16, P], F32)
        cand2 = Buf(s2b_.ap.rearrange("p a b -> p (a b)").rearrange("p (h c) -> p h c", h=PH), s2b_.res)
        mxs = [cx.sb(f"mxs{i}", [P, 16, 16], F32) for i in range(NB)]
        ixs = [cx.sb(f"ixs{i}", [P, 16, 16], U32) for i in range(NB)]
        ixf = [cx.sb(f"ixf{i}", [P, 16, 16], F32) for i in range(NB)]
        cand = [cx.sb(f"cand{i}", [P, PH, 256], F32) for i in range(NB)]
        best = [cx.sb(f"best{i}", [P, PH, 16], F32) for i in range(NB)]
        pos = [cx.sb(f"pos{i}", [P, PH, 16], U32) for i in range(NB)]
        posf = [cx.sb(f"posf{i}", [P, PH, 16], F32) for i in range(NB)]
        paf = [cx.sb(f"paf{i}", [P, PH, 16], F32) for i in range(NB)]
        pbf = [cx.sb(f"pbf{i}", [P, PH, 16], F32) for i in range(NB)]
        oh = cx.sb("oh", [P, PH, 16, 16], F32)
        oh2 = cx.sb("oh2", [P, PH, 16, 16], F32)
        i1s = [cx.sb(f"i1s{i}", [P, PH, 16], F32) for i in range(NB)]
        i2s = [cx.sb(f"i2s{i}", [P, PH, 16], F32) for i in range(NB)]
        eidf = [cx.sb(f"eidf{i}", [P, PH * 16], F32) for i in range(NB)]
        eid = [cx.sb(f"eid{i}", [P, PH * 16], U32) for i in range(2)]
        gate = [cx.sb(f"gate{i}", [P, PH, 16], F32) for i in range(2)]
        gst_ = [cx.sb(f"gstat{i}", [P, 16], F32) for i in range(NB)]
        yacc = [cx.sb(f"yacc{i}", [P, D], F32) for i in range(2)]
        st3 = [cx.sb(f"st3{i}", [P, 4], F32) for i in range(2)]
        NSL = 12
        GK = 8
        uvg = [cx.sb(f"uvg{i}", [P, 2 * D], BF16) for i in range(NSL)]
        NGR = PH * 16 // GK
        dgs = [cx.sb(f"dg{i}", [P, GK, P], BF16) for i in range(2)]
        sdg = [cx.sb(f"sdg{i}", [P, GK], F32) for i in range(2)]
        avg = [cx.sb(f"avg{i}", [P, GK], F32) for i in range(2)]
        trot = [0]
        gsl = [0, 0]

        def front_part(tt):
            r = 0
            ts = slice(tt * P, (tt + 1) * P)
            x2, h3_, xn_b, hT3, pq, ssb, mx, ix, ixf_ = x2t[tt % 2], h3[tt % 2], xn3[r], h3T[r], pqT[r], ssb_[r], mxs[r], ixs[r], ixf[r]
            cd, bst, ps_, psf, pa, pb_ = cand[r], best[r], pos[r], posf[r], paf[r], pbf[r]
            st = st3[tt % 2]
            k.dma('sp', x2.ap, x2_s.ap[ts, :], reads=[x2_s.res], writes=[x2.res])
            k.op('act', 'activation', R=[x2.res], W=[junk2.res, st.res], out=junk2.ap, in_=x2.ap, func=AF.Square, accum_out=st.ap[:, 0:1])
            k.op('act', 'activation', R=[st.res], W=[st.res], out=st.ap[:, 1:2], in_=st.ap[:, 0:1], func=AF.Ln, scale=1.0 / D, bias=EPS)
            k.op('act', 'activation', R=[st.res], W=[st.res], out=st.ap[:, 1:2], in_=st.ap[:, 1:2], func=AF.Exp, scale=-0.5)
            k.op('dve', 'tensor_scalar', R=[x2.res, st.res], W=[xn_b.res], out=xn_b.ap, in0=x2.ap, scalar1=st.ap[:, 1:2], scalar2=None, op0=ALU.mult)
            k.op('dve', 'scalar_tensor_tensor', R=[x2.res, st.res, ffn_bc.res], W=[h3_.res], out=h3_.ap, in0=x2.ap, scalar=st.ap[:, 1:2],
                 in1=ffn_bc.ap, op0=ALU.mult, op1=ALU.mult)
            pb = banks[trot[0] % 2]
            trot[0] += 1
            pview = pb.ap.bitcast(BF16).rearrange("p (c t) -> p c t", c=DC)
            for c in range(DC):
                k.op('pe', 'transpose', R=[xn_b.res, ident_b.res], W=[pb.res], out=pview[:, c, :], in_=xn_b.ap[:, c * P:(c + 1) * P], identity=ident_b.ap)
            k.op('dve', 'tensor_tensor', R=[pb.res, g_ffn.res], W=[hT3.res], out=hT3.ap, in0=pview, in1=g_ffn.ap, op=ALU.mult)
            for bq in range(4):
                b = banks[2 + bq]
                for cc in range(4):
                    c = bq * 4 + cc
                    for dc in range(DC):
                        k.op('pe', 'matmul', R=[pwq.res, hT3.res], W=[b.res], out=b.ap[:, cc * P:(cc + 1) * P], lhsT=pwq.ap[:, dc, c * P:(c + 1) * P],
                             rhs=hT3.ap[:, dc, :], start=(dc == 0), stop=(dc == DC - 1))
                k.op('act', 'copy', R=[b.res], W=[pq.res], out=pq.ap[:, bq * 4:(bq + 1) * 4, :], in_=b.ap.rearrange("p (c t) -> p c t", c=4))
            for bq in range(4):
                b = banks[2 + (bq + 2) % 4 + 0] if False else banks[6 + bq % 2] if False else banks[2 + bq]
                for cc in range(4):
                    hp = bq * 4 + cc
                    k.op('pe', 'matmul', R=[pq.res, skT.res], W=[b.res], out=b.ap[:, cc * P:(cc + 1) * P], lhsT=pq.ap[:, hp, :], rhs=skT.ap[:, hp, :],
                         start=True, stop=True)
                k.op('act', 'copy', R=[b.res], W=[ssb.res], out=ssb.ap[:, bq * 4:(bq + 1) * 4, :], in_=b.ap.rearrange("p (c t) -> p c t", c=4))
            for hp in range(16):
                k.op('dve', 'max', R=[ssb.res], W=[mx.res], out=mx.ap[:, hp, 0:8], in_=ssb.ap[:, hp, :])
                k.op('dve', 'match_replace', R=[mx.res, ssb.res], W=[s2b_.res], out=s2b_.ap[:, hp, :], in_to_replace=mx.ap[:, hp, 0:8],
                     in_values=ssb.ap[:, hp, :], imm_value=NEG)
                k.op('dve', 'max', R=[s2b_.res], W=[mx.res], out=mx.ap[:, hp, 8:16], in_=s2b_.ap[:, hp, :])
                k.op('dve', 'max_index', R=[mx.res, ssb.res], W=[ix.res], out=ix.ap[:, hp, 0:8], in_max=mx.ap[:, hp, 0:8], in_values=ssb.ap[:, hp, :])
                k.op('dve', 'max_index', R=[mx.res, ssb.res], W=[ix.res], out=ix.ap[:, hp, 8:16], in_max=mx.ap[:, hp, 8:16], in_values=ssb.ap[:, hp, :])
            k.op('dve', 'tensor_copy', R=[ix.res], W=[ixf_.res], out=ixf_.ap, in_=ix.ap)
            mx4 = mx.ap.rearrange("p (h two) k -> p h two k", two=2)
            cd4 = cd.ap.rearrange("p h (a b) -> p h a b", a=16)
            k.op('dve', 'tensor_tensor', R=[mx.res], W=[cd.res], out=cd4, in0=bc_last(mx4[:, :, 0, :], 16), in1=bc_mid(mx4[:, :, 1, :], 2, 16), op=ALU.add)
            for hh in range(PH):
                k.op('dve', 'max', R=[cd.res], W=[bst.res], out=bst.ap[:, hh, 0:8], in_=cd.ap[:, hh, :])
                k.op('dve', 'match_replace', R=[bst.res, cd.res], W=[cand2.res], out=cand2.ap[:, hh, :], in_to_replace=bst.ap[:, hh, 0:8],
                     in_values=cd.ap[:, hh, :], imm_value=NEG)
                k.op('dve', 'max', R=[cand2.res], W=[bst.res], out=bst.ap[:, hh, 8:16], in_=cand2.ap[:, hh, :])
                k.op('dve', 'max_index', R=[bst.res, cd.res], W=[ps_.res], out=ps_.ap[:, hh, 0:8], in_max=bst.ap[:, hh, 0:8], in_values=cd.ap[:, hh, :])
                k.op('dve', 'max_index', R=[bst.res, cd.res], W=[ps_.res], out=ps_.ap[:, hh, 8:16], in_max=bst.ap[:, hh, 8:16], in_values=cd.ap[:, hh, :])
            gs_, gt_ = gst_[r], gate[tt % 2]
            k.op('dve', 'tensor_tensor', R=[bst.res], W=[gt_.res], out=gt_.ap, in0=bst.ap, in1=bc_last(bst.ap[:, :, 0], 16), op=ALU.subtract)
            k.op('act', 'activation', R=[gt_.res], W=[gt_.res], out=gt_.ap, in_=gt_.ap, func=AF.Exp)
            k.op('dve', 'tensor_reduce', R=[gt_.res], W=[gs_.res], out=gs_.ap[:, 0:8], in_=gt_.ap, axis=AX.X, op=ALU.add)
            k.op('dve', 'reciprocal', R=[gs_.res], W=[gs_.res], out=gs_.ap[:, 8:16], in_=gs_.ap[:, 0:8])
            k.op('dve', 'tensor_tensor', R=[gt_.res, gs_.res], W=[gt_.res], out=gt_.ap, in0=gt_.ap, in1=bc_last(gs_.ap[:, 8:16], 16), op=ALU.mult)
            k.op('dve', 'tensor_copy', R=[ps_.res], W=[psf.res], out=psf.ap, in_=ps_.ap)
            thr_bc = bc_mid(bc_mid(thr16.ap, 1, 16), 1, PH)
            iota_bc = bc_mid(bc_mid(iota16.ap, 1, 16), 1, PH)
            k.op('dve', 'tensor_tensor', R=[psf.res, thr16.res], W=[oh.res], out=oh.ap, in0=bc_last(psf.ap, 16), in1=thr_bc, op=ALU.is_ge)
            k.op('dve', 'tensor_reduce', R=[oh.res], W=[pa.res], out=pa.ap, in_=oh.ap, axis=AX.X, op=ALU.add)
            k.op('dve', 'scalar_tensor_tensor', R=[pa.res, psf.res], W=[pb_.res], out=pb_.ap, in0=pa.ap, scalar=-16.0, in1=psf.ap, op0=ALU.mult, op1=ALU.add)
            ixf4 = ixf_.ap.rearrange("p (h two) k -> p h two k", two=2)
            for which, pidx, dsti in ((0, pa, i1s[r]), (1, pb_, i2s[r])):
                k.op('dve', 'tensor_tensor', R=[pidx.res, iota16.res], W=[oh.res], out=oh.ap, in0=bc_last(pidx.ap, 16), in1=iota_bc, op=ALU.is_equal)
                k.op('dve', 'tensor_tensor', R=[oh.res, ixf_.res], W=[oh2.res], out=oh2.ap, in0=oh.ap, in1=bc_mid(ixf4[:, :, which, :], 2, 16), op=ALU.mult)
                k.op('dve', 'tensor_reduce', R=[oh2.res], W=[dsti.res], out=dsti.ap, in_=oh2.ap, axis=AX.X, op=ALU.add)
            ef, ei = eidf[r], eid[tt % 2]
            k.op('dve', 'scalar_tensor_tensor', R=[i1s[r].res, i2s[r].res], W=[ef.res], out=ef.ap, in0=i1s[r].ap.rearrange("p h k -> p (h k)"),
                 scalar=float(PK), in1=i2s[r].ap.rearrange("p h k -> p (h k)"), op0=ALU.mult, op1=ALU.add)
            k.op('dve', 'tensor_copy', R=[ef.res], W=[ei.res], out=ei.ap, in_=ef.ap)

        def expert_group(tt, kg):
            r = 0
            h3_, ei, gt_ = h3[tt % 2], eid[tt % 2], gate[tt % 2]
            gflat = gt_.ap.rearrange("p h k -> p (h k)")
            sd, av = sdg[kg % 2], avg[kg % 2]
            slots = []
            for kk in range(kg * GK, (kg + 1) * GK):
                u_ = uvg[gsl[0] % NSL]
                gsl[0] += 1
                slots.append(u_)
                k.dma('pool', u_.ap, uv_s.ap, reads=[ei.res, uv_s.res], writes=[u_.res],
                      indirect=bass.IndirectOffsetOnAxis(ap=ei.ap[:, kk:kk + 1], axis=0))
                k.op('dve', 'scalar_tensor_tensor', R=[u_.res, h3_.res], W=[junk2.res, sd.res], out=junk2.ap, in0=u_.ap[:, 0:D], scalar=1.0,
                     in1=h3_.ap, op0=ALU.mult, op1=ALU.mult, accum_out=sd.ap[:, kk - kg * GK:kk - kg * GK + 1])
            k.op('act', 'activation', R=[sd.res], W=[av.res], out=av.ap, in_=sd.ap, func=AF.Gelu)
            k.op('dve', 'tensor_tensor', R=[av.res, gt_.res], W=[av.res], out=av.ap, in0=av.ap, in1=gflat[:, kg * GK:(kg + 1) * GK], op=ALU.mult)
            dg = dgs[kg % 2]
            k.op('dve', 'tensor_tensor', R=[ident_b.res, av.res], W=[dg.res], out=dg.ap, in0=bc_mid(ident_b.ap, 1, GK), in1=bc_last(av.ap, P), op=ALU.mult)
            for i_, u_ in enumerate(slots):
                first, last = (kg == 0 and i_ == 0), (kg == NGR - 1 and i_ == GK - 1)
                for hf in range(2):
                    yb = banks[6 + hf]
                    k.op('pe', 'matmul', R=[dg.res, u_.res], W=[yb.res], out=yb.ap, lhsT=dg.ap[:, i_, :], rhs=u_.ap[:, D + hf * 512:D + (hf + 1) * 512],
                         start=first, stop=last)

        def tail_part(tt):
            r = 0
            ts = slice(tt * P, (tt + 1) * P)
            x2, ya, st = x2t[tt % 2], yacc[tt % 2], st3[tt % 2]
            for hf in range(2):
                yb = banks[6 + hf]
                k.op('dve', 'tensor_tensor', R=[yb.res, x2.res], W=[ya.res], out=ya.ap[:, hf * 512:(hf + 1) * 512], in0=yb.ap,
                     in1=x2.ap[:, hf * 512:(hf + 1) * 512], op=ALU.add)
            k.op('act', 'activation', R=[ya.res], W=[junk2.res, st.res], out=junk2.ap, in_=ya.ap, func=AF.Square, accum_out=st.ap[:, 2:3])
            k.op('act', 'activation', R=[st.res], W=[st.res], out=st.ap[:, 3:4], in_=st.ap[:, 2:3], func=AF.Ln, scale=1.0 / D, bias=EPS)
            k.op('act', 'activation', R=[st.res], W=[st.res], out=st.ap[:, 3:4], in_=st.ap[:, 3:4], func=AF.Exp, scale=-0.5)
            k.op('dve', 'scalar_tensor_tensor', R=[ya.res, st.res, fin_bc.res], W=[ya.res], out=ya.ap, in0=ya.ap, scalar=st.ap[:, 3:4], in1=fin_bc.ap,
                 op0=ALU.mult, op1=ALU.mult)
            o_ = ya
            k.dma('sp', out_d.ap[ts, :], o_.ap, reads=[o_.res], writes=[out_d.res])

        k.play(k.record(lambda: front_part(0)))
        for tt in range(NT):
            nxt = k.record(lambda: front_part(tt + 1)) if tt + 1 < NT else []
            per = -(-len(nxt) // NGR) if nxt else 0
            for kg in range(NGR):
                k.play(k.record(lambda: expert_group(tt, kg)))
                k.play(nxt[kg * per:(kg + 1) * per])
            k.play(k.record(lambda: tail_part(tt)))
        cx.pop()

    finals = []
    for b in (qkm_s, sbq_s, sbk_s, vm_s, og_s, vs_s, gates_s, mixed_s, x2_s, out_d):
        if b is out_d or b.res.name in debug:
            finals.extend(b.res.w)
    k.emit(finals)
    return nc, k


def prep_shared(inp):
    f = lambda a: np.ascontiguousarray(np.asarray(a, dtype=np.float32))
    g = lambda v: f(v).reshape(DC, P).T
    mixed_g = np.concatenate([f(inp['mlstm_norm_g'][0]), f(inp['sb_norm_g'][0])])
    gvec = np.stack([g(inp['mix_norm_g'][0]), g(inp['xattn_norm_g'][0]), g(inp['mem_norm_g'][0]),
                     g(inp['ffn_norm_g'][0]), g(mixed_g)], axis=1)
    sh = {
        'w_in': f(inp['w_in'][0]), 'w_out': f(inp['w_out'][0]), 'xattn_wq': f(inp['xattn_wq'][0]),
        'xattn_wkv': f(inp['xattn_wkv'][0]), 'xattn_wo': f(inp['xattn_wo'][0]), 'peer_wq': f(inp['peer_wq'][0]),
        'subkeysT': f(np.transpose(f(inp['peer_subkeys'][0]).reshape(16, P, P), (2, 0, 1))),
        'peer_u': f(inp['peer_u'][0]), 'peer_v': f(inp['peer_v'][0]),
        'gvec': f(gvec),
        'grow': f(np.stack([f(inp['ffn_norm_g'][0]), f(inp['final_norm_g'])])),
        'conv_wT': f(np.transpose(f(inp['conv_w'][0]).reshape(4, DC, P), (2, 1, 0))),
        'conv_bT': g(inp['conv_b'][0]),
        'gate_b': f(np.stack([f(inp['igate_b'][0]), f(inp['fgate_b'][0])], axis=1)),
    }
    return sh


_CACHE = {}


def kernel(**inputs):
    x = np.asarray(inputs['x'], dtype=np.float32)
    mem = np.asarray(inputs['mem'], dtype=np.float32)
    B, S, _ = x.shape
    sh = prep_shared(inputs)
    if S not in _CACHE:
        _CACHE[S] = build(S)[0]
    nc = _CACHE[S]
    in_maps = []
    for b in range(B):
        m = dict(sh)
        m['x'] = np.ascontiguousarray(x[b])
        m['mem'] = np.ascontiguousarray(mem[b])
        in_maps.append(m)
    res = run_bass_kernel_spmd(nc, in_maps, core_ids=list(range(B)))
    return np.stack([np.asarray(r['out'], dtype=np.float32) for r in res.results], axis=0)
```
